# Optimizing a Trainium2 kernel written in Bass

```python
import math
import jax
import jax.numpy as jnp
from jax import lax
import numpy as np

D_MODEL = 1024
BATCH = 32
SEQ = 256
DEPTH = 1
DEC_BATCH = 8
DEC_SEQ = 1024
PAST_LEN = 512

GRID_W = 64
N_HEADS = 8
HEAD_DIM = D_MODEL // (2 * N_HEADS)
ATTN_W = N_HEADS * 2 * HEAD_DIM
POOL_GROUPS = 4
POOL_WINDOWS = (2, 4, 8, 16)
POOL_W = D_MODEL // 2
POOL_GROUP_W = POOL_W // POOL_GROUPS
N_BRANCHES = 2
IN_W = 3 * ATTN_W + POOL_W + N_BRANCHES * D_MODEL
ROPE_THETA = 10000.0
ROPE_AXIS_DIM = HEAD_DIM // 2
ROPE_PAIRS = ROPE_AXIS_DIM // 2
Q_BLOCK = 128
N_GROUPS = 4
EXPERTS_PER_GROUP = 4
N_EXPERTS = N_GROUPS * EXPERTS_PER_GROUP
TOP_K_IN_GROUP = 2
D_EXPERT = D_MODEL // 2
ADA_CHUNKS = 6
EPS = 1e-6

kernel_name = 'hybrid_diffattn_pool_hmoe_prefix_step'


def _rmsnorm(x, g):
    xf = x.astype(jnp.float32)
    y = xf * lax.rsqrt(jnp.mean(xf * xf, axis=-1, keepdims=True) + EPS)
    return (y * g.astype(jnp.float32)).astype(x.dtype)


def _adaln(cond, w, b):
    m = jax.nn.silu(cond) @ w + b
    return m.reshape(cond.shape[0], ADA_CHUNKS, D_MODEL)


def _modulate(h, shift, scale):
    return h * (1 + scale[:, None, :]) + shift[:, None, :]


def _axial_rope_tables(n_tokens):
    rows = n_tokens // GRID_W
    row_ids = jnp.repeat(jnp.arange(rows, dtype=jnp.float32), GRID_W)
    col_ids = jnp.tile(jnp.arange(GRID_W, dtype=jnp.float32), rows)
    inv_freq = jnp.power(ROPE_THETA, -jnp.arange(ROPE_PAIRS, dtype=jnp.float32) / ROPE_PAIRS)
    ang_r = row_ids[:, None] * inv_freq[None, :]
    ang_c = col_ids[:, None] * inv_freq[None, :]
    ang = jnp.concatenate([ang_r, ang_r, ang_c, ang_c], axis=-1)
    return jnp.cos(ang), jnp.sin(ang)


def _rotate_half_axial(x):
    xs = x.reshape(x.shape[:-1] + (2, 2, ROPE_PAIRS))
    return jnp.concatenate([-xs[..., 1:, :], xs[..., :1, :]], axis=-2).reshape(x.shape)


def _apply_rope(x, cos, sin):
    xf = x.astype(jnp.float32)
    c = cos[None, :, None, None, :]
    s = sin[None, :, None, None, :]
    return (xf * c + _rotate_half_axial(xf) * s).astype(x.dtype)


def _diff_attention(q, k, v, lam):
    B, N = q.shape[0], q.shape[1]
    nb = N // Q_BLOCK
    qb = jnp.moveaxis(q.reshape(B, nb, Q_BLOCK, N_HEADS, 2, HEAD_DIM), 1, 0)
    kf = k.astype(jnp.float32)
    vf = v.astype(jnp.float32)
    scale = HEAD_DIM ** -0.5

    def one_block(qblk):
        s = jnp.einsum('bqhid,bkhid->bihqk', qblk.astype(jnp.float32), kf) * scale
        a = jax.nn.softmax(s, axis=-1)
        w = a[:, 0] - lam * a[:, 1]
        return jnp.einsum('bhqk,bkhe->bqhe', w, vf)

    o = lax.map(one_block, qb)
    return jnp.moveaxis(o, 0, 1).reshape(B, N, N_HEADS, 2 * HEAD_DIM).astype(v.dtype)


def _pool_mixer(p, pool_w, pool_scale):
    B, N, _ = p.shape
    pf = p.astype(jnp.float32).reshape(B, N, POOL_GROUPS, POOL_GROUP_W)
    csum = jnp.concatenate([jnp.zeros((B, 1, POOL_GROUPS, POOL_GROUP_W), jnp.float32),
                            jnp.cumsum(pf, axis=1)], axis=1)
    t = jnp.arange(N)
    outs = []
    for gi, w in enumerate(POOL_WINDOWS):
        half = w // 2
        lo = jnp.clip(t - half, 0, N)
        hi = jnp.clip(t + half, 0, N)
        cs = csum[:, :, gi]
        sums = cs[:, hi] - cs[:, lo]
        cnt = (hi - lo).astype(jnp.float32)[None, :, None]
        outs.append(sums / cnt - pf[:, :, gi])
    pooled = jnp.stack(outs, axis=2)
    mixed = jnp.einsum('bngc,gce->bnge', pooled, pool_w.astype(jnp.float32))
    return (mixed.reshape(B, N, POOL_W) * pool_scale.astype(jnp.float32)).astype(p.dtype)


def _hier_moe(h, lp):
    B, N, D = h.shape
    t = h.reshape(B * N, D)
    g_logits = (t @ lp['router_group_w'] + lp['router_group_b']).astype(jnp.float32)
    g_prob = jax.nn.softmax(g_logits, axis=-1)
    g_idx = jnp.argmax(g_logits, axis=-1)
    g_w = jnp.max(g_prob, axis=-1, keepdims=True)
    e_logits = (t @ lp['router_expert_w'] + lp['router_expert_b']).astype(jnp.float32)
    e_logits = e_logits.reshape(B * N, N_GROUPS, EXPERTS_PER_GROUP)
    e_sel = e_logits[jnp.arange(B * N), g_idx]
    top_v, top_i = lax.top_k(e_sel, TOP_K_IN_GROUP)
    wts = jax.nn.softmax(top_v, axis=-1) * g_w
    ids = g_idx[:, None] * EXPERTS_PER_GROUP + top_i
    gates = jnp.sum(jax.nn.one_hot(ids, N_EXPERTS, dtype=jnp.float32) * wts[..., None], axis=1)
    a = jnp.einsum('td,edf->tef', t, lp['expert_w_gate'])
    u = jnp.einsum('td,edf->tef', t, lp['expert_w_up'])
    hid = jax.nn.silu(a) * u * gates[..., None].astype(h.dtype)
    out = jnp.einsum('tef,efd->td', hid, lp['expert_w_down'])
    return out.reshape(B, N, D)


def _layer(x, mod, lp, lambda_init, rope, ctx_k, ctx_v):
    B, N, _ = x.shape
    shift1, scale1, gate1, shift2, scale2, gate2 = [mod[:, i] for i in range(ADA_CHUNKS)]
    h = _modulate(_rmsnorm(x, lp['norm1_g']), shift1, scale1)
    z = h @ lp['w_in']
    q, k, v, p, gl = jnp.split(z, [ATTN_W, 2 * ATTN_W, 3 * ATTN_W, 3 * ATTN_W + POOL_W], axis=-1)
    q = _rmsnorm(q.reshape(B, N, N_HEADS, 2, HEAD_DIM), lp['q_norm_g'])
    k = _rmsnorm(k.reshape(B, N, N_HEADS, 2, HEAD_DIM), lp['k_norm_g'])
    v = v.reshape(B, N, N_HEADS, 2 * HEAD_DIM)
    if rope is not None:
        q = _apply_rope(q, rope[0], rope[1])
        k = _apply_rope(k, rope[0], rope[1])
    if ctx_k is not None:
        k_all = jnp.concatenate([k, ctx_k.astype(k.dtype)], axis=1)
        v_all = jnp.concatenate([v, ctx_v.astype(v.dtype)], axis=1)
    else:
        k_all, v_all = k, v
    lam = (jnp.exp(jnp.sum(lp['lambda_q1'].astype(jnp.float32) * lp['lambda_k1'].astype(jnp.float32)))
           - jnp.exp(jnp.sum(lp['lambda_q2'].astype(jnp.float32) * lp['lambda_k2'].astype(jnp.float32)))
           + lambda_init)
    o = _diff_attention(q, k_all, v_all, lam)
    o = _rmsnorm(o, lp['subln_g']) * (1.0 - lambda_init)
    attn_out = o.reshape(B, N, ATTN_W) @ lp['w_br_attn']
    pool_out = _pool_mixer(p, lp['pool_w'], lp['pool_scale']) @ lp['w_br_pool']
    g = jax.nn.sigmoid((gl + lp['b_gate']).astype(jnp.float32)).astype(x.dtype)
    g = g.reshape(B, N, N_BRANCHES, D_MODEL)
    merged = g[..., 0, :] * attn_out + g[..., 1, :] * pool_out
    x = x + gate1[:, None, :] * (merged @ lp['w_out'])
    h2 = _modulate(_rmsnorm(x, lp['norm2_g']), shift2, scale2)
    x = x + gate2[:, None, :] * _hier_moe(h2, lp)
    return x, k, v


def setup_inputs(seed: int = 0) -> dict:
    key = jax.random.key(seed)
    ks = jax.random.split(key, 32)

    def nrm(k, shape, s):
        return jax.random.normal(k, shape, jnp.float32) * s

    def gain(k, shape):
        return 1.0 + nrm(k, shape, 0.02)

    D = D_MODEL
    return {
        'x_prompt': nrm(ks[0], (BATCH, SEQ, D), 1.0),
        'x_sample': nrm(ks[1], (DEC_BATCH, DEC_SEQ, D), 1.0),
        'c': nrm(ks[2], (DEC_BATCH, D), 1.0),
        'cache_k': nrm(ks[3], (DEC_BATCH, DEPTH, PAST_LEN, N_HEADS, 2 * HEAD_DIM), 1.0),
        'cache_v': nrm(ks[4], (DEC_BATCH, DEPTH, PAST_LEN, N_HEADS, 2 * HEAD_DIM), 1.0),
        'c_ctx': nrm(ks[5], (D,), 1.0),
        'w_ada': nrm(ks[6], (DEPTH, D, ADA_CHUNKS * D), 0.5 * D ** -0.5),
        'b_ada': nrm(ks[7], (DEPTH, ADA_CHUNKS * D), 0.02),
        'norm1_g': gain(ks[8], (DEPTH, D)),
        'w_in': nrm(ks[9], (DEPTH, D, IN_W), D ** -0.5),
        'b_gate': nrm(ks[10], (DEPTH, N_BRANCHES * D), 0.02),
        'q_norm_g': gain(ks[11], (DEPTH, HEAD_DIM)),
        'k_norm_g': gain(ks[12], (DEPTH, HEAD_DIM)),
        'lambda_q1': nrm(ks[13], (DEPTH, HEAD_DIM), 0.1),
        'lambda_k1': nrm(ks[14], (DEPTH, HEAD_DIM), 0.1),
        'lambda_q2': nrm(ks[15], (DEPTH, HEAD_DIM), 0.1),
        'lambda_k2': nrm(ks[16], (DEPTH, HEAD_DIM), 0.1),
        'subln_g': gain(ks[17], (DEPTH, 2 * HEAD_DIM)),
        'pool_w': nrm(ks[18], (DEPTH, POOL_GROUPS, POOL_GROUP_W, POOL_GROUP_W), POOL_GROUP_W ** -0.5),
        'pool_scale': gain(ks[19], (DEPTH, POOL_W)),
        'w_br_attn': nrm(ks[20], (DEPTH, ATTN_W, D), ATTN_W ** -0.5),
        'w_br_pool': nrm(ks[21], (DEPTH, POOL_W, D), POOL_W ** -0.5),
        'w_out': nrm(ks[22], (DEPTH, D, D), D ** -0.5),
        'norm2_g': gain(ks[23], (DEPTH, D)),
        'router_group_w': nrm(ks[24], (DEPTH, D, N_GROUPS), D ** -0.5),
        'router_group_b': nrm(ks[25], (DEPTH, N_GROUPS), 0.01),
        'router_expert_w': nrm(ks[26], (DEPTH, D, N_EXPERTS), D ** -0.5),
        'router_expert_b': nrm(ks[27], (DEPTH, N_EXPERTS), 0.01),
        'expert_w_gate': nrm(ks[28], (DEPTH, N_EXPERTS, D, D_EXPERT), D ** -0.5),
        'expert_w_up': nrm(ks[29], (DEPTH, N_EXPERTS, D, D_EXPERT), D ** -0.5),
        'expert_w_down': nrm(ks[30], (DEPTH, N_EXPERTS, D_EXPERT, D), D_EXPERT ** -0.5),
    }


def reference(x_prompt, x_sample, c, cache_k, cache_v, c_ctx, w_ada, b_ada, norm1_g, w_in, b_gate,
              q_norm_g, k_norm_g, lambda_q1, lambda_k1, lambda_q2, lambda_k2, subln_g, pool_w, pool_scale,
              w_br_attn, w_br_pool, w_out, norm2_g, router_group_w, router_group_b, router_expert_w,
              router_expert_b, expert_w_gate, expert_w_up, expert_w_down):
    rope = _axial_rope_tables(x_sample.shape[1])
    y_prompt = x_prompt
    y_sample = x_sample
    past_len = cache_k.shape[2]
    new_k = []
    new_v = []
    for l in range(DEPTH):
        lp = dict(norm1_g=norm1_g[l], w_in=w_in[l], b_gate=b_gate[l], q_norm_g=q_norm_g[l],
                  k_norm_g=k_norm_g[l], lambda_q1=lambda_q1[l], lambda_k1=lambda_k1[l],
                  lambda_q2=lambda_q2[l], lambda_k2=lambda_k2[l], subln_g=subln_g[l], pool_w=pool_w[l],
                  pool_scale=pool_scale[l], w_br_attn=w_br_attn[l], w_br_pool=w_br_pool[l], w_out=w_out[l],
                  norm2_g=norm2_g[l], router_group_w=router_group_w[l], router_group_b=router_group_b[l],
                  router_expert_w=router_expert_w[l], router_expert_b=router_expert_b[l],
                  expert_w_gate=expert_w_gate[l], expert_w_up=expert_w_up[l], expert_w_down=expert_w_down[l])
        lambda_init = 0.8 - 0.6 * math.exp(-0.3 * l)
        mod_ctx = _adaln(c_ctx[None, :], w_ada[l], b_ada[l])
        mod_lat = _adaln(c, w_ada[l], b_ada[l])
        y_prompt, k_ctx, v_ctx = _layer(y_prompt, mod_ctx, lp, lambda_init, None, None, None)
        new_k.append(k_ctx.reshape(k_ctx.shape[0], k_ctx.shape[1], N_HEADS, 2 * HEAD_DIM))
        new_v.append(v_ctx)
        ck = cache_k[:, l].reshape(cache_k.shape[0], past_len, N_HEADS, 2, HEAD_DIM)
        cv = cache_v[:, l]
        y_sample, _, _ = _layer(y_sample, mod_lat, lp, lambda_init, rope, ck, cv)
    new_cache_k = jnp.stack(new_k, axis=1)
    new_cache_v = jnp.stack(new_v, axis=1)
    return (y_prompt, y_sample, new_cache_k, new_cache_v)
```

```python
import numpy as np
import concourse.bass as bass
import concourse.mybir as mybir
from concourse.bass_utils import run_bass_kernel_spmd

F32 = mybir.dt.float32
BF16 = mybir.dt.bfloat16
I32 = mybir.dt.int32
U32 = mybir.dt.uint32
AF = mybir.ActivationFunctionType
ALU = mybir.AluOpType
AX = mybir.AxisListType

EPS = 1e-6
NCORES = 8
LAMBDA_INIT = 0.8 - 0.6 * 1.0
BIG = 1.0e9

REGION_OF = {
    'vt': 'X1', 'x1': 'X1',
    'qT': 'ACC', 'mrgT': 'ACC', 'acc': 'ACC', 'st2': 'ACC', 'ost': 'ACC',
    'kT': 'K', 'silu': 'K', 'hid': 'K',
    'pl': 'PL',
    'ew0': 'X1', 'fr': 'X1', 'ew1': 'ACC', 'mg': 'K', 'hT': 'H', 'yst': 'H',
}


class Sched:
    def __init__(self, nc, scratch):
        self.nc = nc
        self.ops = []
        self.lastw = {}
        self.readers = {}
        self.scratch = scratch
        self.seen = {}

    def add(self, eng, fn, reads=(), writes=(), dma=None):
        writes = list(writes) + [k for k in reads if isinstance(k, tuple) and k[0] == 'ps' and k not in writes]
        reads = [k for k in reads if not (isinstance(k, tuple) and k[0] == 'ps')]
        for k in reads + writes:
            nm = k[0] if isinstance(k, tuple) else k
            rg = REGION_OF.get(nm)
            if rg is not None:
                self.seen.setdefault(rg, set()).add(k)
                fk = ('fence', rg)
                if fk not in reads:
                    reads.append(fk)
        idx = len(self.ops)
        deps = set()
        for k in reads:
            w = self.lastw.get(k)
            if w is not None:
                deps.add(w)
        for k in writes:
            w = self.lastw.get(k)
            if w is not None:
                deps.add(w)
            for r in self.readers.get(k, ()):
                deps.add(r)
        deps.discard(idx)
        self.ops.append(dict(eng=eng, fn=fn, deps=deps, dma=dma))
        for k in reads:
            self.readers.setdefault(k, []).append(idx)
        for k in writes:
            self.lastw[k] = idx
            self.readers[k] = []
        return idx

    def fence(self, regions=('X1', 'ACC', 'K', 'PL', 'H')):
        for rg in regions:
            keys = list(self.seen.get(rg, ())) + [('fence', rg), 'fscr']
            sc = self.scratch
            idx = len(self.ops)
            deps = set()
            for k in keys:
                w = self.lastw.get(k)
                if w is not None:
                    deps.add(w)
                for r in self.readers.get(k, ()):
                    deps.add(r)
            self.ops.append(dict(eng='dve', fn=(lambda e: e.memset(sc, 0.0)), deps=deps, dma=None))
            for k in keys:
                self.lastw[k] = idx
                self.readers[k] = []

    def mm(self, out, lhsT, rhs, start, stop, reads, writes, skip=False):
        if skip:
            return self.add('pe', lambda e: e.matmul(out, lhsT, rhs, start=start, stop=stop, skip_group_check=True),
                            reads, writes)
        return self.add('pe', lambda e: e.matmul(out, lhsT, rhs, start=start, stop=stop), reads, writes)

    def tr(self, out, in_, ident, reads, writes):
        return self.add('pe', lambda e: e.transpose(out, in_, ident), reads, writes)

    def act(self, out, in_, func, reads, writes, bias=None, scale=None, accum_out=None):
        kw = {}
        if bias is not None:
            kw['bias'] = bias
        if scale is not None:
            kw['scale'] = scale
        if accum_out is not None:
            kw['accum_out'] = accum_out
        return self.add('act', lambda e: e.activation(out, in_, func, **kw), reads, writes)

    def tt(self, eng, out, in0, in1, op, reads, writes):
        return self.add(eng, lambda e: e.tensor_tensor(out, in0, in1, op), reads, writes)

    def ts(self, eng, out, in0, s1, s2, op0, op1, reads, writes):
        if op1 is None:
            return self.add(eng, lambda e: e.tensor_scalar(out, in0, s1, None, op0), reads, writes)
        return self.add(eng, lambda e: e.tensor_scalar(out, in0, s1, s2, op0, op1), reads, writes)

    def stt(self, out, in0, scalar, in1, op0, op1, reads, writes):
        return self.add('dve', lambda e: e.scalar_tensor_tensor(out, in0, scalar, in1, op0, op1), reads, writes)

    def red(self, out, in_, op, reads, writes):
        return self.add('dve', lambda e: e.tensor_reduce(out, in_, AX.X, op), reads, writes)

    def recip(self, out, in_, reads, writes):
        return self.add('dve', lambda e: e.reciprocal(out, in_), reads, writes)

    def cp(self, eng, out, in_, reads, writes):
        if eng == 'act':
            return self.add('act', lambda e: e.copy(out, in_), reads, writes)
        return self.add(eng, lambda e: e.tensor_copy(out, in_), reads, writes)

    def memset(self, eng, ap, val, writes):
        return self.add(eng, lambda e: e.memset(ap, val), (), writes)

    def dma(self, q, out, in_, reads, writes, key, slow=False):
        if slow:
            return self.add(q, lambda e: e.dma_start(out=out, in_=in_, allow_slow_non_contiguous=True),
                            reads, writes, dma=key)
        return self.add(q, lambda e: e.dma_start(out=out, in_=in_), reads, writes, dma=key)

    def emit(self, final_wait_eng='sp'):
        nc = self.nc
        ops = self.ops
        n = len(ops)
        has_dep = [False] * n
        for i, o in enumerate(ops):
            latest = {}
            keep = set()
            for d in o['deps']:
                od = ops[d]
                if od['dma'] is not None:
                    keep.add(d)
                    continue
                if od['eng'] == 'pe' and o['eng'] == 'pe' and o['dma'] is None:
                    continue
                if d > latest.get(od['eng'], -1):
                    latest[od['eng']] = d
            keep.update(latest.values())
            o['deps'] = keep
            for d in keep:
                has_dep[d] = True
        eng_names = ['sp', 'act', 'pool', 'dve', 'pe']
        eng_sem = {e: nc.alloc_semaphore(name='sem_' + e) for e in eng_names}
        dma_sems = {}
        dma_cnt = {}
        eng_cnt = {e: 0 for e in eng_names}
        sig = [None] * n
        for i, o in enumerate(ops):
            if o['dma'] is not None:
                k = o['dma']
                if k not in dma_sems:
                    dma_sems[k] = nc.alloc_semaphore(name='dsem%d' % len(dma_sems))
                    dma_cnt[k] = 0
                dma_cnt[k] += 16
                sig[i] = (dma_sems[k], dma_cnt[k], 16)
            elif has_dep[i]:
                eng_cnt[o['eng']] += 1
                sig[i] = (eng_sem[o['eng']], eng_cnt[o['eng']], 1)
        self.n_sems = len(dma_sems) + 5
        self.eng_cnt = eng_cnt
        streams = {e: [i for i, o in enumerate(ops) if o['eng'] == e] for e in eng_names}
        finals = [(dma_sems[k], dma_cnt[k]) for k in dma_sems]

        def run_stream(ename, eng):
            waited = {}
            for i in streams[ename]:
                o = ops[i]
                need = {}
                for d in o['deps']:
                    s, v, _ = sig[d]
                    if v > need.get(s.num, (None, 0))[1]:
                        need[s.num] = (s, v)
                for num in sorted(need):
                    s, v = need[num]
                    if waited.get(num, 0) < v:
                        eng.wait_ge(s, v)
                        waited[num] = v
                ins = o['fn'](eng)
                if sig[i] is not None:
                    ins.then_inc(sig[i][0], sig[i][2])
            if ename == final_wait_eng:
                for s, v in finals:
                    if waited.get(s.num, 0) < v:
                        eng.wait_ge(s, v)

        with nc.Block() as block:
            @block.sync
            def _(e):
                run_stream('sp', e)

            @block.scalar
            def _(e):
                run_stream('act', e)

            @block.gpsimd
            def _(e):
                run_stream('pool', e)

            @block.vector
            def _(e):
                run_stream('dve', e)

            @block.tensor
            def _(e):
                run_stream('pe', e)


class WRing:
    R = 10

    def __init__(self, S, ring, units):
        self.S = S
        self.ring = ring
        self.units = units
        self.next_dma = 0
        self.next_acq = 0
        self.rel = set()
        self.pump()

    def pump(self):
        while self.next_dma < len(self.units):
            j = self.next_dma
            if j >= self.R and (j - self.R) not in self.rel:
                break
            src, kc, ncols = self.units[j]
            slot = j % self.R
            dst = self.ring[:, slot, 0:kc * ncols].rearrange("p (k n) -> p k n", k=kc)
            self.S.dma('pool', dst, src, (), [('w', slot)], ('w', slot))
            self.next_dma += 1

    def acquire(self):
        j = self.next_acq
        assert j < self.next_dma, "weight ring deadlock: unit %d not yet issued" % j
        self.next_acq += 1
        src, kc, ncols = self.units[j]
        slot = j % self.R
        view = self.ring[:, slot, 0:kc * ncols].rearrange("p (k n) -> p k n", k=kc)
        return view, ('w', slot), j

    def release(self, j):
        self.rel.add(j)
        self.pump()


def build_program(dump=None):
    nc = bass.Bass("TRN2", target_bir_lowering=False)
    dump = dump or []

    def din(name, shape):
        return nc.dram_tensor(name, list(shape), F32, kind="ExternalInput").ap()

    def dout(name, shape):
        return nc.dram_tensor(name, list(shape), F32, kind="ExternalOutput").ap()

    x = din("x", (2048, 1024))
    cond = din("cond", (2, 1024))
    ck = din("ck", (512, 1024))
    cv = din("cv", (512, 1024))
    w_ada = din("w_ada", (1024, 6144))
    b_ada = din("b_ada", (1, 6144))
    norm1_g = din("norm1_g", (1, 1024))
    w_in = din("w_in", (1024, 5632))
    b_gate = din("b_gate", (1, 2048))
    q_norm_g = din("q_norm_g", (1, 64))
    k_norm_g = din("k_norm_g", (1, 64))
    lam_in = din("lam4", (1, 256))
    subln_g = din("subln_g", (1, 128))
    pool_w = din("pool_w", (4, 128, 128))
    pool_scale = din("pool_scale", (1, 512))
    w_br_attn = din("w_br_attn", (1024, 1024))
    w_br_pool = din("w_br_pool", (512, 1024))
    w_out = din("w_out", (1024, 1024))
    norm2_g = din("norm2_g", (1, 1024))
    router_w = din("router_w", (1024, 20))
    router_b = din("router_b", (1, 20))
    ew_gate = din("ew_gate", (16, 1024, 512))
    ew_up = din("ew_up", (16, 1024, 512))
    ew_down = din("ew_down", (16, 512, 1024))
    ident_d = din("ident", (128, 128))
    cos_d = din("cos_t", (1024, 64))
    sin_d = din("sin_t", (1024, 64))
    rcb_d = din("rcb", (1, 64))

    tri_d = din("tri", (128, 128))
    thr_d = din("thr", (1, 16))
    tval_d = din("tval", (1, 48))
    rtab_d = din("rtab", (1, 64))
    NPAIR = 31
    NTS = 2 * NPAIR
    modrows = nc.dram_tensor("modrows", [2, 2, 1024], F32, kind="Internal").ap()
    g2row = nc.dram_tensor("g2row", [2, 1024], F32, kind="Internal").ap()
    gps = nc.dram_tensor("gps", [2, 1024], F32, kind="Internal").ap()
    h2d = nc.dram_tensor("h2d", [2048, 1024], BF16, kind="Internal").ap()
    x1d = nc.dram_tensor("x1d", [2048, 1024], F32, kind="Internal").ap()
    slot_tok = nc.dram_tensor("slot_tok", [64 * 128, 16], I32, kind="Internal").ap()
    yslot = nc.dram_tensor("yslot", [NTS * 128, 1024], F32, kind="Internal").ap()
    y = dout("y", (2048, 1024))
    nk = dout("nk", (1024, 1024))
    nv = dout("nv", (1024, 1024))
    dumps = {}
    for nm, shp in dump:
        dumps[nm] = dout("dbg_" + nm, shp)

    A = nc.alloc_sbuf_tensor
    ring = A("ring", [128, 10, 2048], BF16)
    hT = A("hT", [128, 8, 1024], BF16)
    X1 = A("X1", [128, 8192], F32)
    x1 = X1[:].rearrange("p (t d) -> p t d", t=8)
    vtok = X1[:].bitcast(BF16)[:, 0:12 * 8 * 132].rearrange("p (j h e) -> p j h e", j=12, h=8)
    ACC = A("ACC", [128, 8192], F32)
    acc = ACC[:].rearrange("p (t d) -> p t d", t=8)
    accb = ACC[:].bitcast(BF16)
    qT = accb[:, 0:8192].rearrange("p (h t) -> p h t", h=8)
    mrgT = accb[:, 8192:16384].rearrange("p (h t) -> p h t", h=8)
    mrg_f = ACC[:, 4096:8192]
    KR = A("KR", [128, 8 * 1536], BF16)
    kT = KR[:].rearrange("p (h t) -> p h t", h=8)
    silu_b = [KR[:, i * 512:(i + 1) * 512] for i in range(2)]
    hid = [KR[:, 1024 + i * 4096: 1024 + (i + 1) * 4096].rearrange("p (b f t) -> p b f t", b=2, f=4) for i in range(2)]
    mixT = A("mixT", [128, 4, 1024], BF16)
    PL = A("PL", [128, 4096], F32)
    PLb = PL[:].bitcast(BF16)
    xst = [A("xst%d" % i, [128, 1024], F32) for i in range(2)]
    xn = [A("xn%d" % i, [128, 1024], BF16) for i in range(2)]
    ident = A("identb", [128, 128], BF16)
    cosb = A("cosb", [128, 8, 64], F32)
    sinb = A("sinb", [128, 8, 64], F32)
    gq_b = A("gq_b", [128, 64], F32)
    gk_b = A("gk_b", [128, 64], F32)
    gsub_b = A("gsub_b", [128, 128], F32)
    rcb = A("rcbs", [128, 64], F32)
    lam_b = A("lam_b", [128, 256], F32)
    lam_t = A("lam_t", [128, 128], F32)
    lam_s = A("lam_s", [128, 8], F32)
    sc32 = A("sc32", [128, 8, 2], F32)
    scT2 = A("scT2", [128, 8, 2], BF16)
    screp = A("screp", [128, 8, 128], BF16)
    screp1 = A("screp1", [128, 8, 128], BF16)
    modT2 = A("modT2", [128, 48, 2], F32)
    bT = A("bT", [128, 48], F32)
    n1gT = A("n1gT", [128, 8], F32)
    n2gT = A("n2gT", [128, 8], F32)
    a1T2 = A("a1T2", [128, 8, 2], F32)
    a2T2 = A("a2T2", [128, 8, 2], F32)
    g1b = A("g1b", [128, 1024], F32)
    g2b = A("g2b", [128, 1024], F32)
    bgT = A("bgT", [128, 16], F32)
    pscT = A("pscT", [128, 4], F32)
    rb_b = A("rb_b", [128, 20], F32)
    poolw = A("poolw", [128, 4, 128], BF16)
    rw = A("rw", [128, 8, 20], BF16)
    ss = A("ss", [128, 8], F32)
    rstd = A("rstd", [128, 8], F32)
    ss8 = A("ss8", [128, 2, 8], F32)
    rs8 = A("rs8", [128, 2, 8, 1], F32)
    ss8c = A("ss8c", [128, 8], F32)
    rs8c = A("rs8c", [128, 8, 1], F32)
    rz = A("rz", [128, 4, 2, 1], F32)
    r2n = A("r2n", [128, 4, 1], F32)
    O1g = A("O1g", [128, 16, 16], F32)
    O2g = A("O2g", [128, 16, 16], F32)
    W1g = A("W1g", [128, 16, 1], F32)
    W2g = A("W2g", [128, 16, 1], F32)
    Mb = A("Mb", [128, 16, 16], BF16)
    s12i = A("s12i", [128, 2, 16], I32)
    widx = A("widx", [128, 48, 2], I32)
    gidx = A("gidx", [128, 2], I32)
    trib = A("trib", [128, 128], BF16)
    onesb = A("onesb", [128, 128], BF16)
    thr = A("thr_sb", [128, 16], F32)
    tval = A("tval_sb", [128, 48], F32)
    tok16 = A("tok16", [128, 16, 16], I32)
    p2col = A("p2col", [128, 1], F32)
    rtab = A("rtab_sb", [128, 64], F32)
    fscr = A("fscr", [128, 1], F32)

    PSALL = nc.alloc_psum_tensor("psall", [128, 4096], F32)
    PSALLb = PSALL[:].bitcast(BF16)
    PS = [PSALL[:, i * 512:(i + 1) * 512] for i in range(8)]
    PSb = [PSALLb[:, i * 1024:(i + 1) * 1024] for i in range(8)]

    S = Sched(nc, fscr[:])

    def psk(i):
        return ('ps', i)

    def cols(w, c0, n, kc):
        return (w[:, c0:c0 + n].rearrange("(k p) n -> p k n", p=128), kc, n)

    pass_units = []
    for u in range(24):
        pass_units.append(cols(w_ada, 256 * u, 256, 8))
    for u in range(14):
        pass_units.append(cols(w_in, 256 * u, 256, 8))
    for ng in range(4):
        pass_units.append(cols(w_in, 3584 + 256 * ng, 256, 8))
        pass_units.append(cols(w_in, 4608 + 256 * ng, 256, 8))
        pass_units.append(cols(w_br_attn, 256 * ng, 256, 8))
        pass_units.append(cols(w_br_pool, 256 * ng, 256, 4))
    for j in range(4):
        pass_units.append(cols(w_out, 256 * j, 256, 8))
    W = WRing(S, ring, pass_units + pass_units[24:])

    def cload(dst, src, key, q='sp', slow=False):
        S.dma(q, dst, src, (), [key], key, slow=slow)

    for j in range(2):
        S.dma('sp', sc32[:, :, j], cond[j, :].rearrange("(k p) -> p k", p=128), (), [('sc32', j)], ('sc32', j), slow=True)

    cload(ident[:], ident_d[:, :], 'ident', q='pool')
    cload(cosb[:], cos_d.rearrange("(t p) d -> p t d", p=128), 'cosb')
    cload(sinb[:], sin_d.rearrange("(t p) d -> p t d", p=128), 'sinb')
    cload(gq_b[:], q_norm_g[0:1, :].to_broadcast([128, 64]), 'gq_b')
    cload(gk_b[:], k_norm_g[0:1, :].to_broadcast([128, 64]), 'gk_b')
    cload(gsub_b[:], subln_g[0:1, :].to_broadcast([128, 128]), 'gsub_b')
    cload(rcb[:], rcb_d[0:1, :].to_broadcast([128, 64]), 'rcb')
    cload(lam_b[:], lam_in[0:1, :].to_broadcast([128, 256]), 'lam_b')
    cload(bT[:], b_ada[0, :].rearrange("(c p) -> p c", p=128), 'bT', slow=True)
    cload(n1gT[:], norm1_g[0, :].rearrange("(c p) -> p c", p=128), 'n1gT', slow=True)
    cload(n2gT[:], norm2_g[0, :].rearrange("(c p) -> p c", p=128), 'n2gT', slow=True)
    cload(bgT[:], b_gate[0, :].rearrange("(c p) -> p c", p=128), 'bgT', slow=True)
    cload(pscT[:], pool_scale[0, :].rearrange("(c p) -> p c", p=128), 'pscT', slow=True)
    cload(rb_b[:], router_b[0:1, :].to_broadcast([128, 20]), 'rb_b')
    cload(poolw[:], pool_w.rearrange("g c e -> c g e"), 'poolw', q='pool')
    cload(rw[:], router_w.rearrange("(k p) n -> p k n", p=128), 'rw', q='pool')

    cload(trib[:], tri_d[:, :], 'trib', q='pool')
    cload(thr[:], thr_d[0:1, :].to_broadcast([128, 16]), 'thr')
    cload(tval[:], tval_d[0:1, :].to_broadcast([128, 48]), 'tval')
    cload(rtab[:], rtab_d[0:1, :].to_broadcast([128, 64]), 'rtab')
    S.memset('dve', onesb[:], 1.0, ['onesb'])
    S.add('pool', lambda e: e.iota(tok16[:], [[128, 16], [0, 16]], base=0, channel_multiplier=1), (), ['tok16'])
    S.add('pool', lambda e: e.iota(gidx[:, 0:1], [[0, 1]], base=0, channel_multiplier=2), (), ['p2i'])
    S.cp('dve', p2col[:], gidx[:, 0:1], ['p2i'], ['p2col'])
    S.ts('dve', gq_b[:], gq_b[:], 8.0 * 0.125, None, ALU.mult, None, ['gq_b'], ['gq_b'])
    S.ts('dve', gk_b[:], gk_b[:], 8.0, None, ALU.mult, None, ['gk_b'], ['gk_b'])
    S.ts('dve', gsub_b[:], gsub_b[:], (1.0 - LAMBDA_INIT) * (128.0 ** 0.5), None, ALU.mult, None, ['gsub_b'], ['gsub_b'])
    lb = lam_b[:].rearrange("p (a b d) -> p a b d", a=2, b=2)
    lt = lam_t[:].rearrange("p (a d) -> p a d", a=2)
    S.tt('dve', lt, lb[:, :, 0, :], lb[:, :, 1, :], ALU.mult, ['lam_b'], ['lam_t'])
    S.red(lam_s[:, 0:2], lt, ALU.add, ['lam_t'], ['lam_s'])
    S.act(lam_s[:, 2:4], lam_s[:, 0:2], AF.Exp, ['lam_s'], ['lam_s2'])
    S.tt('dve', lam_s[:, 4:5], lam_s[:, 2:3], lam_s[:, 3:4], ALU.subtract, ['lam_s2'], ['lam_s3'])
    S.ts('dve', lam_s[:, 5:6], lam_s[:, 4:5], LAMBDA_INIT, -1.0, ALU.add, ALU.mult, ['lam_s3'], ['neg_lam'])
    neg_lam = lam_s[:, 5:6]

    def dbg(name, src_ap, reads):
        if name in dumps:
            S.dma('pool', dumps[name], src_ap, reads, [], 'dbg_' + name)

    def norm_T(p, src, aT, shT, hkey):
        for blk in range(2):
            banks = [4 * blk + i for i in range(4)]
            for t in range(4):
                tile = blk * 4 + t
                sl = tile % 2
                if src == 'x':
                    xs = xst[sl][:]
                    xkey = ('xst', sl)
                    S.dma('sp', xs, x[p * 1024 + tile * 128: p * 1024 + (tile + 1) * 128, :], (), [xkey], xkey)
                else:
                    xs = x1[:, tile, :]
                    xkey = ('x1', tile)
                S.act(xn[sl][:], xs, AF.Square, [xkey], [('xn', sl), ('ss', tile)], accum_out=ss[:, tile:tile + 1])
                S.act(rstd[:, tile:tile + 1], ss[:, tile:tile + 1], AF.Ln, [('ss', tile)], [('rstd', tile)],
                      bias=EPS, scale=1.0 / 1024.0)
                S.act(rstd[:, tile:tile + 1], rstd[:, tile:tile + 1], AF.Exp, [('rstd', tile)], [('rstd', tile)], scale=-0.5)
                S.act(xn[sl][:], xs, AF.Copy, [xkey, ('rstd', tile)], [('xn', sl)], scale=rstd[:, tile:tile + 1])
                if src == 'x1':
                    tmp = PL[:, sl * 1024:(sl + 1) * 1024]
                    hb = PL[:, 2048 + sl * 512: 2560 + sl * 512].bitcast(BF16)
                    S.tt('pool', tmp, xs, g1b[:], ALU.mult, [xkey, 'g1b'], [('pl', 'h2t', sl)])
                    S.stt(hb, tmp, rstd[:, tile:tile + 1], xst[0][:], ALU.mult, ALU.add,
                          [('pl', 'h2t', sl), ('rstd', tile), ('xst', 0)], [('pl', 'h2b', sl)])
                    S.dma('sp', h2d[p * 1024 + tile * 128: p * 1024 + (tile + 1) * 128, :], hb, [('pl', 'h2b', sl)],
                          [('h2d', p, tile)], ('h2o', sl))
                    S.dma('sp', x1d[p * 1024 + tile * 128: p * 1024 + (tile + 1) * 128, :], xs, [xkey],
                          [('x1d', p, tile)], 'x1o')
                for c in range(8):
                    bv = PSb[banks[c // 2]].rearrange("p (c t) -> p c t", c=2)
                    S.tr(bv[:, c % 2, t * 128:(t + 1) * 128], xn[sl][:, c * 128:(c + 1) * 128], ident[:],
                         [('xn', sl), 'ident'], [psk(banks[c // 2])])
            for c in range(8):
                bv = PSb[banks[c // 2]].rearrange("p (c t) -> p c t", c=2)
                dst = hT[:, c, blk * 512:(blk + 1) * 512]
                if c % 2 == 0:
                    S.act(dst, bv[:, c % 2, :], AF.Identity, [psk(banks[c // 2]), aT[1], shT[1]], [(hkey, blk)],
                          bias=shT[0][:, c:c + 1], scale=aT[0][:, c:c + 1])
                else:
                    S.ts('dve', dst, bv[:, c % 2, :], aT[0][:, c:c + 1], shT[0][:, c:c + 1], ALU.mult, ALU.add,
                         [psk(banks[c // 2]), aT[1], shT[1]], [(hkey, blk)])

    def run_pass(p):
        nseq, L = (4, 256) if p == 0 else (1, 1024)
        nkt = 8 if p == 0 else 12
        S.fence()
        if p == 0:
            S.act(scT2[:], sc32[:], AF.Silu, [('sc32', 0), ('sc32', 1)], ['scT'])
            S.cp('dve', screp[:], scT2[:, :, 0:1].to_broadcast([128, 8, 128]), ['scT'], ['screp'])
            S.cp('dve', screp1[:], scT2[:, :, 1:2].to_broadcast([128, 8, 128]), ['scT'], ['screp1'])
            S.dma('sp', g1b[:], b_ada[0:1, 2048:3072].to_broadcast([128, 1024]), (), ['g1b'], 'g1b')
            S.dma('sp', g2b[:], b_ada[0:1, 5120:6144].to_broadcast([128, 1024]), (), ['g2b'], 'g2b')
            mod2 = PS[0][:, 0:96].rearrange("p (c j) -> p c j", j=2)
            for u in range(24):
                wv, wk, wj = W.acquire()
                if u in (8, 9, 10, 11, 20, 21, 22, 23):
                    gb, gkey, base = (g1b, 'g1b', 8) if u < 12 else (g2b, 'g2b', 20)
                    gi_ = 0 if u < 12 else 1
                    bank = 1 + (u % 2)
                    for kc in range(8):
                        S.mm(PS[bank][:, 0:256], screp[:, kc, :], wv[:, kc, :], kc == 0, kc == 7,
                             ['screp', wk], [psk(bank)])
                    c0 = (u - base) * 256
                    S.tt('dve', gb[:, c0:c0 + 256], PS[bank][:, 0:256], gb[:, c0:c0 + 256], ALU.add,
                         [psk(bank), gkey], [gkey])
                    bank2 = 3 + (u % 2)
                    for kc in range(8):
                        S.mm(PS[bank2][:, 0:256], screp1[:, kc, :], wv[:, kc, :], kc == 0, kc == 7,
                             ['screp1', wk], [psk(bank2)])
                    gst = PL[:, (u % 4) * 256:(u % 4 + 1) * 256]
                    S.cp('act', gst, PS[bank2][:, 0:256], [psk(bank2)], [('pl', 'gst', u % 4)])
                    S.dma('sp', gps[gi_:gi_ + 1, c0:c0 + 256], gst[0:1, :], [('pl', 'gst', u % 4)], [('gps', gi_, u)], ('gpso', u % 4))
                else:
                    for cc in range(2):
                        ch = 2 * u + cc
                        for kc in range(8):
                            S.mm(mod2[:, ch, :], wv[:, kc, cc * 128:(cc + 1) * 128], scT2[:, kc, :], kc == 0, kc == 7,
                                 ['scT', wk], [psk(0)])
                W.release(wj)
            bT3 = bT[:].rearrange("p (c o) -> p c o", o=1)
            S.tt('dve', modT2[:, 0:16, :], mod2[:, 0:16, :], bT3[:, 0:16, :].to_broadcast([128, 16, 2]), ALU.add, [psk(0), 'bT'], ['modT'])
            S.tt('dve', modT2[:, 24:40, :], mod2[:, 24:40, :], bT3[:, 24:40, :].to_broadcast([128, 16, 2]), ALU.add, [psk(0), 'bT'], ['modT'])
            for j in range(2):
                S.stt(a1T2[:, :, j], modT2[:, 8:16, j], 1.0, n1gT[:], ALU.add, ALU.mult, ['modT', 'n1gT'], ['a1T'])
                S.stt(a2T2[:, :, j], modT2[:, 32:40, j], 1.0, n2gT[:], ALU.add, ALU.mult, ['modT', 'n2gT'], ['a2T'])
        else:
            gkeys1 = [('gps', 0, u) for u in (8, 9, 10, 11)]
            gkeys2 = [('gps', 1, u) for u in (20, 21, 22, 23)]
            S.dma('sp', g1b[:], b_ada[0:1, 2048:3072].to_broadcast([128, 1024]), (), ['g1b'], 'g1b')
            S.dma('sp', g2b[:], b_ada[0:1, 5120:6144].to_broadcast([128, 1024]), (), ['g2b'], 'g2b')
            S.dma('sp', xst[0][:], gps[0:1, :].to_broadcast([128, 1024]), gkeys1, [('xst', 0)], ('xst', 0))
            S.dma('sp', xst[1][:], gps[1:2, :].to_broadcast([128, 1024]), gkeys2, [('xst', 1)], ('xst', 1))
            S.tt('dve', g1b[:], g1b[:], xst[0][:], ALU.add, ['g1b', ('xst', 0)], ['g1b'])
            S.tt('dve', g2b[:], g2b[:], xst[1][:], ALU.add, ['g2b', ('xst', 1)], ['g2b'])
        a1T = a1T2[:, :, p]
        a2T = a2T2[:, :, p]
        sh1 = (modT2[:, 0:8, p], 'modT')
        sh2 = (modT2[:, 24:32, p], 'modT')

        norm_T(p, 'x', (a1T, 'a1T'), sh1, 'hT')
        if p == 0:
            for j in range(2):
                S.dma('sp', modrows[j, 0, :].rearrange("(c p) -> p c", p=128), a2T2[:, :, j], ['a2T'], [('modrows', j)], 'mro', slow=True)
                S.dma('sp', modrows[j, 1, :].rearrange("(c p) -> p c", p=128), modT2[:, 24:32, j], ['modT'], [('modrows', j)], 'mro', slow=True)
        S.dma('sp', g2row[p:p + 1, :], g2b[0:1, :], ['g2b'], [('g2row', p)], 'g2o')
        if p == 0:
            dbg('hT', hT[:], [('hT', 0), ('hT', 1)])

        sqs = [mrg_f[:, 0:512], mrg_f[:, 3584:4096]]
        zn = mrg_f[:, 512:1024]
        zg = [mrg_f[:, 1024 + i * 512: 1536 + i * 512] for i in range(2)]
        vst = [mrg_f[:, 2048 + i * 512: 2560 + i * 512] for i in range(2)]
        qkb = [mrg_f[:, 3072 + i * 256: 3328 + i * 256].bitcast(BF16) for i in range(2)]
        zns = [zn, vst[0]]
        if p == 1:
            rtab = {}
            for ti, (gb_, gkey) in enumerate(((gq_b, 'gq_b'), (gk_b, 'gk_b'))):
                Cg = PL[:, ti * 1024: ti * 1024 + 512].rearrange("p (t d) -> p t d", t=8)
                Sg = PL[:, ti * 1024 + 512: ti * 1024 + 1024].rearrange("p (t d) -> p t d", t=8)
                S.tt('pool', Cg, cosb[:], gb_[:].rearrange("p (o d) -> p o d", o=1).to_broadcast([128, 8, 64]), ALU.mult,
                     ['cosb', gkey], [('pl', 'Cg', ti)])
                g4 = gb_[:].rearrange("p (a s d) -> p a s d", a=2, s=2)
                for sidx in range(2):
                    for a_ in range(2):
                        S.tt('pool', Sg[:, :, a_ * 32 + sidx * 16: a_ * 32 + sidx * 16 + 16],
                             sinb[:, :, a_ * 32 + sidx * 16: a_ * 32 + sidx * 16 + 16],
                             g4[:, a_, 1 - sidx, :].rearrange("p (o d) -> p o d", o=1).to_broadcast([128, 8, 16]), ALU.mult,
                             ['sinb', gkey], [('pl', 'Sg', ti)])
                rtab[ti] = (Cg, Sg)
        S.memset('dve', vtok[:, :, :, 128:129], 1.0, [('vt', 'ones')])
        if p == 1:
            for jt in range(4):
                S.dma('pool', vtok[:, 8 + jt, :, 0:128],
                      cv[jt * 128:(jt + 1) * 128, :].rearrange("p (h e) -> p h e", h=8), (), [('vt', 8 + jt)], ('vtc', jt))
                sl = jt % 2
                S.dma('pool', xn[sl][:], ck[jt * 128:(jt + 1) * 128, :], (), [('xn', sl)], ('xnc', sl))
                bv = PSb[7].rearrange("p (h t) -> p h t", h=8)
                for h in range(8):
                    S.tr(bv[:, h, :], xn[sl][:, h * 128:(h + 1) * 128], ident[:], [('xn', sl), 'ident'], [psk(7)])
                S.cp('act', kT[:, :, 1024 + jt * 128: 1024 + (jt + 1) * 128], bv, [psk(7)], [('kT', 8 + jt)])
        qk_units = {}

        def qk_M(n):
            ci, tile = divmod(n, 8)
            if tile == 0:
                qk_units[ci] = (W.acquire(), W.acquire())
            (ua, uak, uaj), (ub, ubk, ubj) = qk_units[ci]
            bank = n % 4
            for half, (u_, uk_) in enumerate(((ua, uak), (ub, ubk))):
                for kc in range(8):
                    S.mm(PS[bank][:, half * 256:(half + 1) * 256], hT[:, kc, tile * 128:(tile + 1) * 128],
                         u_[:, kc, :], kc == 0, kc == 7, [('hT', tile // 4), uk_], [psk(bank)])
            if tile == 7:
                W.release(uaj)
                W.release(ubj)

        def qk_E1(n):
            bank = n % 4
            b2 = n % 2
            zps = PS[bank][:, 0:512]
            sqb = sqs[b2]
            S.act(sqb, zps, AF.Square, [psk(bank)], [('st2', 'sq', b2)])
            S.red(ss8[:, b2, :], sqb.rearrange("p (g d) -> p g d", g=8), ALU.add, [('st2', 'sq', b2)], [('ss8', b2)])
            S.act(rs8[:, b2, :, 0], ss8[:, b2, :], AF.Ln, [('ss8', b2)], [('rs8', b2)], bias=64.0 * EPS)
            S.act(rs8[:, b2, :, 0], rs8[:, b2, :, 0], AF.Exp, [('rs8', b2)], [('rs8', b2)], scale=-0.5)

        def qk_E2(n):
            ci, tile = divmod(n, 8)
            isq = ci < 2
            hc = ci % 2
            gb_, gkey = (gq_b, 'gq_b') if isq else (gk_b, 'gk_b')
            bank = n % 4
            s2 = n % 2
            b2 = n % 2
            zps = PS[bank][:, 0:512]
            sqb = sqs[b2]
            znb = zns[b2] if p == 1 else zn
            znk = ('st2', 'zn', b2) if p == 1 else ('st2', 'zn')
            S.tt('dve', znb.rearrange("p (g d) -> p g d", g=8), zps.rearrange("p (g d) -> p g d", g=8),
                 rs8[:, b2, :, :].to_broadcast([128, 8, 64]), ALU.mult, [psk(bank), ('rs8', b2)], [znk])
            gbb = gb_[:].rearrange("p (o d) -> p o d", o=1).to_broadcast([128, 8, 64])
            if p == 0:
                if isq:
                    S.tt('pool', qkb[s2].rearrange("p (g d) -> p g d", g=8), zn.rearrange("p (g d) -> p g d", g=8),
                         gbb, ALU.mult, [znk, gkey], [('st2', 'qkb', s2)])
                else:
                    S.tt('pool', zg[s2].rearrange("p (g d) -> p g d", g=8), zn.rearrange("p (g d) -> p g d", g=8),
                         gbb, ALU.mult, [znk, gkey], [('st2', 'zg', s2)])
                    S.dma('sp', nk[tile * 128:(tile + 1) * 128, hc * 512:(hc + 1) * 512], zg[s2],
                          [('st2', 'zg', s2)], [], ('nk', s2))
                    S.cp('act', qkb[s2], zg[s2], [('st2', 'zg', s2)], [('st2', 'qkb', s2)])
            else:
                ti = 0 if isq else 1
                Cg, Sg = rtab[ti]
                t1 = zg[s2]
                S.tt('dve', t1.rearrange("p (g d) -> p g d", g=8), znb.rearrange("p (g d) -> p g d", g=8),
                     Cg[:, tile:tile + 1, :].to_broadcast([128, 8, 64]), ALU.mult, [znk, ('pl', 'Cg', ti)], [('st2', 'zg', s2)])
                zz = znb.rearrange("p (g a s d) -> p g a s d", g=8, a=2, s=2)
                t2 = vst[1]
                qq = t2.rearrange("p (g a s d) -> p g a s d", g=8, a=2, s=2)
                sg4 = Sg[:, tile:tile + 1, :].rearrange("p o (a s d) -> p o a s d", a=2, s=2)
                for sidx in range(2):
                    sn = sg4[:, :, :, sidx, :].to_broadcast([128, 8, 2, 16])
                    S.tt('pool', qq[:, :, :, sidx, :], zz[:, :, :, 1 - sidx, :], sn, ALU.mult,
                         [znk, ('pl', 'Sg', ti)], [('st2', 't2', sidx)])
                S.tt('dve', qkb[s2], t1, t2, ALU.add, [('st2', 'zg', s2), ('st2', 't2', 0), ('st2', 't2', 1)], [('st2', 'qkb', s2)])

        def qk_T(n):
            ci, tile = divmod(n, 8)
            isq = ci < 2
            hc = ci % 2
            s2 = n % 2
            bk = 6 + (n % 2)
            bv = PSb[bk].rearrange("p (h t) -> p h t", h=8)
            for hh in range(4):
                S.tr(bv[:, hh, :], qkb[s2][:, hh * 128:(hh + 1) * 128], ident[:], [('st2', 'qkb', s2), 'ident'], [psk(bk)])
            if isq:
                S.cp('act', qT[:, 4 * hc:4 * hc + 4, tile * 128:(tile + 1) * 128], bv[:, 0:4, :], [psk(bk)], [('qT', tile // 2)])
            else:
                S.cp('act', kT[:, 4 * hc:4 * hc + 4, tile * 128:(tile + 1) * 128], bv[:, 0:4, :], [psk(bk)], [('kT', tile)])

        NQK = 32
        for s_ in range(NQK + 3):
            if s_ < NQK:
                qk_M(s_)
            if 0 <= s_ - 1 < NQK:
                qk_E1(s_ - 1)
            if 0 <= s_ - 2 < NQK:
                qk_E2(s_ - 2)
            if 0 <= s_ - 3 < NQK:
                qk_T(s_ - 3)
        for ci in range(2):
            ua, uak, uaj = W.acquire()
            ub, ubk, ubj = W.acquire()
            for tile in range(8):
                bank = tile % 4
                for half, (u_, uk_) in enumerate(((ua, uak), (ub, ubk))):
                    for kc in range(8):
                        S.mm(PS[bank][:, half * 256:(half + 1) * 256], hT[:, kc, tile * 128:(tile + 1) * 128],
                             u_[:, kc, :], kc == 0, kc == 7, [('hT', tile // 4), uk_], [psk(bank)])
                zps = PS[bank][:, 0:512]
                S.cp('act', vtok[:, tile, 4 * ci:4 * ci + 4, 0:128], zps.rearrange("p (h e) -> p h e", h=4),
                     [psk(bank)], [('vt', tile)])
                if p == 0:
                    s2 = tile % 2
                    S.cp('dve', vst[s2], zps, [psk(bank)], [('st2', 'vst', s2)])
                    S.dma('sp', nv[tile * 128:(tile + 1) * 128, ci * 512:(ci + 1) * 512], vst[s2],
                          [('st2', 'vst', s2)], [], ('nv', s2))
            W.release(uaj)
            W.release(ubj)
        S.fence(('PL',))
        Wd = L + 16
        Pb = PL[:, 0:1088]
        Ab = PL[:, 1088:2176]
        Bb = PL[:, 2176:3264]
        pooled = PL[:, 3264:3776].bitcast(BF16)
        tmpb = PL[:, 3776:3776 + 32]
        S.memset('dve', Pb, 0.0, [('pl', 'P')])

        def v3(buf):
            return buf[:, 0:nseq * Wd].rearrange("p (s l) -> p s l", s=nseq)

        def rg(buf, a, b):
            return v3(buf)[:, :, 8 + a: 8 + b]

        for half in range(2):
            up, upk, upj = W.acquire()
            for gg in range(2):
                g = 2 * half + gg
                w_ = (2, 4, 8, 16)[g]
                hw = w_ // 2
                for blk in range(2):
                    bank = 4 + blk
                    for kc in range(8):
                        S.mm(PS[bank][:, 0:512], up[:, kc, gg * 128:(gg + 1) * 128], hT[:, kc, blk * 512:(blk + 1) * 512],
                             kc == 0, kc == 7, [('hT', blk), upk], [psk(bank)])
                    if p == 0:
                        S.cp('act', v3(Pb)[:, 2 * blk:2 * blk + 2, 8:8 + 256], PS[bank][:, 0:512].rearrange("p (s l) -> p s l", s=2),
                             [psk(bank)], [('pl', 'P')])
                    else:
                        S.cp('act', Pb[:, 8 + 512 * blk: 8 + 512 * (blk + 1)], PS[bank][:, 0:512], [psk(bank)], [('pl', 'P')])
                pk, ak, bk_ = ('pl', 'P'), ('pl', 'A'), ('pl', 'B')
                S.tt('dve', rg(Ab, -7, L + 8), rg(Pb, -8, L + 7), rg(Pb, -7, L + 8), ALU.add, [pk], [ak])
                src_, sk = Ab, ak
                if g >= 1:
                    S.tt('dve', rg(Bb, -6, L + 7), rg(Ab, -7, L + 6), rg(Ab, -5, L + 8), ALU.add, [ak], [bk_])
                    src_, sk = Bb, bk_
                if g >= 2:
                    S.tt('dve', rg(Ab, -4, L + 5), rg(Bb, -6, L + 3), rg(Bb, -2, L + 7), ALU.add, [bk_], [ak])
                    src_, sk = Ab, ak
                if g >= 3:
                    S.tt('dve', rg(Bb, 0, L), rg(Ab, -4, L - 4), rg(Ab, 4, L + 4), ALU.add, [ak], [bk_])
                    src_, sk = Bb, bk_
                pl3 = pooled.rearrange("p (s l) -> p s l", s=nseq)
                S.stt(pl3, rg(src_, 0, L), 1.0 / w_, rg(Pb, 0, L), ALU.mult, ALU.subtract, [sk, pk], [('pl', 'pooled')])
                tb = tmpb[:, 0:nseq * hw].rearrange("p (s l) -> p s l", s=nseq)
                for side in range(2):
                    lo, hi = (0, hw) if side == 0 else (L - hw, L)
                    rcv = rcb[:, g * 16 + side * 8: g * 16 + side * 8 + hw].rearrange("p (o l) -> p o l", o=1).to_broadcast([128, nseq, hw])
                    S.tt('dve', tb, rg(src_, lo, hi), rcv, ALU.mult, [sk, 'rcb'], [('pl', 'tmpb')])
                    S.tt('dve', pl3[:, :, lo:hi], tb, rg(Pb, lo, hi), ALU.subtract, [('pl', 'tmpb'), pk], [('pl', 'pooled')])
                for blk in range(2):
                    bank = 6 + blk
                    S.mm(PS[bank][:, 0:512], poolw[:, g, :], pooled[:, blk * 512:(blk + 1) * 512], True, True,
                         [('pl', 'pooled'), 'poolw'], [psk(bank)])
                    S.act(mixT[:, g, blk * 512:(blk + 1) * 512], PS[bank][:, 0:512], AF.Copy, [psk(bank), 'pscT'],
                          [('mixT', blk)], scale=pscT[:, g:g + 1])
            W.release(upj)
        if p == 0:
            dbg('qT', qT, [('qT', i) for i in range(4)])
            dbg('kT', kT, [('kT', i) for i in range(8)])
            dbg('mixT', mixT[:], [('mixT', 0), ('mixT', 1)])

        S.fence(('ACC', 'PL'))
        PT = [PLb[:, i * 512:(i + 1) * 512] for i in range(3)]
        sqo = PL[:, 768:1792]
        tO2 = [PL[:, 1792 + i * 128: 1920 + i * 128] for i in range(4)]
        onb = [PL[:, 2304 + i * 512: 2816 + i * 512].bitcast(BF16) for i in range(2)]
        ost = [[mrg_f[:, (a * 2 + b) * 1024:(a * 2 + b + 1) * 1024].rearrange("p (h e) -> p h e", h=8) for b in range(2)]
               for a in range(2)]
        items = []
        for qb in range(4):
            keytiles = [2 * qb, 2 * qb + 1] if p == 0 else list(range(12))
            for h in range(8):
                for jn, j in enumerate(keytiles):
                    items.append((qb, h, jn, j, len(keytiles)))
        pending_T = []
        o2c = [0]

        def at_A(n):
            qb, h, jn, j, nk_ = items[n]
            sbk = n % 3
            sp_ = n % 2
            for i in range(2):
                S.mm(PS[2 * sp_ + i][:, 0:256], kT[i * 64:(i + 1) * 64, h, j * 128:(j + 1) * 128],
                     qT[i * 64:(i + 1) * 64, h, qb * 256:(qb + 1) * 256], True, True,
                     [('kT', j), ('qT', qb)], [psk(2 * sp_ + i)])
            S.act(PT[sbk].rearrange("p (i q) -> p i q", i=2),
                  PSALL[:, 2 * sp_ * 512:(2 * sp_ + 2) * 512].rearrange("p (i q) -> p i q", i=2)[:, :, 0:256],
                  AF.Exp, [psk(2 * sp_), psk(2 * sp_ + 1)], [('pl', 'PT', sbk)])

        def subln(qb):
            for qt in range(2):
                o = ost[qb % 2][qt]
                ok_ = ('ost', qb % 2, qt)
                tile = qb * 2 + qt
                S.tt('dve', sqo.rearrange("p (h e) -> p h e", h=8), o, o, ALU.mult, [ok_], [('pl', 'sqo')])
                S.red(ss8c[:], sqo.rearrange("p (h e) -> p h e", h=8), ALU.add, [('pl', 'sqo')], ['ss8b'])
                S.act(rs8c[:, :, 0], ss8c[:], AF.Ln, ['ss8b'], ['rs8b'], bias=128.0 * EPS)
                S.act(rs8c[:, :, 0], rs8c[:, :, 0], AF.Exp, ['rs8b'], ['rs8b'], scale=-0.5)
                S.tt('pool', o, o, rs8c[:].to_broadcast([128, 8, 128]), ALU.mult, [ok_, 'rs8b'], [ok_])
                ob = onb[qt].rearrange("p (h e) -> p h e", h=8)
                S.tt('pool', ob, o, gsub_b[:].rearrange("p (o e) -> p o e", o=1).to_broadcast([128, 8, 128]), ALU.mult,
                     [ok_, 'gsub_b'], [('pl', 'onb', qt)])

                def T_on(qb=qb, qt=qt, tile=tile):
                    bv = PSb[0].rearrange("p (h t) -> p h t", h=8)
                    for h in range(8):
                        S.tr(bv[:, h, :], onb[qt][:, h * 128:(h + 1) * 128], ident[:], [('pl', 'onb', qt), 'ident'], [psk(0)])
                    S.cp('act', qT[:, :, tile * 128:(tile + 1) * 128], bv, [psk(0)], [('qT', qb)])
                pending_T.append(T_on)

        def at_B(n):
            qb, h, jn, j, nk_ = items[n]
            sbk = n % 3
            ab = [4 + 2 * (h % 2), 5 + 2 * (h % 2)]
            if h == 2 and jn == 0:
                while pending_T:
                    pending_T.pop(0)()
            for qt in range(2):
                for i in range(2):
                    S.mm(PS[ab[qt]][:, i * 132:i * 132 + 129], PT[sbk][:, i * 256 + qt * 128: i * 256 + (qt + 1) * 128],
                         vtok[:, j, h, 0:129], jn == 0 and i == 0, jn == nk_ - 1,
                         [('pl', 'PT', sbk), ('vt', j), ('vt', 'ones')], [psk(ab[qt])], skip=True)
        def at_C(n):
            qb, h, jn, j, nk_ = items[n]
            ab = [4 + 2 * (h % 2), 5 + 2 * (h % 2)]
            if jn == nk_ - 1:
                for qt in range(2):
                    av = PS[ab[qt]][:, 0:264].rearrange("p (i e) -> p i e", i=2)
                    sl4 = o2c[0] % 4
                    o2c[0] += 1
                    S.recip(rz[:, sl4, :, :], av[:, :, 128:129], [psk(ab[qt])], [('rz', sl4)])
                    S.ts('dve', r2n[:, sl4, :], rz[:, sl4, 1, :], neg_lam, None, ALU.mult, None, [('rz', sl4), 'neg_lam'], [('r2n', sl4)])
                    S.act(tO2[sl4], av[:, 1, 0:128], AF.Copy, [psk(ab[qt]), ('r2n', sl4)], [('pl', 'tO2', sl4)], scale=r2n[:, sl4, :])
                    S.stt(ost[qb % 2][qt][:, h, :], av[:, 0, 0:128], rz[:, sl4, 0, :], tO2[sl4], ALU.mult, ALU.add,
                          [psk(ab[qt]), ('rz', sl4), ('pl', 'tO2', sl4)], [('ost', qb % 2, qt)])
                if h == 7:
                    subln(qb)

        NI = len(items)
        CL = 2 if p == 0 else 3
        for s_ in range(NI + CL):
            if s_ < NI:
                at_A(s_)
            if 0 <= s_ - 1 < NI:
                at_B(s_ - 1)
            if 0 <= s_ - CL < NI:
                at_C(s_ - CL)
        while pending_T:
            pending_T.pop(0)()
        if p == 0:
            dbg('onT', qT, [('qT', i) for i in range(4)])

        S.fence(('ACC', 'PL'))
        sg0 = [PLb[:, i * 512:(i + 1) * 512] for i in range(2)]
        sg1 = [PLb[:, 1024 + i * 512: 1536 + i * 512] for i in range(2)]
        t0 = PL[:, 1024:1536]
        t1 = PL[:, 1536:2048]
        wtmp = [PL[:, 2048 + i * 512: 2560 + i * 512] for i in range(2)]
        it = 0
        for ng in range(4):
            ug0, ug0k, ug0j = W.acquire()
            ug1, ug1k, ug1j = W.acquire()
            ua, uak, uaj = W.acquire()
            up, upk, upj = W.acquire()
            for blk in range(2):
                tsl = slice(blk * 512, (blk + 1) * 512)
                for cc in range(2):
                    c = 2 * ng + cc
                    b0 = (it % 2) * 4
                    s2 = it % 2
                    it += 1
                    csl = slice(cc * 128, (cc + 1) * 128)
                    for kc in range(8):
                        S.mm(PS[b0][:, 0:512], ug0[:, kc, csl], hT[:, kc, tsl], kc == 0, kc == 7, [('hT', blk), ug0k], [psk(b0)])
                    for kc in range(8):
                        S.mm(PS[b0 + 1][:, 0:512], ug1[:, kc, csl], hT[:, kc, tsl], kc == 0, kc == 7, [('hT', blk), ug1k], [psk(b0 + 1)])
                    for kc in range(8):
                        S.mm(PS[b0 + 2][:, 0:512], ua[:, kc, csl], qT[:, kc, tsl], kc == 0, kc == 7,
                             [('qT', 2 * blk), ('qT', 2 * blk + 1), uak], [psk(b0 + 2)])
                    for fc in range(4):
                        S.mm(PS[b0 + 3][:, 0:512], up[:, fc, csl], mixT[:, fc, tsl], fc == 0, fc == 3, [('mixT', blk), upk], [psk(b0 + 3)])
                    S.act(sg0[s2], PS[b0][:, 0:512], AF.Sigmoid, [psk(b0), 'bgT'], [('pl', 'sg0', s2)], bias=bgT[:, c:c + 1])
                    S.act(sg1[s2], PS[b0 + 1][:, 0:512], AF.Sigmoid, [psk(b0 + 1), 'bgT'], [('pl', 'sg1', s2)], bias=bgT[:, 8 + c:9 + c])
                    S.tt('dve', t0, PS[b0 + 2][:, 0:512], sg0[s2], ALU.mult, [psk(b0 + 2), ('pl', 'sg0', s2)], [('pl', 't0')])
                    S.tt('dve', t1, PS[b0 + 3][:, 0:512], sg1[s2], ALU.mult, [psk(b0 + 3), ('pl', 'sg1', s2)], [('pl', 't1')])
                    S.tt('pool', mrgT[:, c, tsl], t0, t1, ALU.add, [('pl', 't0'), ('pl', 't1')], [('mrgT', blk)])
            for j_ in (ug0j, ug1j, uaj, upj):
                W.release(j_)
        if p == 0:
            dbg('mrgT', mrgT, [('mrgT', 0), ('mrgT', 1)])
        S.fence(('X1',))
        uo = [W.acquire() for _ in range(4)]
        it = 0
        for tile in range(8):
            sl = tile % 2
            xkey = ('xst', sl)
            S.dma('sp', xst[sl][:], x[p * 1024 + tile * 128: p * 1024 + (tile + 1) * 128, :], (), [xkey], xkey)
            for nh in range(2):
                bank = it % 4
                s2 = it % 2
                it += 1
                for j2 in range(2):
                    u_, uk_, _ = uo[nh * 2 + j2]
                    for kc in range(8):
                        S.mm(PS[bank][:, j2 * 256:(j2 + 1) * 256], mrgT[:, kc, tile * 128:(tile + 1) * 128], u_[:, kc, :],
                             kc == 0, kc == 7, [('mrgT', tile // 4), uk_], [psk(bank)])
                nsl = slice(nh * 512, (nh + 1) * 512)
                S.tt('dve', wtmp[s2], PS[bank][:, 0:512], g1b[:, nsl], ALU.mult, [psk(bank), 'g1b'], [('pl', 'wtmp', s2)])
                S.tt('pool', x1[:, tile, nsl], wtmp[s2], xst[sl][:, nsl], ALU.add, [('pl', 'wtmp', s2), xkey], [('x1', tile)])
        for (_, _, j_) in uo:
            W.release(j_)
        if p == 0:
            dbg('x1', x1[:, 0, :], [('x1', 0)])

        S.fence(('PL',))
        S.dma('sp', g1b[:], modrows[p, 0:1, :].to_broadcast([128, 1024]), [('modrows', p)], ['g1b'], 'g1b')
        S.dma('sp', xst[0][:], modrows[p, 1:2, :].to_broadcast([128, 1024]), [('modrows', p)], [('xst', 0)], ('xst', 0))
        norm_T(p, 'x1', (a2T, 'a2T'), sh2, 'hT')
        S.fence(('PL', 'ACC', 'K'))
        lgp = PS[7][:, 0:256].rearrange("p (t n) -> p t n", t=8)
        for tile in range(8):
            for kc in range(8):
                S.mm(lgp[:, tile, 0:20], hT[:, kc, tile * 128:(tile + 1) * 128], rw[:, kc, :], kc == 0, kc == 7,
                     [('hT', tile // 4), 'rw'], [psk(7)])
        R_ = PL[:, 0:2560]

        def rbuf(i, n):
            return R_[:, i * 160: i * 160 + 8 * n].rearrange("p (t n) -> p t n", t=8)

        lg = rbuf(0, 20)
        S.tt('dve', lg, lgp[:, :, 0:20], rb_b[:].rearrange("p (o n) -> p o n", o=1).to_broadcast([128, 8, 20]), ALU.add,
             [psk(7), 'rb_b'], [('pl', 'lg')])
        gl = lg[:, :, 0:4]
        el = lg[:, :, 4:20]
        gmax = rbuf(1, 1)
        S.red(gmax[:, :, 0], gl, ALU.max, [('pl', 'lg')], [('pl', 'gmax')])
        ge = rbuf(2, 4)
        S.tt('dve', ge, gl, gmax.to_broadcast([128, 8, 4]), ALU.subtract, [('pl', 'lg'), ('pl', 'gmax')], [('pl', 'ge')])
        eg = rbuf(3, 4)
        S.act(eg, ge, AF.Exp, [('pl', 'ge')], [('pl', 'eg')])
        gsum = rbuf(4, 1)
        S.red(gsum[:, :, 0], eg, ALU.add, [('pl', 'eg')], [('pl', 'gsum')])
        gw = rbuf(5, 1)
        S.recip(gw, gsum, [('pl', 'gsum')], [('pl', 'gw')])
        pen = rbuf(6, 4)
        S.ts('dve', pen, ge, 0.0, None, ALU.is_ge, None, [('pl', 'ge')], [('pl', 'pen')])
        S.ts('dve', pen, pen, -1.0, BIG, ALU.add, ALU.mult, [('pl', 'pen')], [('pl', 'pen')])
        msk = rbuf(7, 16)
        pen4 = R_[:, 6 * 160: 6 * 160 + 32].rearrange("p (t g o) -> p t g o", t=8, o=1).to_broadcast([128, 8, 4, 4])
        S.tt('dve', msk.rearrange("p t (g e) -> p t g e", g=4), el.rearrange("p t (g e) -> p t g e", g=4), pen4, ALU.add,
             [('pl', 'lg'), ('pl', 'pen')], [('pl', 'msk')])
        m1 = rbuf(8, 1)
        S.red(m1[:, :, 0], msk, ALU.max, [('pl', 'msk')], [('pl', 'm1')])
        o1 = rbuf(9, 16)
        S.tt('dve', o1, msk, m1.to_broadcast([128, 8, 16]), ALU.subtract, [('pl', 'msk'), ('pl', 'm1')], [('pl', 'o1')])
        S.ts('dve', o1, o1, 0.0, None, ALU.is_ge, None, [('pl', 'o1')], [('pl', 'o1')])
        msk2 = rbuf(10, 16)
        S.stt(msk2, o1, -BIG, msk, ALU.mult, ALU.add, [('pl', 'o1'), ('pl', 'msk')], [('pl', 'msk2')])
        m2 = rbuf(11, 1)
        S.red(m2[:, :, 0], msk2, ALU.max, [('pl', 'msk2')], [('pl', 'm2')])
        o2 = rbuf(12, 16)
        S.tt('dve', o2, msk2, m2.to_broadcast([128, 8, 16]), ALU.subtract, [('pl', 'msk2'), ('pl', 'm2')], [('pl', 'o2')])
        S.ts('dve', o2, o2, 0.0, None, ALU.is_ge, None, [('pl', 'o2')], [('pl', 'o2')])
        e21 = rbuf(4, 1)
        S.tt('dve', e21, m2, m1, ALU.subtract, [('pl', 'm2'), ('pl', 'm1'), ('pl', 'gw')], [('pl', 'gsum')])
        S.act(e21, e21, AF.Exp, [('pl', 'gsum')], [('pl', 'gsum')])
        den = rbuf(1, 1)
        S.ts('dve', den, e21, 1.0, None, ALU.add, None, [('pl', 'gsum'), ('pl', 'ge')], [('pl', 'gmax')])
        S.recip(den, den, [('pl', 'gmax')], [('pl', 'gmax')])
        w1 = rbuf(2, 1)
        S.tt('dve', w1, den, gw, ALU.mult, [('pl', 'gmax'), ('pl', 'gw'), ('pl', 'pen'), ('pl', 'eg')], [('pl', 'ge')])
        w2 = rbuf(3, 1)
        S.tt('dve', w2, w1, e21, ALU.mult, [('pl', 'ge'), ('pl', 'gsum')], [('pl', 'eg')])
        tsl = slice(p * 8, (p + 1) * 8)
        S.cp('pool', O1g[:, tsl, :], o1, [('pl', 'o1')], [('O1g', p)])
        S.cp('pool', O2g[:, tsl, :], o2, [('pl', 'o2')], [('O2g', p)])
        S.tt('dve', Mb[:, tsl, :], o1, o2, ALU.add, [('pl', 'o1'), ('pl', 'o2')], [('Mb', p)])
        S.cp('dve', W1g[:, tsl, :], w1, [('pl', 'ge')], [('W1g', p)])
        S.cp('dve', W2g[:, tsl, :], w2, [('pl', 'eg')], [('W2g', p)])
        if p == 0:
            dbg('gates', O1g[:, 0, :], [('O1g', 0)])
            dbg('h2T', hT[:], [('hT', 0), ('hT', 1)])

    def routing():
        S.fence(('PL',))
        rankp = PS[0][:, 0:256].rearrange("p (t e) -> p t e", t=16)
        cntp = PS[1][:, 0:16]
        mk = [('Mb', 0), ('Mb', 1)]
        for T in range(16):
            S.mm(rankp[:, T, :], trib[:], Mb[:, T, :], True, T == 0, mk + ['trib'], [psk(0)])
            for T2 in range(T):
                S.mm(rankp[:, T, :], onesb[:], Mb[:, T2, :], False, T2 == T - 1, mk + ['onesb'], [psk(0)])
        for T in range(16):
            S.mm(cntp, onesb[:], Mb[:, T, :], T == 0, T == 15, mk + ['onesb'], [psk(1)])
        R_ = PL[:, 0:4096]
        off = [0]
        nbuf = [0]

        def ra(n):
            v = R_[:, off[0]:off[0] + n]
            k = ('pl', 'r', nbuf[0])
            off[0] += n
            nbuf[0] += 1
            assert off[0] <= 4096
            return v, k

        def b3(ap2, shape):
            return ap2.rearrange("p (o n) -> p o n", o=1).to_broadcast(shape)

        def l3(ap2, shape):
            return ap2.rearrange("p (n o) -> p n o", o=1).to_broadcast(shape)

        cnt, kcnt = ra(16)
        S.cp('dve', cnt, cntp, [psk(1)], [kcnt])
        cmp, kcmp = ra(256)
        cmp3 = cmp.rearrange("p (e k) -> p e k", e=16)
        cnth, kcnth = ra(16)
        S.ts('dve', cnth, cnt, 0.5, None, ALU.mult, None, [kcnt], [kcnth])
        S.tt('dve', cmp3, l3(cnth, [128, 16, 16]), b3(thr[:], [128, 16, 16]), ALU.is_gt, [kcnth, 'thr'], [kcmp])
        ntl, kntl = ra(16)
        S.red(ntl, cmp3, ALU.add, [kcmp], [kntl])
        nt2 = ntl.rearrange("p (k two) -> p k two", two=2)
        ptot, kptot = ra(8)
        S.tt('dve', ptot, nt2[:, :, 0], nt2[:, :, 1], ALU.add, [kntl], [kptot])
        m8, km8 = ra(8)
        S.tt('dve', m8, nt2[:, :, 0], nt2[:, :, 1], ALU.min, [kntl], [km8])
        lb8, klb8 = ra(8)
        S.tt('dve', lb8, nt2[:, :, 1], nt2[:, :, 0], ALU.is_gt, [kntl], [klb8])
        ones8, kones8 = ra(8)
        S.memset('dve', ones8, 1.0, [kones8])
        pincl8, kpincl = ra(8)
        S.add('dve', lambda e: e.tensor_tensor_scan(pincl8, ones8, ptot, 0.0, ALU.mult, ALU.add), [kones8, kptot], [kpincl])
        pbase8, kpbase = ra(8)
        S.tt('dve', pbase8, pincl8, ptot, ALU.subtract, [kpincl, kptot], [kpbase])
        pbe, kpbe = ra(16)
        S.cp('dve', pbe.rearrange("p (k two) -> p k two", two=2), l3(pbase8, [128, 8, 2]), [kpbase], [kpbe])
        me, kme = ra(16)
        S.cp('dve', me.rearrange("p (k two) -> p k two", two=2), l3(m8, [128, 8, 2]), [km8], [kme])
        par16 = rtab[:, 0:16]
        kidx8 = rtab[:, 16:24]
        c2t = rtab[:, 24:48]
        prod, kprod = ra(256)
        prod3 = prod.rearrange("p (t e) -> p t e", t=16)
        sf, ksf = ra(32)
        sf3 = sf.rearrange("p (k t) -> p k t", k=2)
        for k, (Og, okey) in enumerate(((O1g, 'O1g'), (O2g, 'O2g'))):
            ok2 = [(okey, 0), (okey, 1)]
            rk, krk = ra(16)
            S.tt('dve', prod3, Og[:], rankp, ALU.mult, ok2 + [psk(0)], [kprod])
            S.red(rk, prod3, ALU.add, [kprod], [krk])
            PBk, kPB = ra(16)
            S.tt('dve', prod3, Og[:], b3(pbe, [128, 16, 16]), ALU.mult, ok2 + [kpbe], [kprod])
            S.red(PBk, prod3, ALU.add, [kprod], [kPB])
            Mk, kM = ra(16)
            S.tt('dve', prod3, Og[:], b3(me, [128, 16, 16]), ALU.mult, ok2 + [kme], [kprod])
            S.red(Mk, prod3, ALU.add, [kprod], [kM])
            PARk, kPAR = ra(16)
            S.tt('dve', prod3, Og[:], b3(par16, [128, 16, 16]), ALU.mult, ok2 + ['rtab'], [kprod])
            S.red(PARk, prod3, ALU.add, [kprod], [kPAR])
            jc, kjc = ra(240)
            jc3 = jc.rearrange("p (t q) -> p t q", t=16)
            rkh, krkh = ra(16)
            S.ts('dve', rkh, rk, 0.5, None, ALU.mult, None, [krk], [krkh])
            S.tt('dve', jc3, l3(rkh, [128, 16, 15]), b3(thr[:, 1:16], [128, 16, 15]), ALU.is_ge, [krkh, 'thr'], [kjc])
            jk, kjk = ra(16)
            S.red(jk, jc3, ALU.add, [kjc], [kjk])
            mn, kmn = ra(16)
            S.tt('dve', mn, jk, Mk, ALU.min, [kjk, kM], [kmn])
            lt, klt = ra(16)
            S.tt('dve', lt, jk, Mk, ALU.is_lt, [kjk, kM], [klt])
            S.tt('dve', lt, lt, PARk, ALU.mult, [klt, kPAR], [klt])
            S.tt('dve', mn, mn, PBk, ALU.add, [kmn, kPB], [kmn])
            S.tt('dve', mn, mn, lt, ALU.add, [kmn, klt], [kmn])
            S.stt(sf3[:, k, :], mn, 256.0, rk, ALU.mult, ALU.add, [kmn, krk], [(ksf, k)])
        S.cp('dve', s12i[:], sf3, [(ksf, 0), (ksf, 1)], ['s12i'])
        OHa, kOHa = ra(384)
        OHb, kOHb = ra(384)
        OHa3 = OHa.rearrange("p (t k) -> p t k", t=48)
        OHb3 = OHb.rearrange("p (t k) -> p t k", t=48)
        tv3 = l3(tval[:], [128, 48, 8])
        S.tt('dve', OHa3, b3(pbase8, [128, 48, 8]), tv3, ALU.is_le, [kpbase, 'tval'], [kOHa])
        S.tt('dve', OHb3, b3(pincl8, [128, 48, 8]), tv3, ALU.is_gt, [kpincl, 'tval'], [kOHb])
        S.tt('dve', OHa3, OHa3, OHb3, ALU.mult, [kOHa, kOHb], [kOHa])
        gath = {}
        for nm, (tab, tkey) in (('kt', (kidx8, 'rtab')), ('pbt', (pbase8, kpbase)), ('mt', (m8, km8)), ('lbt', (lb8, klb8))):
            g_, kg = ra(48)
            S.tt('dve', OHb3, OHa3, b3(tab, [128, 48, 8]), ALU.mult, [kOHa, tkey], [kOHb])
            S.red(g_, OHb3, ALU.add, [kOHb], [kg])
            gath[nm] = (g_, kg)
        kt_, kkt = gath['kt']
        pbt, kpbt = gath['pbt']
        mt_, kmt = gath['mt']
        lbt, klbt = gath['lbt']
        q_, kq = ra(48)
        S.tt('dve', q_, tval[:], pbt, ALU.subtract, ['tval', kpbt], [kq])
        qc, kqc = ra(1152)
        qc3 = qc.rearrange("p (t c) -> p t c", t=48)
        S.tt('dve', qc3, l3(q_, [128, 48, 24]), b3(c2t, [128, 48, 24]), ALU.is_ge, [kq, 'rtab'], [kqc])
        qh, kqh = ra(48)
        S.red(qh, qc3, ALU.add, [kqc], [kqh])
        qpar, kqpar = ra(48)
        S.stt(qpar, qh, -2.0, q_, ALU.mult, ALU.add, [kqh, kq], [kqpar])
        S.ts('dve', mt_, mt_, 2.0, None, ALU.mult, None, [kmt], [kmt])
        c1, kc1 = ra(48)
        S.tt('dve', c1, q_, mt_, ALU.is_lt, [kq, kmt], [kc1])
        S.tt('dve', qpar, qpar, lbt, ALU.subtract, [kqpar, klbt], [kqpar])
        S.tt('dve', qpar, qpar, c1, ALU.mult, [kqpar, kc1], [kqpar])
        S.tt('dve', qpar, qpar, lbt, ALU.add, [kqpar, klbt], [kqpar])
        etf, ketf = ra(48)
        S.stt(etf, kt_, 2.0, qpar, ALU.mult, ALU.add, [kkt, kqpar], [ketf])
        wifb, kwif = ra(96)
        wif = wifb.rearrange("p (t h) -> p t h", h=2)
        S.ts('dve', wif[:, :, 0], etf, 256.0, p2col[:, 0:1], ALU.mult, ALU.add, [ketf, 'p2col'], [kwif])
        S.ts('dve', wif[:, :, 1], wif[:, :, 0], 1.0, None, ALU.add, None, [kwif], [kwif])
        S.cp('dve', widx[:], wif, [kwif], ['widx'])
        zt = mixT[:].rearrange("p g t -> p (g t)").bitcast(I32)[:, 0:1024]
        mk2 = [('mixT', 0), ('mixT', 1)]
        S.memset('dve', zt, 0, mk2)
        S.dma('sp', slot_tok.rearrange("(p r) c -> p (r c)", p=128), zt, mk2, ['stok0'], 'stok0')
        for T in range(16):
            for k in range(2):
                S.add('pool', (lambda e, T=T, k=k: e.indirect_dma_start(
                    out=slot_tok[:, :], out_offset=bass.IndirectOffsetOnAxis(ap=s12i[:, k, T:T + 1].bitcast(U32), axis=0),
                    in_=tok16[:, T, :], in_offset=None)),
                    ['stok0', 's12i', 'tok16'], [('stok', T, k)], dma='sct')

    def moe_sparse():
        S.fence(('X1', 'ACC', 'K', 'H', 'PL'))
        X1b = X1[:].bitcast(BF16)
        EW = [[X1b[:, u * 2048:(u + 1) * 2048] for u in range(6)],
              [accb[:, u * 2048:(u + 1) * 2048] for u in range(6)]]
        G = [KR[:, i * 1024:(i + 1) * 1024] for i in range(2)]
        hsT = [KR[:, 2048 + i * 1024: 3072 + i * 1024].rearrange("p (k s) -> p k s", k=8) for i in range(2)]
        sil = [KR[:, 4096 + i * 512: 4608 + i * 512] for i in range(2)]
        hidb = [KR[:, 5120 + i * 512: 5632 + i * 512] for i in range(2)]
        hidT = [KR[:, 6144 + i * 512: 6656 + i * 512].rearrange("p (k s) -> p k s", k=4) for i in range(2)]
        hTf = hT[:].rearrange("p c t -> p (c t)").bitcast(F32)
        Yst = [hTf[:, i * 1024:(i + 1) * 1024] for i in range(2)]
        wsrc = [ew_gate.rearrange("e k n -> (e k n)").rearrange("(r c) -> r c", c=2048),
                ew_up.rearrange("e k n -> (e k n)").rearrange("(r c) -> r c", c=2048),
                ew_down.rearrange("e k n -> (e k n)").rearrange("(r c) -> r c", c=2048)]
        stok_keys = [('stok', T, k) for T in range(16) for k in range(2)]
        h2keys = [('h2d', p_, t_) for p_ in range(2) for t_ in range(8)]

        bcreg = {}

        def wload(e, P, u, m, h, b):
            return e.indirect_dma_start(
                out=EW[b][u], out_offset=None, in_=wsrc[m],
                in_offset=bass.IndirectOffsetOnAxis(ap=widx[:, P, h:h + 1].bitcast(U32), axis=0))

        def L_w(P, units=range(6)):
            b = P % 2
            for u in units:
                m, h = divmod(u, 2)
                S.add('pool', (lambda e, P=P, u=u, m=m, h=h, b=b: wload(e, P, u, m, h, b)),
                      ['widx'], [('ew%d' % b, u)], dma=('ew', b, u))

        def L_g(t):
            b = t % 2
            S.dma('sp', gidx[:, b:b + 1], slot_tok[t * 128:(t + 1) * 128, 0:1], stok_keys, [('gidx', b)], ('gidx', b), slow=True)
            S.add('pool', (lambda e, t=t, b=b: e.indirect_dma_start(
                out=G[b], out_offset=None, in_=h2d[:, :],
                in_offset=bass.IndirectOffsetOnAxis(ap=gidx[:, b:b + 1].bitcast(U32), axis=0))),
                [('gidx', b)] + h2keys, [('mg', 'G', b)], dma=('G', b))

        def T1(t):
            b = t % 2
            bv = PSb[b].rearrange("p (k s) -> p k s", k=8)
            g3 = G[b].rearrange("s (p k) -> s k p", k=8)
            for kc in range(8):
                S.tr(bv[:, kc, :], g3[:, kc, :], ident[:], [('mg', 'G', b), 'ident'], [psk(b)])
            S.cp('act', hsT[b], bv, [psk(b)], [('mg', 'hsT', b)])

        def A_(t):
            b = t % 2
            wb = (t // 2) % 2
            for kc in range(8):
                S.mm(PS[2][:, 0:512], hsT[b][:, kc, :], EW[wb][kc // 4][:, (kc % 4) * 512:(kc % 4 + 1) * 512], kc == 0, kc == 7,
                     [('mg', 'hsT', b), ('ew%d' % wb, kc // 4)], [psk(2)])
            for kc in range(8):
                S.mm(PS[3][:, 0:512], hsT[b][:, kc, :], EW[wb][2 + kc // 4][:, (kc % 4) * 512:(kc % 4 + 1) * 512], kc == 0, kc == 7,
                     [('mg', 'hsT', b), ('ew%d' % wb, 2 + kc // 4)], [psk(3)])
            S.act(sil[b], PS[2][:, 0:512], AF.Silu, [psk(2)], [('mg', 'sil', b)])
            S.tt('dve', hidb[b], PS[3][:, 0:512], sil[b], ALU.mult, [psk(3), ('mg', 'sil', b)], [('mg', 'hid', b)])

        def T2D(t):
            b = t % 2
            bv = PSb[4].rearrange("p (k s) -> p k s", k=8)
            h3 = hidb[b].rearrange("s (p k) -> s k p", k=4)
            for fc in range(4):
                S.tr(bv[:, fc, :], h3[:, fc, :], ident[:], [('mg', 'hid', b), 'ident'], [psk(4)])
            S.cp('act', hidT[b], bv[:, 0:4, :], [psk(4)], [('mg', 'hidT', b)])
            for nh in range(2):
                for fc in range(4):
                    S.mm(PS[5 + nh][:, 0:512], hidT[b][:, fc, :], EW[(t // 2) % 2][4 + fc // 2][:, (fc % 2) * 1024 + nh * 512:(fc % 2) * 1024 + (nh + 1) * 512],
                         fc == 0, fc == 3, [('mg', 'hidT', b), ('ew%d' % ((t // 2) % 2), 4 + fc // 2)], [psk(5 + nh)])
            S.cp('act', Yst[b][:, 0:512], PS[5][:, 0:512], [psk(5)], [('yst', b, 0)])
            S.cp('dve', Yst[b][:, 512:1024], PS[6][:, 0:512], [psk(6)], [('yst', b, 1)])
            S.dma('sp', yslot[t * 128:(t + 1) * 128, :], Yst[b], [('yst', b, 0), ('yst', b, 1)], [('ysl', t)], ('yso', b))

        L_g(0)
        L_w(0)
        L_g(1)
        L_w(1)
        T1(0)
        for s_ in range(NTS + 1):
            if s_ + 1 < NTS:
                T1(s_ + 1)
            if s_ < NTS:
                A_(s_)
                if s_ % 2 == 1 and s_ // 2 + 2 < NPAIR:
                    L_w(s_ // 2 + 2, range(4))
            if s_ + 2 < NTS:
                L_g(s_ + 2)
            if 0 <= s_ - 1 < NTS:
                T2D(s_ - 1)
                if (s_ - 1) % 2 == 1 and (s_ - 1) // 2 + 2 < NPAIR:
                    L_w((s_ - 1) // 2 + 2, range(4, 6))

    def final(p):
        S.fence(('X1', 'PL'))
        S.dma('sp', g2b[:], g2row[p:p + 1, :].to_broadcast([128, 1024]), [('g2row', p)], ['g2b'], 'g2b')
        FR = [[X1[:, (k * 2 + i) * 1024:(k * 2 + i + 1) * 1024] for i in range(2)] for k in range(2)]
        ysl_keys = [('ysl', t) for t in range(NTS)]
        for tile in range(8):
            T = p * 8 + tile
            sl = tile % 2
            xkey = ('xst', sl)
            S.dma('sp', xst[sl][:], x1d[p * 1024 + tile * 128: p * 1024 + (tile + 1) * 128, :],
                  [('x1d', p, t_) for t_ in range(8)], [xkey], xkey)
            for k in range(2):
                S.add('pool', (lambda e, k=k, T=T, sl=sl: e.indirect_dma_start(
                    out=FR[k][sl], out_offset=None, in_=yslot[:, :],
                    in_offset=bass.IndirectOffsetOnAxis(ap=s12i[:, k, T:T + 1].bitcast(U32), axis=0))),
                    ['s12i'] + ysl_keys, [('fr', k, sl)], dma=('fr', k, sl))
            r1, r2 = FR[0][sl], FR[1][sl]
            S.act(r1, r1, AF.Copy, [('fr', 0, sl), ('W1g', p)], [('fr', 0, sl)], scale=W1g[:, T, :])
            S.stt(r1, r2, W2g[:, T, :], r1, ALU.mult, ALU.add, [('fr', 1, sl), ('fr', 0, sl), ('W2g', p)], [('fr', 0, sl)])
            S.tt('dve', r1, r1, g2b[:], ALU.mult, [('fr', 0, sl), 'g2b'], [('fr', 0, sl)])
            S.tt('dve', r1, r1, xst[sl][:], ALU.add, [('fr', 0, sl), xkey], [('fr', 0, sl)])
            S.dma('sp', y[p * 1024 + tile * 128: p * 1024 + (tile + 1) * 128, :], r1, [('fr', 0, sl)], [], ('yo', sl))

    run_pass(0)
    run_pass(1)
    routing()
    moe_sparse()
    final(0)
    final(1)
    S.emit()
    return nc, S


_CACHE = {}


def _consts():
    rows = 1024 // 64
    row_ids = np.repeat(np.arange(rows, dtype=np.float32), 64)
    col_ids = np.tile(np.arange(64, dtype=np.float32), rows)
    inv_freq = np.power(np.float32(10000.0), -np.arange(16, dtype=np.float32) / np.float32(16)).astype(np.float32)
    ang_r = row_ids[:, None] * inv_freq[None, :]
    ang_c = col_ids[:, None] * inv_freq[None, :]
    ang = np.concatenate([ang_r, ang_r, ang_c, ang_c], axis=-1).astype(np.float32)
    cos = np.cos(ang).astype(np.float32)
    sin = np.sin(ang).astype(np.float32)
    sgn = np.concatenate([-np.ones(16), np.ones(16), -np.ones(16), np.ones(16)]).astype(np.float32)
    sin_f = (sin * sgn[None, :]).astype(np.float32)
    rcb = np.zeros((1, 64), np.float32)
    for g, w in enumerate((2, 4, 8, 16)):
        hw = w // 2
        for t in range(hw):
            rcb[0, g * 16 + t] = 1.0 / (t + hw)
            rcb[0, g * 16 + 8 + t] = 1.0 / (w - t)
    return cos, sin_f, rcb, np.eye(128, dtype=np.float32)


def kernel(x_prompt, x_sample, c, cache_k, cache_v, c_ctx, w_ada, b_ada, norm1_g, w_in, b_gate,
           q_norm_g, k_norm_g, lambda_q1, lambda_k1, lambda_q2, lambda_k2, subln_g, pool_w, pool_scale,
           w_br_attn, w_br_pool, w_out, norm2_g, router_group_w, router_group_b, router_expert_w,
           router_expert_b, expert_w_gate, expert_w_up, expert_w_down, _dump=None):
    f = lambda a: np.ascontiguousarray(np.asarray(a, dtype=np.float32))
    key = tuple(_dump) if _dump else None
    if key not in _CACHE:
        _CACHE[key] = build_program(_dump)
    nc, S = _CACHE[key]
    cos, sin_f, rcb, eye = _consts()
    x_prompt = f(x_prompt); x_sample = f(x_sample); c = f(c); c_ctx = f(c_ctx)
    cache_k = f(cache_k); cache_v = f(cache_v)
    shared = {
        "w_ada": f(w_ada)[0], "b_ada": f(b_ada), "norm1_g": f(norm1_g), "w_in": f(w_in)[0], "b_gate": f(b_gate),
        "q_norm_g": f(q_norm_g), "k_norm_g": f(k_norm_g),
        "lam4": np.concatenate([f(lambda_q1), f(lambda_k1), f(lambda_q2), f(lambda_k2)], axis=1),
        "subln_g": f(subln_g), "pool_w": f(pool_w)[0], "pool_scale": f(pool_scale),
        "w_br_attn": f(w_br_attn)[0], "w_br_pool": f(w_br_pool)[0], "w_out": f(w_out)[0], "norm2_g": f(norm2_g),
        "router_w": np.concatenate([f(router_group_w)[0], f(router_expert_w)[0]], axis=1),
        "router_b": np.concatenate([f(router_group_b), f(router_expert_b)], axis=1),
        "ew_gate": f(expert_w_gate)[0], "ew_up": f(expert_w_up)[0], "ew_down": f(expert_w_down)[0],
        "ident": eye, "cos_t": cos, "sin_t": sin_f, "rcb": rcb,
        "tri": np.triu(np.ones((128, 128), np.float32), k=1),
        "thr": (128.0 * np.arange(16, dtype=np.float32)).reshape(1, 16),
        "tval": np.arange(48, dtype=np.float32).reshape(1, 48),
        "rtab": np.concatenate([np.tile(np.array([0.0, 1.0], np.float32), 8), np.arange(8, dtype=np.float32),
                                2.0 * np.arange(1, 25, dtype=np.float32), np.zeros(16, np.float32)]).reshape(1, 64),
    }
    in_maps = []
    for i in range(NCORES):
        m = dict(shared)
        m["x"] = np.concatenate([x_prompt[4 * i:4 * i + 4].reshape(1024, 1024), x_sample[i]], axis=0)
        m["cond"] = np.stack([c_ctx, c[i]], axis=0)
        m["ck"] = cache_k[i, 0].reshape(512, 1024)
        m["cv"] = cache_v[i, 0].reshape(512, 1024)
        in_maps.append(m)
    res = run_bass_kernel_spmd(nc, in_maps, core_ids=list(range(NCORES)))
    R = res.results
    y_prompt = np.concatenate([R[i]["y"][0:1024].reshape(4, 256, 1024) for i in range(NCORES)], axis=0)
    y_sample = np.stack([R[i]["y"][1024:2048] for i in range(NCORES)], axis=0)
    new_k = np.concatenate([R[i]["nk"].reshape(4, 1, 256, 8, 128) for i in range(NCORES)], axis=0)
    new_v = np.concatenate([R[i]["nv"].reshape(4, 1, 256, 8, 128) for i in range(NCORES)], axis=0)
    if _dump:
        kernel.last_dumps = [{nm: R[i]["dbg_" + nm] for nm, _ in _dump} for i in range(NCORES)]
    return (y_prompt.astype(np.float32), y_sample.astype(np.float32), new_k.astype(np.float32), new_v.astype(np.float32))
```

```python
import numpy as np
import concourse.bass as bass
import concourse.mybir as mybir
from concourse.bass_utils import run_bass_kernel_spmd

F32 = mybir.dt.float32
BF16 = mybir.dt.bfloat16
I32 = mybir.dt.int32
U32 = mybir.dt.uint32
AF = mybir.ActivationFunctionType
ALU = mybir.AluOpType
AX = mybir.AxisListType

EPS = 1e-6
NCORES = 8
LAMBDA_INIT = 0.8 - 0.6 * 1.0
BIG = 1.0e9

REGION_OF = {
    'vt': 'X1', 'x1': 'X1',
    'qT': 'ACC', 'mrgT': 'ACC', 'acc': 'ACC', 'st2': 'ACC', 'ost': 'ACC',
    'kT': 'K', 'silu': 'K', 'hid': 'K',
    'pl': 'PL',
    'ew0': 'X1', 'fr': 'X1', 'ew1': 'ACC', 'mg': 'K', 'hT': 'H', 'yst': 'H',
}


class Sched:
    def __init__(self, nc, scratch):
        self.nc = nc
        self.ops = []
        self.lastw = {}
        self.readers = {}
        self.scratch = scratch
        self.seen = {}

    def add(self, eng, fn, reads=(), writes=(), dma=None):
        writes = list(writes) + [k for k in reads if isinstance(k, tuple) and k[0] == 'ps' and k not in writes]
        reads = [k for k in reads if not (isinstance(k, tuple) and k[0] == 'ps')]
        for k in reads + writes:
            nm = k[0] if isinstance(k, tuple) else k
            rg = REGION_OF.get(nm)
            if rg is not None:
                self.seen.setdefault(rg, set()).add(k)
                fk = ('fence', rg)
                if fk not in reads:
                    reads.append(fk)
        idx = len(self.ops)
        deps = set()
        for k in reads:
            w = self.lastw.get(k)
            if w is not None:
                deps.add(w)
        for k in writes:
            w = self.lastw.get(k)
            if w is not None:
                deps.add(w)
            for r in self.readers.get(k, ()):
                deps.add(r)
        deps.discard(idx)
        self.ops.append(dict(eng=eng, fn=fn, deps=deps, dma=dma))
        for k in reads:
            self.readers.setdefault(k, []).append(idx)
        for k in writes:
            self.lastw[k] = idx
            self.readers[k] = []
        return idx

    def fence(self, regions=('X1', 'ACC', 'K', 'PL', 'H')):
        for rg in regions:
            keys = list(self.seen.get(rg, ())) + [('fence', rg), 'fscr']
            sc = self.scratch
            idx = len(self.ops)
            deps = set()
            for k in keys:
                w = self.lastw.get(k)
                if w is not None:
                    deps.add(w)
                for r in self.readers.get(k, ()):
                    deps.add(r)
            self.ops.append(dict(eng='dve', fn=(lambda e: e.memset(sc, 0.0)), deps=deps, dma=None))
            for k in keys:
                self.lastw[k] = idx
                self.readers[k] = []

    def mm(self, out, lhsT, rhs, start, stop, reads, writes, skip=False):
        if skip:
            return self.add('pe', lambda e: e.matmul(out, lhsT, rhs, start=start, stop=stop, skip_group_check=True),
                            reads, writes)
        return self.add('pe', lambda e: e.matmul(out, lhsT, rhs, start=start, stop=stop), reads, writes)

    def tr(self, out, in_, ident, reads, writes):
        return self.add('pe', lambda e: e.transpose(out, in_, ident), reads, writes)

    def act(self, out, in_, func, reads, writes, bias=None, scale=None, accum_out=None):
        kw = {}
        if bias is not None:
            kw['bias'] = bias
        if scale is not None:
            kw['scale'] = scale
        if accum_out is not None:
            kw['accum_out'] = accum_out
        return self.add('act', lambda e: e.activation(out, in_, func, **kw), reads, writes)

    def tt(self, eng, out, in0, in1, op, reads, writes):
        return self.add(eng, lambda e: e.tensor_tensor(out, in0, in1, op), reads, writes)

    def ts(self, eng, out, in0, s1, s2, op0, op1, reads, writes):
        if op1 is None:
            return self.add(eng, lambda e: e.tensor_scalar(out, in0, s1, None, op0), reads, writes)
        return self.add(eng, lambda e: e.tensor_scalar(out, in0, s1, s2, op0, op1), reads, writes)

    def stt(self, out, in0, scalar, in1, op0, op1, reads, writes):
        return self.add('dve', lambda e: e.scalar_tensor_tensor(out, in0, scalar, in1, op0, op1), reads, writes)

    def red(self, out, in_, op, reads, writes):
        return self.add('dve', lambda e: e.tensor_reduce(out, in_, AX.X, op), reads, writes)

    def recip(self, out, in_, reads, writes):
        return self.add('dve', lambda e: e.reciprocal(out, in_), reads, writes)

    def cp(self, eng, out, in_, reads, writes):
        if eng == 'act':
            return self.add('act', lambda e: e.copy(out, in_), reads, writes)
        return self.add(eng, lambda e: e.tensor_copy(out, in_), reads, writes)

    def memset(self, eng, ap, val, writes):
        return self.add(eng, lambda e: e.memset(ap, val), (), writes)

    def dma(self, q, out, in_, reads, writes, key, slow=False):
        if slow:
            return self.add(q, lambda e: e.dma_start(out=out, in_=in_, allow_slow_non_contiguous=True),
                            reads, writes, dma=key)
        return self.add(q, lambda e: e.dma_start(out=out, in_=in_), reads, writes, dma=key)

    def emit(self, final_wait_eng='sp'):
        nc = self.nc
        ops = self.ops
        n = len(ops)
        has_dep = [False] * n
        for i, o in enumerate(ops):
            latest = {}
            keep = set()
            for d in o['deps']:
                od = ops[d]
                if od['dma'] is not None:
                    keep.add(d)
                    continue
                if od['eng'] == 'pe' and o['eng'] == 'pe' and o['dma'] is None:
                    continue
                if d > latest.get(od['eng'], -1):
                    latest[od['eng']] = d
            keep.update(latest.values())
            o['deps'] = keep
            for d in keep:
                has_dep[d] = True
        eng_names = ['sp', 'act', 'pool', 'dve', 'pe']
        eng_sem = {e: nc.alloc_semaphore(name='sem_' + e) for e in eng_names}
        dma_sems = {}
        dma_cnt = {}
        eng_cnt = {e: 0 for e in eng_names}
        sig = [None] * n
        for i, o in enumerate(ops):
            if o['dma'] is not None:
                k = o['dma']
                if k not in dma_sems:
                    dma_sems[k] = nc.alloc_semaphore(name='dsem%d' % len(dma_sems))
                    dma_cnt[k] = 0
                dma_cnt[k] += 16
                sig[i] = (dma_sems[k], dma_cnt[k], 16)
            elif has_dep[i]:
                eng_cnt[o['eng']] += 1
                sig[i] = (eng_sem[o['eng']], eng_cnt[o['eng']], 1)
        self.n_sems = len(dma_sems) + 5
        self.eng_cnt = eng_cnt
        streams = {e: [i for i, o in enumerate(ops) if o['eng'] == e] for e in eng_names}
        finals = [(dma_sems[k], dma_cnt[k]) for k in dma_sems]

        def run_stream(ename, eng):
            waited = {}
            for i in streams[ename]:
                o = ops[i]
                need = {}
                for d in o['deps']:
                    s, v, _ = sig[d]
                    if v > need.get(s.num, (None, 0))[1]:
                        need[s.num] = (s, v)
                for num in sorted(need):
                    s, v = need[num]
                    if waited.get(num, 0) < v:
                        eng.wait_ge(s, v)
                        waited[num] = v
                ins = o['fn'](eng)
                if sig[i] is not None:
                    ins.then_inc(sig[i][0], sig[i][2])
            if ename == final_wait_eng:
                for s, v in finals:
                    if waited.get(s.num, 0) < v:
                        eng.wait_ge(s, v)

        with nc.Block() as block:
            @block.sync
            def _(e):
                run_stream('sp', e)

            @block.scalar
            def _(e):
                run_stream('act', e)

            @block.gpsimd
            def _(e):
                run_stream('pool', e)

            @block.vector
            def _(e):
                run_stream('dve', e)

            @block.tensor
            def _(e):
                run_stream('pe', e)


class WRing:
    R = 10

    def __init__(self, S, ring, units):
        self.S = S
        self.ring = ring
        self.units = units
        self.next_dma = 0
        self.next_acq = 0
        self.rel = set()
        self.pump()

    def pump(self):
        while self.next_dma < len(self.units):
            j = self.next_dma
            if j >= self.R and (j - self.R) not in self.rel:
                break
            src, kc, ncols = self.units[j]
            slot = j % self.R
            dst = self.ring[:, slot, 0:kc * ncols].rearrange("p (k n) -> p k n", k=kc)
            self.S.dma('pool', dst, src, (), [('w', slot)], ('w', slot))
            self.next_dma += 1

    def acquire(self):
        j = self.next_acq
        assert j < self.next_dma, "weight ring deadlock: unit %d not yet issued" % j
        self.next_acq += 1
        src, kc, ncols = self.units[j]
        slot = j % self.R
        view = self.ring[:, slot, 0:kc * ncols].rearrange("p (k n) -> p k n", k=kc)
        return view, ('w', slot), j

    def release(self, j):
        self.rel.add(j)
        self.pump()


def build_program(dump=None):
    nc = bass.Bass("TRN2", target_bir_lowering=False)
    dump = dump or []

    def din(name, shape):
        return nc.dram_tensor(name, list(shape), F32, kind="ExternalInput").ap()

    def dout(name, shape):
        return nc.dram_tensor(name, list(shape), F32, kind="ExternalOutput").ap()

    x = din("x", (2048, 1024))
    cond = din("cond", (2, 1024))
    ck = din("ck", (512, 1024))
    cv = din("cv", (512, 1024))
    w_ada = din("w_ada", (1024, 6144))
    b_ada = din("b_ada", (1, 6144))
    norm1_g = din("norm1_g", (1, 1024))
    w_in = din("w_in", (1024, 5632))
    b_gate = din("b_gate", (1, 2048))
    q_norm_g = din("q_norm_g", (1, 64))
    k_norm_g = din("k_norm_g", (1, 64))
    lam_in = din("lam4", (1, 256))
    subln_g = din("subln_g", (1, 128))
    pool_w = din("pool_w", (4, 128, 128))
    pool_scale = din("pool_scale", (1, 512))
    w_br_attn = din("w_br_attn", (1024, 1024))
    w_br_pool = din("w_br_pool", (512, 1024))
    w_out = din("w_out", (1024, 1024))
    norm2_g = din("norm2_g", (1, 1024))
    router_w = din("router_w", (1024, 20))
    router_b = din("router_b", (1, 20))
    ew_gate = din("ew_gate", (16, 1024, 512))
    ew_up = din("ew_up", (16, 1024, 512))
    ew_down = din("ew_down", (16, 512, 1024))
    ident_d = din("ident", (128, 128))
    cos_d = din("cos_t", (1024, 64))
    sin_d = din("sin_t", (1024, 64))
    rcb_d = din("rcb", (1, 64))

    tri_d = din("tri", (128, 128))
    thr_d = din("thr", (1, 16))
    tval_d = din("tval", (1, 48))
    rtab_d = din("rtab", (1, 64))
    NMAIN = 48
    NTB = 14
    NTAIL = 2 * NTB
    NTS = NMAIN + NTAIL
    modrows = nc.dram_tensor("modrows", [2, 2, 1024], F32, kind="Internal").ap()
    g2row = nc.dram_tensor("g2row", [2, 1024], F32, kind="Internal").ap()
    gps = nc.dram_tensor("gps", [2, 1024], F32, kind="Internal").ap()
    h2d = nc.dram_tensor("h2d", [2048, 1024], BF16, kind="Internal").ap()
    x1d = nc.dram_tensor("x1d", [2048, 1024], F32, kind="Internal").ap()
    slot_tok = nc.dram_tensor("slot_tok", [80 * 128, 16], I32, kind="Internal").ap()
    yslot = nc.dram_tensor("yslot", [NTS * 128, 1024], F32, kind="Internal").ap()
    y = dout("y", (2048, 1024))
    nk = dout("nk", (1024, 1024))
    nv = dout("nv", (1024, 1024))
    dumps = {}
    for nm, shp in dump:
        dumps[nm] = dout("dbg_" + nm, shp)

    A = nc.alloc_sbuf_tensor
    ring = A("ring", [128, 10, 2048], BF16)
    hT = A("hT", [128, 8, 1024], BF16)
    X1 = A("X1", [128, 8192], F32)
    x1 = X1[:].rearrange("p (t d) -> p t d", t=8)
    vtok = X1[:].bitcast(BF16)[:, 0:12 * 8 * 132].rearrange("p (j h e) -> p j h e", j=12, h=8)
    ACC = A("ACC", [128, 8192], F32)
    acc = ACC[:].rearrange("p (t d) -> p t d", t=8)
    accb = ACC[:].bitcast(BF16)
    qT = accb[:, 0:8192].rearrange("p (h t) -> p h t", h=8)
    mrgT = accb[:, 8192:16384].rearrange("p (h t) -> p h t", h=8)
    mrg_f = ACC[:, 4096:8192]
    KR = A("KR", [128, 8 * 1536], BF16)
    kT = KR[:].rearrange("p (h t) -> p h t", h=8)
    silu_b = [KR[:, i * 512:(i + 1) * 512] for i in range(2)]
    hid = [KR[:, 1024 + i * 4096: 1024 + (i + 1) * 4096].rearrange("p (b f t) -> p b f t", b=2, f=4) for i in range(2)]
    mixT = A("mixT", [128, 4, 1024], BF16)
    PL = A("PL", [128, 4096], F32)
    PLb = PL[:].bitcast(BF16)
    xst = [A("xst%d" % i, [128, 1024], F32) for i in range(2)]
    xn = [A("xn%d" % i, [128, 1024], BF16) for i in range(2)]
    ident = A("identb", [128, 128], BF16)
    cosb = A("cosb", [128, 8, 64], F32)
    sinb = A("sinb", [128, 8, 64], F32)
    gq_b = A("gq_b", [128, 64], F32)
    gk_b = A("gk_b", [128, 64], F32)
    gsub_b = A("gsub_b", [128, 128], F32)
    rcb = A("rcbs", [128, 64], F32)
    lam_b = A("lam_b", [128, 256], F32)
    lam_t = A("lam_t", [128, 128], F32)
    lam_s = A("lam_s", [128, 8], F32)
    sc32 = A("sc32", [128, 8, 2], F32)
    scT2 = A("scT2", [128, 8, 2], BF16)
    screp = A("screp", [128, 8, 128], BF16)
    screp1 = A("screp1", [128, 8, 128], BF16)
    modT2 = A("modT2", [128, 48, 2], F32)
    bT = A("bT", [128, 48], F32)
    n1gT = A("n1gT", [128, 8], F32)
    n2gT = A("n2gT", [128, 8], F32)
    a1T2 = A("a1T2", [128, 8, 2], F32)
    a2T2 = A("a2T2", [128, 8, 2], F32)
    g1b = A("g1b", [128, 1024], F32)
    g2b = A("g2b", [128, 1024], F32)
    bgT = A("bgT", [128, 16], F32)
    pscT = A("pscT", [128, 4], F32)
    rb_b = A("rb_b", [128, 20], F32)
    poolw = A("poolw", [128, 4, 128], BF16)
    rw = A("rw", [128, 8, 20], BF16)
    ss = A("ss", [128, 8], F32)
    rstd = A("rstd", [128, 8], F32)
    ss8 = A("ss8", [128, 2, 8], F32)
    rs8 = A("rs8", [128, 2, 8, 1], F32)
    ss8c = A("ss8c", [128, 8], F32)
    rs8c = A("rs8c", [128, 8, 1], F32)
    rz = A("rz", [128, 4, 2, 1], F32)
    r2n = A("r2n", [128, 4, 1], F32)
    O1g = A("O1g", [128, 16, 16], F32)
    O2g = A("O2g", [128, 16, 16], F32)
    W1g = A("W1g", [128, 16, 1], F32)
    W2g = A("W2g", [128, 16, 1], F32)
    Mb = A("Mb", [128, 16, 16], BF16)
    s12i = A("s12i", [128, 2, 16], I32)
    widx = A("widx", [128, 28, 2], I32)
    gidx = A("gidx", [128, 76], I32)
    trib = A("trib", [128, 128], BF16)
    onesb = A("onesb", [128, 128], BF16)
    thr = A("thr_sb", [128, 16], F32)
    tval = A("tval_sb", [128, 48], F32)
    tok16 = A("tok16", [128, 16, 16], I32)
    p2col = A("p2col", [128, 1], F32)
    rtab = A("rtab_sb", [128, 64], F32)
    fscr = A("fscr", [128, 1], F32)

    PSALL = nc.alloc_psum_tensor("psall", [128, 4096], F32)
    PSALLb = PSALL[:].bitcast(BF16)
    PS = [PSALL[:, i * 512:(i + 1) * 512] for i in range(8)]
    PSb = [PSALLb[:, i * 1024:(i + 1) * 1024] for i in range(8)]

    S = Sched(nc, fscr[:])

    def psk(i):
        return ('ps', i)

    def cols(w, c0, n, kc):
        return (w[:, c0:c0 + n].rearrange("(k p) n -> p k n", p=128), kc, n)

    pass_units = []
    for u in range(24):
        pass_units.append(cols(w_ada, 256 * u, 256, 8))
    for u in range(14):
        pass_units.append(cols(w_in, 256 * u, 256, 8))
    for ng in range(4):
        pass_units.append(cols(w_in, 3584 + 256 * ng, 256, 8))
        pass_units.append(cols(w_in, 4608 + 256 * ng, 256, 8))
        pass_units.append(cols(w_br_attn, 256 * ng, 256, 8))
        pass_units.append(cols(w_br_pool, 256 * ng, 256, 4))
    for j in range(4):
        pass_units.append(cols(w_out, 256 * j, 256, 8))
    W = WRing(S, ring, pass_units + pass_units[24:])

    def cload(dst, src, key, q='sp', slow=False):
        S.dma(q, dst, src, (), [key], key, slow=slow)

    for j in range(2):
        S.dma('sp', sc32[:, :, j], cond[j, :].rearrange("(k p) -> p k", p=128), (), [('sc32', j)], ('sc32', j), slow=True)

    cload(ident[:], ident_d[:, :], 'ident', q='pool')
    cload(cosb[:], cos_d.rearrange("(t p) d -> p t d", p=128), 'cosb')
    cload(sinb[:], sin_d.rearrange("(t p) d -> p t d", p=128), 'sinb')
    cload(gq_b[:], q_norm_g[0:1, :].to_broadcast([128, 64]), 'gq_b')
    cload(gk_b[:], k_norm_g[0:1, :].to_broadcast([128, 64]), 'gk_b')
    cload(gsub_b[:], subln_g[0:1, :].to_broadcast([128, 128]), 'gsub_b')
    cload(rcb[:], rcb_d[0:1, :].to_broadcast([128, 64]), 'rcb')
    cload(lam_b[:], lam_in[0:1, :].to_broadcast([128, 256]), 'lam_b')
    cload(bT[:], b_ada[0, :].rearrange("(c p) -> p c", p=128), 'bT', slow=True)
    cload(n1gT[:], norm1_g[0, :].rearrange("(c p) -> p c", p=128), 'n1gT', slow=True)
    cload(n2gT[:], norm2_g[0, :].rearrange("(c p) -> p c", p=128), 'n2gT', slow=True)
    cload(bgT[:], b_gate[0, :].rearrange("(c p) -> p c", p=128), 'bgT', slow=True)
    cload(pscT[:], pool_scale[0, :].rearrange("(c p) -> p c", p=128), 'pscT', slow=True)
    cload(rb_b[:], router_b[0:1, :].to_broadcast([128, 20]), 'rb_b')
    cload(poolw[:], pool_w.rearrange("g c e -> c g e"), 'poolw', q='pool')
    cload(rw[:], router_w.rearrange("(k p) n -> p k n", p=128), 'rw', q='pool')

    cload(trib[:], tri_d[:, :], 'trib', q='pool')
    cload(thr[:], thr_d[0:1, :].to_broadcast([128, 16]), 'thr')
    cload(tval[:], tval_d[0:1, :].to_broadcast([128, 48]), 'tval')
    cload(rtab[:], rtab_d[0:1, :].to_broadcast([128, 64]), 'rtab')
    S.memset('dve', onesb[:], 1.0, ['onesb'])
    S.add('pool', lambda e: e.iota(tok16[:], [[128, 16], [0, 16]], base=0, channel_multiplier=1), (), ['tok16'])
    S.add('pool', lambda e: e.iota(gidx[:, 0:1], [[0, 1]], base=0, channel_multiplier=2), (), ['p2i'])
    S.cp('dve', p2col[:], gidx[:, 0:1], ['p2i'], ['p2col'])
    S.ts('dve', gq_b[:], gq_b[:], 8.0 * 0.125, None, ALU.mult, None, ['gq_b'], ['gq_b'])
    S.ts('dve', gk_b[:], gk_b[:], 8.0, None, ALU.mult, None, ['gk_b'], ['gk_b'])
    S.ts('dve', gsub_b[:], gsub_b[:], (1.0 - LAMBDA_INIT) * (128.0 ** 0.5), None, ALU.mult, None, ['gsub_b'], ['gsub_b'])
    lb = lam_b[:].rearrange("p (a b d) -> p a b d", a=2, b=2)
    lt = lam_t[:].rearrange("p (a d) -> p a d", a=2)
    S.tt('dve', lt, lb[:, :, 0, :], lb[:, :, 1, :], ALU.mult, ['lam_b'], ['lam_t'])
    S.red(lam_s[:, 0:2], lt, ALU.add, ['lam_t'], ['lam_s'])
    S.act(lam_s[:, 2:4], lam_s[:, 0:2], AF.Exp, ['lam_s'], ['lam_s2'])
    S.tt('dve', lam_s[:, 4:5], lam_s[:, 2:3], lam_s[:, 3:4], ALU.subtract, ['lam_s2'], ['lam_s3'])
    S.ts('dve', lam_s[:, 5:6], lam_s[:, 4:5], LAMBDA_INIT, -1.0, ALU.add, ALU.mult, ['lam_s3'], ['neg_lam'])
    neg_lam = lam_s[:, 5:6]

    def dbg(name, src_ap, reads):
        if name in dumps:
            S.dma('pool', dumps[name], src_ap, reads, [], 'dbg_' + name)

    def norm_T(p, src, aT, shT, hkey):
        for blk in range(2):
            banks = [4 * blk + i for i in range(4)]
            for t in range(4):
                tile = blk * 4 + t
                sl = tile % 2
                if src == 'x':
                    xs = xst[sl][:]
                    xkey = ('xst', sl)
                    S.dma('sp', xs, x[p * 1024 + tile * 128: p * 1024 + (tile + 1) * 128, :], (), [xkey], xkey)
                else:
                    xs = x1[:, tile, :]
                    xkey = ('x1', tile)
                S.act(xn[sl][:], xs, AF.Square, [xkey], [('xn', sl), ('ss', tile)], accum_out=ss[:, tile:tile + 1])
                S.act(rstd[:, tile:tile + 1], ss[:, tile:tile + 1], AF.Ln, [('ss', tile)], [('rstd', tile)],
                      bias=EPS, scale=1.0 / 1024.0)
                S.act(rstd[:, tile:tile + 1], rstd[:, tile:tile + 1], AF.Exp, [('rstd', tile)], [('rstd', tile)], scale=-0.5)
                S.act(xn[sl][:], xs, AF.Copy, [xkey, ('rstd', tile)], [('xn', sl)], scale=rstd[:, tile:tile + 1])
                if src == 'x1':
                    tmp = PL[:, sl * 1024:(sl + 1) * 1024]
                    hb = PL[:, 2048 + sl * 512: 2560 + sl * 512].bitcast(BF16)
                    S.tt('pool', tmp, xs, g1b[:], ALU.mult, [xkey, 'g1b'], [('pl', 'h2t', sl)])
                    S.stt(hb, tmp, rstd[:, tile:tile + 1], xst[0][:], ALU.mult, ALU.add,
                          [('pl', 'h2t', sl), ('rstd', tile), ('xst', 0)], [('pl', 'h2b', sl)])
                    S.dma('sp', h2d[p * 1024 + tile * 128: p * 1024 + (tile + 1) * 128, :], hb, [('pl', 'h2b', sl)],
                          [('h2d', p, tile)], ('h2o', sl))
                    S.dma('sp', x1d[p * 1024 + tile * 128: p * 1024 + (tile + 1) * 128, :], xs, [xkey],
                          [('x1d', p, tile)], 'x1o')
                for c in range(8):
                    bv = PSb[banks[c // 2]].rearrange("p (c t) -> p c t", c=2)
                    S.tr(bv[:, c % 2, t * 128:(t + 1) * 128], xn[sl][:, c * 128:(c + 1) * 128], ident[:],
                         [('xn', sl), 'ident'], [psk(banks[c // 2])])
            for c in range(8):
                bv = PSb[banks[c // 2]].rearrange("p (c t) -> p c t", c=2)
                dst = hT[:, c, blk * 512:(blk + 1) * 512]
                if c % 2 == 0:
                    S.act(dst, bv[:, c % 2, :], AF.Identity, [psk(banks[c // 2]), aT[1], shT[1]], [(hkey, blk)],
                          bias=shT[0][:, c:c + 1], scale=aT[0][:, c:c + 1])
                else:
                    S.ts('dve', dst, bv[:, c % 2, :], aT[0][:, c:c + 1], shT[0][:, c:c + 1], ALU.mult, ALU.add,
                         [psk(banks[c // 2]), aT[1], shT[1]], [(hkey, blk)])

    def run_pass(p):
        nseq, L = (4, 256) if p == 0 else (1, 1024)
        nkt = 8 if p == 0 else 12
        S.fence()
        if p == 0:
            S.act(scT2[:], sc32[:], AF.Silu, [('sc32', 0), ('sc32', 1)], ['scT'])
            S.cp('dve', screp[:], scT2[:, :, 0:1].to_broadcast([128, 8, 128]), ['scT'], ['screp'])
            S.cp('dve', screp1[:], scT2[:, :, 1:2].to_broadcast([128, 8, 128]), ['scT'], ['screp1'])
            S.dma('sp', g1b[:], b_ada[0:1, 2048:3072].to_broadcast([128, 1024]), (), ['g1b'], 'g1b')
            S.dma('sp', g2b[:], b_ada[0:1, 5120:6144].to_broadcast([128, 1024]), (), ['g2b'], 'g2b')
            mod2 = PS[0][:, 0:96].rearrange("p (c j) -> p c j", j=2)
            for u in range(24):
                wv, wk, wj = W.acquire()
                if u in (8, 9, 10, 11, 20, 21, 22, 23):
                    gb, gkey, base = (g1b, 'g1b', 8) if u < 12 else (g2b, 'g2b', 20)
                    gi_ = 0 if u < 12 else 1
                    bank = 1 + (u % 2)
                    for kc in range(8):
                        S.mm(PS[bank][:, 0:256], screp[:, kc, :], wv[:, kc, :], kc == 0, kc == 7,
                             ['screp', wk], [psk(bank)])
                    c0 = (u - base) * 256
                    S.tt('dve', gb[:, c0:c0 + 256], PS[bank][:, 0:256], gb[:, c0:c0 + 256], ALU.add,
                         [psk(bank), gkey], [gkey])
                    bank2 = 3 + (u % 2)
                    for kc in range(8):
                        S.mm(PS[bank2][:, 0:256], screp1[:, kc, :], wv[:, kc, :], kc == 0, kc == 7,
                             ['screp1', wk], [psk(bank2)])
                    gst = PL[:, (u % 4) * 256:(u % 4 + 1) * 256]
                    S.cp('act', gst, PS[bank2][:, 0:256], [psk(bank2)], [('pl', 'gst', u % 4)])
                    S.dma('sp', gps[gi_:gi_ + 1, c0:c0 + 256], gst[0:1, :], [('pl', 'gst', u % 4)], [('gps', gi_, u)], ('gpso', u % 4))
                else:
                    for cc in range(2):
                        ch = 2 * u + cc
                        for kc in range(8):
                            S.mm(mod2[:, ch, :], wv[:, kc, cc * 128:(cc + 1) * 128], scT2[:, kc, :], kc == 0, kc == 7,
                                 ['scT', wk], [psk(0)])
                W.release(wj)
            bT3 = bT[:].rearrange("p (c o) -> p c o", o=1)
            S.tt('dve', modT2[:, 0:16, :], mod2[:, 0:16, :], bT3[:, 0:16, :].to_broadcast([128, 16, 2]), ALU.add, [psk(0), 'bT'], ['modT'])
            S.tt('dve', modT2[:, 24:40, :], mod2[:, 24:40, :], bT3[:, 24:40, :].to_broadcast([128, 16, 2]), ALU.add, [psk(0), 'bT'], ['modT'])
            for j in range(2):
                S.stt(a1T2[:, :, j], modT2[:, 8:16, j], 1.0, n1gT[:], ALU.add, ALU.mult, ['modT', 'n1gT'], ['a1T'])
                S.stt(a2T2[:, :, j], modT2[:, 32:40, j], 1.0, n2gT[:], ALU.add, ALU.mult, ['modT', 'n2gT'], ['a2T'])
        else:
            gkeys1 = [('gps', 0, u) for u in (8, 9, 10, 11)]
            gkeys2 = [('gps', 1, u) for u in (20, 21, 22, 23)]
            S.dma('sp', g1b[:], b_ada[0:1, 2048:3072].to_broadcast([128, 1024]), (), ['g1b'], 'g1b')
            S.dma('sp', g2b[:], b_ada[0:1, 5120:6144].to_broadcast([128, 1024]), (), ['g2b'], 'g2b')
            S.dma('sp', xst[0][:], gps[0:1, :].to_broadcast([128, 1024]), gkeys1, [('xst', 0)], ('xst', 0))
            S.dma('sp', xst[1][:], gps[1:2, :].to_broadcast([128, 1024]), gkeys2, [('xst', 1)], ('xst', 1))
            S.tt('dve', g1b[:], g1b[:], xst[0][:], ALU.add, ['g1b', ('xst', 0)], ['g1b'])
            S.tt('dve', g2b[:], g2b[:], xst[1][:], ALU.add, ['g2b', ('xst', 1)], ['g2b'])
        a1T = a1T2[:, :, p]
        a2T = a2T2[:, :, p]
        sh1 = (modT2[:, 0:8, p], 'modT')
        sh2 = (modT2[:, 24:32, p], 'modT')

        norm_T(p, 'x', (a1T, 'a1T'), sh1, 'hT')
        if p == 0:
            for j in range(2):
                S.dma('sp', modrows[j, 0, :].rearrange("(c p) -> p c", p=128), a2T2[:, :, j], ['a2T'], [('modrows', j)], 'mro', slow=True)
                S.dma('sp', modrows[j, 1, :].rearrange("(c p) -> p c", p=128), modT2[:, 24:32, j], ['modT'], [('modrows', j)], 'mro', slow=True)
        S.dma('sp', g2row[p:p + 1, :], g2b[0:1, :], ['g2b'], [('g2row', p)], 'g2o')
        if p == 0:
            dbg('hT', hT[:], [('hT', 0), ('hT', 1)])

        sqs = [mrg_f[:, 0:512], mrg_f[:, 3584:4096]]
        zn = mrg_f[:, 512:1024]
        zg = [mrg_f[:, 1024 + i * 512: 1536 + i * 512] for i in range(2)]
        vst = [mrg_f[:, 2048 + i * 512: 2560 + i * 512] for i in range(2)]
        qkb = [mrg_f[:, 3072 + i * 256: 3328 + i * 256].bitcast(BF16) for i in range(2)]
        zns = [zn, vst[0]]
        if p == 1:
            rtab = {}
            for ti, (gb_, gkey) in enumerate(((gq_b, 'gq_b'), (gk_b, 'gk_b'))):
                Cg = PL[:, ti * 1024: ti * 1024 + 512].rearrange("p (t d) -> p t d", t=8)
                Sg = PL[:, ti * 1024 + 512: ti * 1024 + 1024].rearrange("p (t d) -> p t d", t=8)
                S.tt('pool', Cg, cosb[:], gb_[:].rearrange("p (o d) -> p o d", o=1).to_broadcast([128, 8, 64]), ALU.mult,
                     ['cosb', gkey], [('pl', 'Cg', ti)])
                g4 = gb_[:].rearrange("p (a s d) -> p a s d", a=2, s=2)
                for sidx in range(2):
                    for a_ in range(2):
                        S.tt('pool', Sg[:, :, a_ * 32 + sidx * 16: a_ * 32 + sidx * 16 + 16],
                             sinb[:, :, a_ * 32 + sidx * 16: a_ * 32 + sidx * 16 + 16],
                             g4[:, a_, 1 - sidx, :].rearrange("p (o d) -> p o d", o=1).to_broadcast([128, 8, 16]), ALU.mult,
                             ['sinb', gkey], [('pl', 'Sg', ti)])
                rtab[ti] = (Cg, Sg)
        S.memset('dve', vtok[:, :, :, 128:129], 1.0, [('vt', 'ones')])
        if p == 1:
            for jt in range(4):
                S.dma('pool', vtok[:, 8 + jt, :, 0:128],
                      cv[jt * 128:(jt + 1) * 128, :].rearrange("p (h e) -> p h e", h=8), (), [('vt', 8 + jt)], ('vtc', jt))
                sl = jt % 2
                S.dma('pool', xn[sl][:], ck[jt * 128:(jt + 1) * 128, :], (), [('xn', sl)], ('xnc', sl))
                bv = PSb[7].rearrange("p (h t) -> p h t", h=8)
                for h in range(8):
                    S.tr(bv[:, h, :], xn[sl][:, h * 128:(h + 1) * 128], ident[:], [('xn', sl), 'ident'], [psk(7)])
                S.cp('act', kT[:, :, 1024 + jt * 128: 1024 + (jt + 1) * 128], bv, [psk(7)], [('kT', 8 + jt)])
        qk_units = {}

        def qk_M(n):
            ci, tile = divmod(n, 8)
            if tile == 0:
                qk_units[ci] = (W.acquire(), W.acquire())
            (ua, uak, uaj), (ub, ubk, ubj) = qk_units[ci]
            bank = n % 4
            for half, (u_, uk_) in enumerate(((ua, uak), (ub, ubk))):
                for kc in range(8):
                    S.mm(PS[bank][:, half * 256:(half + 1) * 256], hT[:, kc, tile * 128:(tile + 1) * 128],
                         u_[:, kc, :], kc == 0, kc == 7, [('hT', tile // 4), uk_], [psk(bank)])
            if tile == 7:
                W.release(uaj)
                W.release(ubj)

        def qk_E1(n):
            bank = n % 4
            b2 = n % 2
            zps = PS[bank][:, 0:512]
            sqb = sqs[b2]
            S.act(sqb, zps, AF.Square, [psk(bank)], [('st2', 'sq', b2)])
            S.red(ss8[:, b2, :], sqb.rearrange("p (g d) -> p g d", g=8), ALU.add, [('st2', 'sq', b2)], [('ss8', b2)])
            S.act(rs8[:, b2, :, 0], ss8[:, b2, :], AF.Ln, [('ss8', b2)], [('rs8', b2)], bias=64.0 * EPS)
            S.act(rs8[:, b2, :, 0], rs8[:, b2, :, 0], AF.Exp, [('rs8', b2)], [('rs8', b2)], scale=-0.5)

        def qk_E2(n):
            ci, tile = divmod(n, 8)
            isq = ci < 2
            hc = ci % 2
            gb_, gkey = (gq_b, 'gq_b') if isq else (gk_b, 'gk_b')
            bank = n % 4
            s2 = n % 2
            b2 = n % 2
            zps = PS[bank][:, 0:512]
            sqb = sqs[b2]
            znb = zns[b2] if p == 1 else zn
            znk = ('st2', 'zn', b2) if p == 1 else ('st2', 'zn')
            S.tt('dve', znb.rearrange("p (g d) -> p g d", g=8), zps.rearrange("p (g d) -> p g d", g=8),
                 rs8[:, b2, :, :].to_broadcast([128, 8, 64]), ALU.mult, [psk(bank), ('rs8', b2)], [znk])
            gbb = gb_[:].rearrange("p (o d) -> p o d", o=1).to_broadcast([128, 8, 64])
            if p == 0:
                if isq:
                    S.tt('pool', qkb[s2].rearrange("p (g d) -> p g d", g=8), zn.rearrange("p (g d) -> p g d", g=8),
                         gbb, ALU.mult, [znk, gkey], [('st2', 'qkb', s2)])
                else:
                    S.tt('pool', zg[s2].rearrange("p (g d) -> p g d", g=8), zn.rearrange("p (g d) -> p g d", g=8),
                         gbb, ALU.mult, [znk, gkey], [('st2', 'zg', s2)])
                    S.dma('sp', nk[tile * 128:(tile + 1) * 128, hc * 512:(hc + 1) * 512], zg[s2],
                          [('st2', 'zg', s2)], [], ('nk', s2))
                    S.cp('act', qkb[s2], zg[s2], [('st2', 'zg', s2)], [('st2', 'qkb', s2)])
            else:
                ti = 0 if isq else 1
                Cg, Sg = rtab[ti]
                t1 = zg[s2]
                S.tt('dve', t1.rearrange("p (g d) -> p g d", g=8), znb.rearrange("p (g d) -> p g d", g=8),
                     Cg[:, tile:tile + 1, :].to_broadcast([128, 8, 64]), ALU.mult, [znk, ('pl', 'Cg', ti)], [('st2', 'zg', s2)])
                zz = znb.rearrange("p (g a s d) -> p g a s d", g=8, a=2, s=2)
                t2 = vst[1]
                qq = t2.rearrange("p (g a s d) -> p g a s d", g=8, a=2, s=2)
                sg4 = Sg[:, tile:tile + 1, :].rearrange("p o (a s d) -> p o a s d", a=2, s=2)
                for sidx in range(2):
                    sn = sg4[:, :, :, sidx, :].to_broadcast([128, 8, 2, 16])
                    S.tt('pool', qq[:, :, :, sidx, :], zz[:, :, :, 1 - sidx, :], sn, ALU.mult,
                         [znk, ('pl', 'Sg', ti)], [('st2', 't2', sidx)])
                S.tt('dve', qkb[s2], t1, t2, ALU.add, [('st2', 'zg', s2), ('st2', 't2', 0), ('st2', 't2', 1)], [('st2', 'qkb', s2)])

        def qk_T(n):
            ci, tile = divmod(n, 8)
            isq = ci < 2
            hc = ci % 2
            s2 = n % 2
            bk = 6 + (n % 2)
            bv = PSb[bk].rearrange("p (h t) -> p h t", h=8)
            for hh in range(4):
                S.tr(bv[:, hh, :], qkb[s2][:, hh * 128:(hh + 1) * 128], ident[:], [('st2', 'qkb', s2), 'ident'], [psk(bk)])
            if isq:
                S.cp('act', qT[:, 4 * hc:4 * hc + 4, tile * 128:(tile + 1) * 128], bv[:, 0:4, :], [psk(bk)], [('qT', tile // 2)])
            else:
                S.cp('act', kT[:, 4 * hc:4 * hc + 4, tile * 128:(tile + 1) * 128], bv[:, 0:4, :], [psk(bk)], [('kT', tile)])

        NQK = 32
        for s_ in range(NQK + 3):
            if s_ < NQK:
                qk_M(s_)
            if 0 <= s_ - 1 < NQK:
                qk_E1(s_ - 1)
            if 0 <= s_ - 2 < NQK:
                qk_E2(s_ - 2)
            if 0 <= s_ - 3 < NQK:
                qk_T(s_ - 3)
        for ci in range(2):
            ua, uak, uaj = W.acquire()
            ub, ubk, ubj = W.acquire()
            for tile in range(8):
                bank = tile % 4
                for half, (u_, uk_) in enumerate(((ua, uak), (ub, ubk))):
                    for kc in range(8):
                        S.mm(PS[bank][:, half * 256:(half + 1) * 256], hT[:, kc, tile * 128:(tile + 1) * 128],
                             u_[:, kc, :], kc == 0, kc == 7, [('hT', tile // 4), uk_], [psk(bank)])
                zps = PS[bank][:, 0:512]
                S.cp('act', vtok[:, tile, 4 * ci:4 * ci + 4, 0:128], zps.rearrange("p (h e) -> p h e", h=4),
                     [psk(bank)], [('vt', tile)])
                if p == 0:
                    s2 = tile % 2
                    S.cp('dve', vst[s2], zps, [psk(bank)], [('st2', 'vst', s2)])
                    S.dma('sp', nv[tile * 128:(tile + 1) * 128, ci * 512:(ci + 1) * 512], vst[s2],
                          [('st2', 'vst', s2)], [], ('nv', s2))
            W.release(uaj)
            W.release(ubj)
        S.fence(('PL',))
        Wd = L + 16
        Pb = PL[:, 0:1088]
        Ab = PL[:, 1088:2176]
        Bb = PL[:, 2176:3264]
        pooled = PL[:, 3264:3776].bitcast(BF16)
        tmpb = PL[:, 3776:3776 + 32]
        S.memset('dve', Pb, 0.0, [('pl', 'P')])

        def v3(buf):
            return buf[:, 0:nseq * Wd].rearrange("p (s l) -> p s l", s=nseq)

        def rg(buf, a, b):
            return v3(buf)[:, :, 8 + a: 8 + b]

        for half in range(2):
            up, upk, upj = W.acquire()
            for gg in range(2):
                g = 2 * half + gg
                w_ = (2, 4, 8, 16)[g]
                hw = w_ // 2
                for blk in range(2):
                    bank = 4 + blk
                    for kc in range(8):
                        S.mm(PS[bank][:, 0:512], up[:, kc, gg * 128:(gg + 1) * 128], hT[:, kc, blk * 512:(blk + 1) * 512],
                             kc == 0, kc == 7, [('hT', blk), upk], [psk(bank)])
                    if p == 0:
                        S.cp('act', v3(Pb)[:, 2 * blk:2 * blk + 2, 8:8 + 256], PS[bank][:, 0:512].rearrange("p (s l) -> p s l", s=2),
                             [psk(bank)], [('pl', 'P')])
                    else:
                        S.cp('act', Pb[:, 8 + 512 * blk: 8 + 512 * (blk + 1)], PS[bank][:, 0:512], [psk(bank)], [('pl', 'P')])
                pk, ak, bk_ = ('pl', 'P'), ('pl', 'A'), ('pl', 'B')
                S.tt('dve', rg(Ab, -7, L + 8), rg(Pb, -8, L + 7), rg(Pb, -7, L + 8), ALU.add, [pk], [ak])
                src_, sk = Ab, ak
                if g >= 1:
                    S.tt('dve', rg(Bb, -6, L + 7), rg(Ab, -7, L + 6), rg(Ab, -5, L + 8), ALU.add, [ak], [bk_])
                    src_, sk = Bb, bk_
                if g >= 2:
                    S.tt('dve', rg(Ab, -4, L + 5), rg(Bb, -6, L + 3), rg(Bb, -2, L + 7), ALU.add, [bk_], [ak])
                    src_, sk = Ab, ak
                if g >= 3:
                    S.tt('dve', rg(Bb, 0, L), rg(Ab, -4, L - 4), rg(Ab, 4, L + 4), ALU.add, [ak], [bk_])
                    src_, sk = Bb, bk_
                pl3 = pooled.rearrange("p (s l) -> p s l", s=nseq)
                S.stt(pl3, rg(src_, 0, L), 1.0 / w_, rg(Pb, 0, L), ALU.mult, ALU.subtract, [sk, pk], [('pl', 'pooled')])
                tb = tmpb[:, 0:nseq * hw].rearrange("p (s l) -> p s l", s=nseq)
                for side in range(2):
                    lo, hi = (0, hw) if side == 0 else (L - hw, L)
                    rcv = rcb[:, g * 16 + side * 8: g * 16 + side * 8 + hw].rearrange("p (o l) -> p o l", o=1).to_broadcast([128, nseq, hw])
                    S.tt('dve', tb, rg(src_, lo, hi), rcv, ALU.mult, [sk, 'rcb'], [('pl', 'tmpb')])
                    S.tt('dve', pl3[:, :, lo:hi], tb, rg(Pb, lo, hi), ALU.subtract, [('pl', 'tmpb'), pk], [('pl', 'pooled')])
                for blk in range(2):
                    bank = 6 + blk
                    S.mm(PS[bank][:, 0:512], poolw[:, g, :], pooled[:, blk * 512:(blk + 1) * 512], True, True,
                         [('pl', 'pooled'), 'poolw'], [psk(bank)])
                    S.act(mixT[:, g, blk * 512:(blk + 1) * 512], PS[bank][:, 0:512], AF.Copy, [psk(bank), 'pscT'],
                          [('mixT', blk)], scale=pscT[:, g:g + 1])
            W.release(upj)
        if p == 0:
            dbg('qT', qT, [('qT', i) for i in range(4)])
            dbg('kT', kT, [('kT', i) for i in range(8)])
            dbg('mixT', mixT[:], [('mixT', 0), ('mixT', 1)])

        S.fence(('ACC', 'PL'))
        PT = [PLb[:, i * 512:(i + 1) * 512] for i in range(3)]
        sqo = PL[:, 768:1792]
        tO2 = [PL[:, 1792 + i * 128: 1920 + i * 128] for i in range(4)]
        onb = [PL[:, 2304 + i * 512: 2816 + i * 512].bitcast(BF16) for i in range(2)]
        ost = [[mrg_f[:, (a * 2 + b) * 1024:(a * 2 + b + 1) * 1024].rearrange("p (h e) -> p h e", h=8) for b in range(2)]
               for a in range(2)]
        items = []
        for qb in range(4):
            keytiles = [2 * qb, 2 * qb + 1] if p == 0 else list(range(12))
            for h in range(8):
                for jn, j in enumerate(keytiles):
                    items.append((qb, h, jn, j, len(keytiles)))
        pending_T = []
        o2c = [0]

        def at_A(n):
            qb, h, jn, j, nk_ = items[n]
            sbk = n % 3
            sp_ = n % 2
            for i in range(2):
                S.mm(PS[2 * sp_ + i][:, 0:256], kT[i * 64:(i + 1) * 64, h, j * 128:(j + 1) * 128],
                     qT[i * 64:(i + 1) * 64, h, qb * 256:(qb + 1) * 256], True, True,
                     [('kT', j), ('qT', qb)], [psk(2 * sp_ + i)])
            S.act(PT[sbk].rearrange("p (i q) -> p i q", i=2),
                  PSALL[:, 2 * sp_ * 512:(2 * sp_ + 2) * 512].rearrange("p (i q) -> p i q", i=2)[:, :, 0:256],
                  AF.Exp, [psk(2 * sp_), psk(2 * sp_ + 1)], [('pl', 'PT', sbk)])

        def subln(qb):
            for qt in range(2):
                o = ost[qb % 2][qt]
                ok_ = ('ost', qb % 2, qt)
                tile = qb * 2 + qt
                S.tt('dve', sqo.rearrange("p (h e) -> p h e", h=8), o, o, ALU.mult, [ok_], [('pl', 'sqo')])
                S.red(ss8c[:], sqo.rearrange("p (h e) -> p h e", h=8), ALU.add, [('pl', 'sqo')], ['ss8b'])
                S.act(rs8c[:, :, 0], ss8c[:], AF.Ln, ['ss8b'], ['rs8b'], bias=128.0 * EPS)
                S.act(rs8c[:, :, 0], rs8c[:, :, 0], AF.Exp, ['rs8b'], ['rs8b'], scale=-0.5)
                S.tt('pool', o, o, rs8c[:].to_broadcast([128, 8, 128]), ALU.mult, [ok_, 'rs8b'], [ok_])
                ob = onb[qt].rearrange("p (h e) -> p h e", h=8)
                S.tt('pool', ob, o, gsub_b[:].rearrange("p (o e) -> p o e", o=1).to_broadcast([128, 8, 128]), ALU.mult,
                     [ok_, 'gsub_b'], [('pl', 'onb', qt)])

                def T_on(qb=qb, qt=qt, tile=tile):
                    bv = PSb[0].rearrange("p (h t) -> p h t", h=8)
                    for h in range(8):
                        S.tr(bv[:, h, :], onb[qt][:, h * 128:(h + 1) * 128], ident[:], [('pl', 'onb', qt), 'ident'], [psk(0)])
                    S.cp('act', qT[:, :, tile * 128:(tile + 1) * 128], bv, [psk(0)], [('qT', qb)])
                pending_T.append(T_on)

        def at_B(n):
            qb, h, jn, j, nk_ = items[n]
            sbk = n % 3
            ab = [4 + 2 * (h % 2), 5 + 2 * (h % 2)]
            if h == 2 and jn == 0:
                while pending_T:
                    pending_T.pop(0)()
            for qt in range(2):
                for i in range(2):
                    S.mm(PS[ab[qt]][:, i * 132:i * 132 + 129], PT[sbk][:, i * 256 + qt * 128: i * 256 + (qt + 1) * 128],
                         vtok[:, j, h, 0:129], jn == 0 and i == 0, jn == nk_ - 1,
                         [('pl', 'PT', sbk), ('vt', j), ('vt', 'ones')], [psk(ab[qt])], skip=True)
        def at_C(n):
            qb, h, jn, j, nk_ = items[n]
            ab = [4 + 2 * (h % 2), 5 + 2 * (h % 2)]
            if jn == nk_ - 1:
                for qt in range(2):
                    av = PS[ab[qt]][:, 0:264].rearrange("p (i e) -> p i e", i=2)
                    sl4 = o2c[0] % 4
                    o2c[0] += 1
                    S.recip(rz[:, sl4, :, :], av[:, :, 128:129], [psk(ab[qt])], [('rz', sl4)])
                    S.ts('dve', r2n[:, sl4, :], rz[:, sl4, 1, :], neg_lam, None, ALU.mult, None, [('rz', sl4), 'neg_lam'], [('r2n', sl4)])
                    S.act(tO2[sl4], av[:, 1, 0:128], AF.Copy, [psk(ab[qt]), ('r2n', sl4)], [('pl', 'tO2', sl4)], scale=r2n[:, sl4, :])
                    S.stt(ost[qb % 2][qt][:, h, :], av[:, 0, 0:128], rz[:, sl4, 0, :], tO2[sl4], ALU.mult, ALU.add,
                          [psk(ab[qt]), ('rz', sl4), ('pl', 'tO2', sl4)], [('ost', qb % 2, qt)])
                if h == 7:
                    subln(qb)

        NI = len(items)
        CL = 2 if p == 0 else 3
        for s_ in range(NI + CL):
            if s_ < NI:
                at_A(s_)
            if 0 <= s_ - 1 < NI:
                at_B(s_ - 1)
            if 0 <= s_ - CL < NI:
                at_C(s_ - CL)
        while pending_T:
            pending_T.pop(0)()
        if p == 0:
            dbg('onT', qT, [('qT', i) for i in range(4)])

        S.fence(('ACC', 'PL'))
        sg0 = [PLb[:, i * 512:(i + 1) * 512] for i in range(2)]
        sg1 = [PLb[:, 1024 + i * 512: 1536 + i * 512] for i in range(2)]
        t0 = PL[:, 1024:1536]
        t1 = PL[:, 1536:2048]
        wtmp = [PL[:, 2048 + i * 512: 2560 + i * 512] for i in range(2)]
        it = 0
        for ng in range(4):
            ug0, ug0k, ug0j = W.acquire()
            ug1, ug1k, ug1j = W.acquire()
            ua, uak, uaj = W.acquire()
            up, upk, upj = W.acquire()
            for blk in range(2):
                tsl = slice(blk * 512, (blk + 1) * 512)
                for cc in range(2):
                    c = 2 * ng + cc
                    b0 = (it % 2) * 4
                    s2 = it % 2
                    it += 1
                    csl = slice(cc * 128, (cc + 1) * 128)
                    for kc in range(8):
                        S.mm(PS[b0][:, 0:512], ug0[:, kc, csl], hT[:, kc, tsl], kc == 0, kc == 7, [('hT', blk), ug0k], [psk(b0)])
                    for kc in range(8):
                        S.mm(PS[b0 + 1][:, 0:512], ug1[:, kc, csl], hT[:, kc, tsl], kc == 0, kc == 7, [('hT', blk), ug1k], [psk(b0 + 1)])
                    for kc in range(8):
                        S.mm(PS[b0 + 2][:, 0:512], ua[:, kc, csl], qT[:, kc, tsl], kc == 0, kc == 7,
                             [('qT', 2 * blk), ('qT', 2 * blk + 1), uak], [psk(b0 + 2)])
                    for fc in range(4):
                        S.mm(PS[b0 + 3][:, 0:512], up[:, fc, csl], mixT[:, fc, tsl], fc == 0, fc == 3, [('mixT', blk), upk], [psk(b0 + 3)])
                    S.act(sg0[s2], PS[b0][:, 0:512], AF.Sigmoid, [psk(b0), 'bgT'], [('pl', 'sg0', s2)], bias=bgT[:, c:c + 1])
                    S.act(sg1[s2], PS[b0 + 1][:, 0:512], AF.Sigmoid, [psk(b0 + 1), 'bgT'], [('pl', 'sg1', s2)], bias=bgT[:, 8 + c:9 + c])
                    S.tt('dve', t0, PS[b0 + 2][:, 0:512], sg0[s2], ALU.mult, [psk(b0 + 2), ('pl', 'sg0', s2)], [('pl', 't0')])
                    S.tt('dve', t1, PS[b0 + 3][:, 0:512], sg1[s2], ALU.mult, [psk(b0 + 3), ('pl', 'sg1', s2)], [('pl', 't1')])
                    S.tt('pool', mrgT[:, c, tsl], t0, t1, ALU.add, [('pl', 't0'), ('pl', 't1')], [('mrgT', blk)])
            for j_ in (ug0j, ug1j, uaj, upj):
                W.release(j_)
        if p == 0:
            dbg('mrgT', mrgT, [('mrgT', 0), ('mrgT', 1)])
        S.fence(('X1',))
        uo = [W.acquire() for _ in range(4)]
        it = 0
        for tile in range(8):
            sl = tile % 2
            xkey = ('xst', sl)
            S.dma('sp', xst[sl][:], x[p * 1024 + tile * 128: p * 1024 + (tile + 1) * 128, :], (), [xkey], xkey)
            for nh in range(2):
                bank = it % 4
                s2 = it % 2
                it += 1
                for j2 in range(2):
                    u_, uk_, _ = uo[nh * 2 + j2]
                    for kc in range(8):
                        S.mm(PS[bank][:, j2 * 256:(j2 + 1) * 256], mrgT[:, kc, tile * 128:(tile + 1) * 128], u_[:, kc, :],
                             kc == 0, kc == 7, [('mrgT', tile // 4), uk_], [psk(bank)])
                nsl = slice(nh * 512, (nh + 1) * 512)
                S.tt('dve', wtmp[s2], PS[bank][:, 0:512], g1b[:, nsl], ALU.mult, [psk(bank), 'g1b'], [('pl', 'wtmp', s2)])
                S.tt('pool', x1[:, tile, nsl], wtmp[s2], xst[sl][:, nsl], ALU.add, [('pl', 'wtmp', s2), xkey], [('x1', tile)])
        for (_, _, j_) in uo:
            W.release(j_)
        if p == 0:
            dbg('x1', x1[:, 0, :], [('x1', 0)])

        S.fence(('PL',))
        S.dma('sp', g1b[:], modrows[p, 0:1, :].to_broadcast([128, 1024]), [('modrows', p)], ['g1b'], 'g1b')
        S.dma('sp', xst[0][:], modrows[p, 1:2, :].to_broadcast([128, 1024]), [('modrows', p)], [('xst', 0)], ('xst', 0))
        norm_T(p, 'x1', (a2T, 'a2T'), sh2, 'hT')
        S.fence(('PL', 'ACC', 'K'))
        lgp = PS[7][:, 0:256].rearrange("p (t n) -> p t n", t=8)
        for tile in range(8):
            for kc in range(8):
                S.mm(lgp[:, tile, 0:20], hT[:, kc, tile * 128:(tile + 1) * 128], rw[:, kc, :], kc == 0, kc == 7,
                     [('hT', tile // 4), 'rw'], [psk(7)])
        R_ = PL[:, 0:2560]

        def rbuf(i, n):
            return R_[:, i * 160: i * 160 + 8 * n].rearrange("p (t n) -> p t n", t=8)

        lg = rbuf(0, 20)
        S.tt('dve', lg, lgp[:, :, 0:20], rb_b[:].rearrange("p (o n) -> p o n", o=1).to_broadcast([128, 8, 20]), ALU.add,
             [psk(7), 'rb_b'], [('pl', 'lg')])
        gl = lg[:, :, 0:4]
        el = lg[:, :, 4:20]
        gmax = rbuf(1, 1)
        S.red(gmax[:, :, 0], gl, ALU.max, [('pl', 'lg')], [('pl', 'gmax')])
        ge = rbuf(2, 4)
        S.tt('dve', ge, gl, gmax.to_broadcast([128, 8, 4]), ALU.subtract, [('pl', 'lg'), ('pl', 'gmax')], [('pl', 'ge')])
        eg = rbuf(3, 4)
        S.act(eg, ge, AF.Exp, [('pl', 'ge')], [('pl', 'eg')])
        gsum = rbuf(4, 1)
        S.red(gsum[:, :, 0], eg, ALU.add, [('pl', 'eg')], [('pl', 'gsum')])
        gw = rbuf(5, 1)
        S.recip(gw, gsum, [('pl', 'gsum')], [('pl', 'gw')])
        pen = rbuf(6, 4)
        S.ts('dve', pen, ge, 0.0, None, ALU.is_ge, None, [('pl', 'ge')], [('pl', 'pen')])
        S.ts('dve', pen, pen, -1.0, BIG, ALU.add, ALU.mult, [('pl', 'pen')], [('pl', 'pen')])
        msk = rbuf(7, 16)
        pen4 = R_[:, 6 * 160: 6 * 160 + 32].rearrange("p (t g o) -> p t g o", t=8, o=1).to_broadcast([128, 8, 4, 4])
        S.tt('dve', msk.rearrange("p t (g e) -> p t g e", g=4), el.rearrange("p t (g e) -> p t g e", g=4), pen4, ALU.add,
             [('pl', 'lg'), ('pl', 'pen')], [('pl', 'msk')])
        m1 = rbuf(8, 1)
        S.red(m1[:, :, 0], msk, ALU.max, [('pl', 'msk')], [('pl', 'm1')])
        o1 = rbuf(9, 16)
        S.tt('dve', o1, msk, m1.to_broadcast([128, 8, 16]), ALU.subtract, [('pl', 'msk'), ('pl', 'm1')], [('pl', 'o1')])
        S.ts('dve', o1, o1, 0.0, None, ALU.is_ge, None, [('pl', 'o1')], [('pl', 'o1')])
        msk2 = rbuf(10, 16)
        S.stt(msk2, o1, -BIG, msk, ALU.mult, ALU.add, [('pl', 'o1'), ('pl', 'msk')], [('pl', 'msk2')])
        m2 = rbuf(11, 1)
        S.red(m2[:, :, 0], msk2, ALU.max, [('pl', 'msk2')], [('pl', 'm2')])
        o2 = rbuf(12, 16)
        S.tt('dve', o2, msk2, m2.to_broadcast([128, 8, 16]), ALU.subtract, [('pl', 'msk2'), ('pl', 'm2')], [('pl', 'o2')])
        S.ts('dve', o2, o2, 0.0, None, ALU.is_ge, None, [('pl', 'o2')], [('pl', 'o2')])
        e21 = rbuf(4, 1)
        S.tt('dve', e21, m2, m1, ALU.subtract, [('pl', 'm2'), ('pl', 'm1'), ('pl', 'gw')], [('pl', 'gsum')])
        S.act(e21, e21, AF.Exp, [('pl', 'gsum')], [('pl', 'gsum')])
        den = rbuf(1, 1)
        S.ts('dve', den, e21, 1.0, None, ALU.add, None, [('pl', 'gsum'), ('pl', 'ge')], [('pl', 'gmax')])
        S.recip(den, den, [('pl', 'gmax')], [('pl', 'gmax')])
        w1 = rbuf(2, 1)
        S.tt('dve', w1, den, gw, ALU.mult, [('pl', 'gmax'), ('pl', 'gw'), ('pl', 'pen'), ('pl', 'eg')], [('pl', 'ge')])
        w2 = rbuf(3, 1)
        S.tt('dve', w2, w1, e21, ALU.mult, [('pl', 'ge'), ('pl', 'gsum')], [('pl', 'eg')])
        tsl = slice(p * 8, (p + 1) * 8)
        S.cp('pool', O1g[:, tsl, :], o1, [('pl', 'o1')], [('O1g', p)])
        S.cp('pool', O2g[:, tsl, :], o2, [('pl', 'o2')], [('O2g', p)])
        S.tt('dve', Mb[:, tsl, :], o1, o2, ALU.add, [('pl', 'o1'), ('pl', 'o2')], [('Mb', p)])
        S.cp('dve', W1g[:, tsl, :], w1, [('pl', 'ge')], [('W1g', p)])
        S.cp('dve', W2g[:, tsl, :], w2, [('pl', 'eg')], [('W2g', p)])
        if p == 0:
            dbg('gates', O1g[:, 0, :], [('O1g', 0)])
            dbg('h2T', hT[:], [('hT', 0), ('hT', 1)])

    def routing():
        S.fence(('PL',))
        rankp = PS[0][:, 0:256].rearrange("p (t e) -> p t e", t=16)
        cntp = PS[1][:, 0:16]
        mk = [('Mb', 0), ('Mb', 1)]
        for T in range(16):
            S.mm(rankp[:, T, :], trib[:], Mb[:, T, :], True, T == 0, mk + ['trib'], [psk(0)])
            for T2 in range(T):
                S.mm(rankp[:, T, :], onesb[:], Mb[:, T2, :], False, T2 == T - 1, mk + ['onesb'], [psk(0)])
        for T in range(16):
            S.mm(cntp, onesb[:], Mb[:, T, :], T == 0, T == 15, mk + ['onesb'], [psk(1)])
        R_ = PL[:, 0:4096]
        off = [0]
        nbuf = [0]

        def ra(n):
            v = R_[:, off[0]:off[0] + n]
            k = ('pl', 'r', nbuf[0])
            off[0] += n
            nbuf[0] += 1
            assert off[0] <= 4096
            return v, k

        def b3(ap2, shape):
            return ap2.rearrange("p (o n) -> p o n", o=1).to_broadcast(shape)

        def l3(ap2, shape):
            return ap2.rearrange("p (n o) -> p n o", o=1).to_broadcast(shape)

        cnt, kcnt = ra(16)
        S.cp('dve', cnt, cntp, [psk(1)], [kcnt])
        cmp, kcmp = ra(256)
        cmp3 = cmp.rearrange("p (e k) -> p e k", e=16)
        S.tt('dve', cmp3, l3(cnt, [128, 16, 16]), b3(thr[:], [128, 16, 16]), ALU.is_gt, [kcnt, 'thr'], [kcmp])
        ntl, kntl = ra(16)
        S.red(ntl, cmp3, ALU.add, [kcmp], [kntl])
        thr2 = rtab[:, 0:13]
        eidx = rtab[:, 16:32]
        ocmp, kocmp = ra(208)
        ocmp3 = ocmp.rearrange("p (e q) -> p e q", e=16)
        S.tt('dve', ocmp3, l3(cnt, [128, 16, 13]), b3(thr2, [128, 16, 13]), ALU.is_gt, [kcnt, 'rtab'], [kocmp])
        ovt, kovt = ra(16)
        S.red(ovt, ocmp3, ALU.add, [kocmp], [kovt])
        ones16, kones16 = ra(16)
        S.memset('dve', ones16, 1.0, [kones16])
        ovincl, kovincl = ra(16)
        S.add('dve', lambda e: e.tensor_tensor_scan(ovincl, ones16, ovt, 0.0, ALU.mult, ALU.add), [kones16, kovt], [kovincl])
        ovb, kovb = ra(16)
        S.tt('dve', ovb, ovincl, ovt, ALU.subtract, [kovincl, kovt], [kovb])
        prod, kprod = ra(256)
        prod3 = prod.rearrange("p (t e) -> p t e", t=16)
        sf, ksf = ra(32)
        sf3 = sf.rearrange("p (k t) -> p k t", k=2)
        for k, (Og, okey) in enumerate(((O1g, 'O1g'), (O2g, 'O2g'))):
            ok2 = [(okey, 0), (okey, 1)]
            rk, krk = ra(16)
            S.tt('dve', prod3, Og[:], rankp, ALU.mult, ok2 + [psk(0)], [kprod])
            S.red(rk, prod3, ALU.add, [kprod], [krk])
            Ek, kE = ra(16)
            S.tt('dve', prod3, Og[:], b3(eidx, [128, 16, 16]), ALU.mult, ok2 + ['rtab'], [kprod])
            S.red(Ek, prod3, ALU.add, [kprod], [kE])
            OBk, kOB = ra(16)
            S.tt('dve', prod3, Og[:], b3(ovb, [128, 16, 16]), ALU.mult, ok2 + [kovb], [kprod])
            S.red(OBk, prod3, ALU.add, [kprod], [kOB])
            mainp, kmain = ra(16)
            S.stt(mainp, Ek, 384.0, rk, ALU.mult, ALU.add, [kE, krk], [kmain])
            tailp, ktail = ra(16)
            S.stt(tailp, OBk, 256.0, rk, ALU.mult, ALU.add, [kOB, krk], [ktail])
            S.ts('dve', tailp, tailp, float(NMAIN * 128 - 384), None, ALU.add, None, [ktail], [ktail])
            S.tt('dve', tailp, tailp, mainp, ALU.subtract, [ktail, kmain], [ktail])
            isov, kisov = ra(16)
            S.ts('dve', isov, rk, 384.0, None, ALU.is_ge, None, [krk], [kisov])
            S.tt('dve', tailp, tailp, isov, ALU.mult, [ktail, kisov], [ktail])
            S.tt('dve', sf3[:, k, :], mainp, tailp, ALU.add, [kmain, ktail], [(ksf, k)])
        S.cp('dve', s12i[:], sf3, [(ksf, 0), (ksf, 1)], ['s12i'])
        cmp2, kcmp2 = ra(28 * 16)
        cmp23 = cmp2.rearrange("p (t e) -> p t e", t=28)
        S.tt('dve', cmp23, b3(ovincl, [128, 28, 16]), l3(tval[:, 0:28], [128, 28, 16]), ALU.is_le, [kovincl, 'tval'], [kcmp2])
        etf, ketf = ra(28)
        S.red(etf, cmp23, ALU.add, [kcmp2], [ketf])
        skp, kskp = ra(28)
        S.memset('dve', skp[:, 0:2], 0.0, [kskp])
        S.tt('dve', skp[:, 2:28], etf[:, 2:28], etf[:, 0:26], ALU.is_equal, [ketf], [kskp])
        wifb, kwif = ra(56)
        wif = wifb.rearrange("p (t h) -> p t h", h=2)
        S.ts('dve', wif[:, :, 0], etf, 256.0, p2col[:, 0:1], ALU.mult, ALU.add, [ketf, 'p2col'], [kwif])
        S.stt(wif[:, :, 0], skp, 65536.0, wif[:, :, 0], ALU.mult, ALU.add, [kskp, kwif], [kwif])
        S.ts('dve', wif[:, :, 1], wif[:, :, 0], 1.0, None, ALU.add, None, [kwif], [kwif])
        S.cp('dve', widx[:], wif, [kwif], ['widx'])
        zt = mixT[:].rearrange("p g t -> p (g t)").bitcast(I32)[:, 0:1280]
        mk2 = [('mixT', 0), ('mixT', 1)]
        S.memset('dve', zt, 0, mk2)
        S.dma('sp', slot_tok.rearrange("(p r) c -> p (r c)", p=128), zt, mk2, ['stok0'], 'stok0')
        for T in range(16):
            for k in range(2):
                S.add('pool', (lambda e, T=T, k=k: e.indirect_dma_start(
                    out=slot_tok[:, :], out_offset=bass.IndirectOffsetOnAxis(ap=s12i[:, k, T:T + 1].bitcast(U32), axis=0),
                    in_=tok16[:, T, :], in_offset=None)),
                    ['stok0', 's12i', 'tok16'], [('stok', T, k)], dma='sct')

    def moe_sparse():
        S.fence(('X1', 'ACC', 'K', 'H', 'PL'))
        X1b = X1[:].bitcast(BF16)
        EW = [[X1b[:, m * 4096:(m + 1) * 4096] for m in range(3)],
              [accb[:, m * 4096:(m + 1) * 4096] for m in range(3)]]
        NG = 4
        G = [KR[:, i * 1024:(i + 1) * 1024] for i in range(NG)]
        hsT = [KR[:, 4096 + i * 1024: 5120 + i * 1024].rearrange("p (k s) -> p k s", k=8) for i in range(2)]
        sil = [KR[:, 6144 + i * 512: 6656 + i * 512] for i in range(2)]
        hidb = [KR[:, 7168 + i * 512: 7680 + i * 512] for i in range(2)]
        hidT = [KR[:, 8192 + i * 512: 8704 + i * 512].rearrange("p (k s) -> p k s", k=4) for i in range(2)]
        hTf = hT[:].rearrange("p c t -> p (c t)").bitcast(F32)
        Yst = [hTf[:, i * 1024:(i + 1) * 1024] for i in range(2)]
        wmats = (ew_gate, ew_up, ew_down)
        wsrc = [w_.rearrange("e k n -> (e k n)").rearrange("(r c) -> r c", c=2048) for w_ in wmats]
        stok_keys = [('stok', T, k) for T in range(16) for k in range(2)]
        h2keys = [('h2d', p_, t_) for p_ in range(2) for t_ in range(8)]
        for t in range(NTS):
            S.dma('sp', gidx[:, t:t + 1], slot_tok[t * 128:(t + 1) * 128, 0:1], stok_keys + ['p2col'], [('gidx', t)], 'gidx', slow=True)
        gkeys = [('gidx', t_) for t_ in range(NTS)]
        bcreg = {}

        def wbuf(t):
            return (t // 3) % 2 if t < NMAIN else ((t - NMAIN) // 2) % 2

        def L_static(e, mats=range(3)):
            b = e % 2
            for m in mats:
                src = wmats[m][e].rearrange("k n -> (k n)").rearrange("(p c) -> p c", p=128)
                for h in range(2):
                    S.dma('pool', EW[b][m][:, h * 2048:(h + 1) * 2048], src[:, h * 2048:(h + 1) * 2048], (),
                          [('ew%d' % b, m)], ('ew', b, m, h))

        def wload(e, i, m, h, b):
            if 'r' not in bcreg:
                bcreg['r'] = e.alloc_register("wbound")
                e.reg_mov(bcreg['r'], 4095)
            return e.indirect_dma_start(
                out=EW[b][m][:, h * 2048:(h + 1) * 2048], out_offset=None, in_=wsrc[m],
                in_offset=bass.IndirectOffsetOnAxis(ap=widx[:, i, h:h + 1].bitcast(U32), axis=0),
                bounds_check=bcreg['r'], oob_is_err=False)

        def L_dyn(i, mats=range(3)):
            b = i % 2
            for m in mats:
                for h in range(2):
                    S.add('pool', (lambda e, i=i, m=m, h=h, b=b: wload(e, i, m, h, b)),
                          ['widx'], [('ew%d' % b, m)], dma=('ew', b, m, h))

        def L_g(t):
            b = t % NG
            S.add('pool', (lambda e, t=t, b=b: e.indirect_dma_start(
                out=G[b], out_offset=None, in_=h2d[:, :],
                in_offset=bass.IndirectOffsetOnAxis(ap=gidx[:, t:t + 1].bitcast(U32), axis=0))),
                gkeys + h2keys, [('mg', 'G', b)], dma=('G', b))

        def T1(t):
            b = t % 2
            gb = t % NG
            bv = PSb[b].rearrange("p (k s) -> p k s", k=8)
            g3 = G[gb].rearrange("s (p k) -> s k p", k=8)
            for kc in range(8):
                S.tr(bv[:, kc, :], g3[:, kc, :], ident[:], [('mg', 'G', gb), 'ident'], [psk(b)])
            S.cp('act', hsT[b], bv, [psk(b)], [('mg', 'hsT', b)])

        def A_(t):
            b = t % 2
            wb = wbuf(t)
            for kc in range(8):
                S.mm(PS[2][:, 0:512], hsT[b][:, kc, :], EW[wb][0][:, kc * 512:(kc + 1) * 512], kc == 0, kc == 7,
                     [('mg', 'hsT', b), ('ew%d' % wb, 0)], [psk(2)])
            for kc in range(8):
                S.mm(PS[3][:, 0:512], hsT[b][:, kc, :], EW[wb][1][:, kc * 512:(kc + 1) * 512], kc == 0, kc == 7,
                     [('mg', 'hsT', b), ('ew%d' % wb, 1)], [psk(3)])
            S.act(sil[b], PS[2][:, 0:512], AF.Silu, [psk(2)], [('mg', 'sil', b)])
            S.tt('dve', hidb[b], PS[3][:, 0:512], sil[b], ALU.mult, [psk(3), ('mg', 'sil', b)], [('mg', 'hid', b)])

        def T2D(t):
            b = t % 2
            wb = wbuf(t)
            bv = PSb[4].rearrange("p (k s) -> p k s", k=8)
            h3 = hidb[b].rearrange("s (p k) -> s k p", k=4)
            for fc in range(4):
                S.tr(bv[:, fc, :], h3[:, fc, :], ident[:], [('mg', 'hid', b), 'ident'], [psk(4)])
            S.cp('act', hidT[b], bv[:, 0:4, :], [psk(4)], [('mg', 'hidT', b)])
            for nh in range(2):
                for fc in range(4):
                    S.mm(PS[5 + nh][:, 0:512], hidT[b][:, fc, :], EW[wb][2][:, fc * 1024 + nh * 512:fc * 1024 + (nh + 1) * 512],
                         fc == 0, fc == 3, [('mg', 'hidT', b), ('ew%d' % wb, 2)], [psk(5 + nh)])
            S.cp('act', Yst[b][:, 0:512], PS[5][:, 0:512], [psk(5)], [('yst', b, 0)])
            S.cp('dve', Yst[b][:, 512:1024], PS[6][:, 0:512], [psk(6)], [('yst', b, 1)])
            S.dma('sp', yslot[t * 128:(t + 1) * 128, :], Yst[b], [('yst', b, 0), ('yst', b, 1)], [('ysl', t)], ('yso', b))

        def loads_after_A(t):
            if t < NMAIN:
                e, j = divmod(t, 3)
                if j == 2 and e + 2 < 16:
                    L_static(e + 2, (0, 1))
                if j == 2 and e + 2 >= 16:
                    L_dyn(e + 2 - 16, (0, 1))
            else:
                i, j = divmod(t - NMAIN, 2)
                if j == 1 and i + 2 < NTB:
                    L_dyn(i + 2, (0, 1))

        def loads_after_D(t):
            if t < NMAIN:
                e, j = divmod(t, 3)
                if j == 2 and e + 2 < 16:
                    L_static(e + 2, (2,))
                if j == 2 and e + 2 >= 16:
                    L_dyn(e + 2 - 16, (2,))
            else:
                i, j = divmod(t - NMAIN, 2)
                if j == 1 and i + 2 < NTB:
                    L_dyn(i + 2, (2,))

        for t in range(3):
            L_g(t)
        L_static(0)
        L_static(1)
        T1(0)
        for s_ in range(NTS + 1):
            if s_ + 3 < NTS:
                L_g(s_ + 3)
            if s_ + 1 < NTS:
                T1(s_ + 1)
            if s_ < NTS:
                A_(s_)
                loads_after_A(s_)
            if 0 <= s_ - 1 < NTS:
                T2D(s_ - 1)
                loads_after_D(s_ - 1)

    def final(p):
        S.fence(('X1', 'PL'))
        S.dma('sp', g2b[:], g2row[p:p + 1, :].to_broadcast([128, 1024]), [('g2row', p)], ['g2b'], 'g2b')
        FR = [[X1[:, (k * 2 + i) * 1024:(k * 2 + i + 1) * 1024] for i in range(2)] for k in range(2)]
        ysl_keys = [('ysl', t) for t in range(NTS)]
        for tile in range(8):
            T = p * 8 + tile
            sl = tile % 2
            xkey = ('xst', sl)
            S.dma('sp', xst[sl][:], x1d[p * 1024 + tile * 128: p * 1024 + (tile + 1) * 128, :],
                  [('x1d', p, t_) for t_ in range(8)], [xkey], xkey)
            for k in range(2):
                S.add('pool', (lambda e, k=k, T=T, sl=sl: e.indirect_dma_start(
                    out=FR[k][sl], out_offset=None, in_=yslot[:, :],
                    in_offset=bass.IndirectOffsetOnAxis(ap=s12i[:, k, T:T + 1].bitcast(U32), axis=0))),
                    ['s12i'] + ysl_keys, [('fr', k, sl)], dma=('fr', k, sl))
            r1, r2 = FR[0][sl], FR[1][sl]
            S.act(r1, r1, AF.Copy, [('fr', 0, sl), ('W1g', p)], [('fr', 0, sl)], scale=W1g[:, T, :])
            S.stt(r1, r2, W2g[:, T, :], r1, ALU.mult, ALU.add, [('fr', 1, sl), ('fr', 0, sl), ('W2g', p)], [('fr', 0, sl)])
            S.tt('dve', r1, r1, g2b[:], ALU.mult, [('fr', 0, sl), 'g2b'], [('fr', 0, sl)])
            S.tt('dve', r1, r1, xst[sl][:], ALU.add, [('fr', 0, sl), xkey], [('fr', 0, sl)])
            S.dma('sp', y[p * 1024 + tile * 128: p * 1024 + (tile + 1) * 128, :], r1, [('fr', 0, sl)], [], ('yo', sl))

    run_pass(0)
    run_pass(1)
    routing()
    moe_sparse()
    final(0)
    final(1)
    S.emit()
    return nc, S


_CACHE = {}


def _consts():
    rows = 1024 // 64
    row_ids = np.repeat(np.arange(rows, dtype=np.float32), 64)
    col_ids = np.tile(np.arange(64, dtype=np.float32), rows)
    inv_freq = np.power(np.float32(10000.0), -np.arange(16, dtype=np.float32) / np.float32(16)).astype(np.float32)
    ang_r = row_ids[:, None] * inv_freq[None, :]
    ang_c = col_ids[:, None] * inv_freq[None, :]
    ang = np.concatenate([ang_r, ang_r, ang_c, ang_c], axis=-1).astype(np.float32)
    cos = np.cos(ang).astype(np.float32)
    sin = np.sin(ang).astype(np.float32)
    sgn = np.concatenate([-np.ones(16), np.ones(16), -np.ones(16), np.ones(16)]).astype(np.float32)
    sin_f = (sin * sgn[None, :]).astype(np.float32)
    rcb = np.zeros((1, 64), np.float32)
    for g, w in enumerate((2, 4, 8, 16)):
        hw = w // 2
        for t in range(hw):
            rcb[0, g * 16 + t] = 1.0 / (t + hw)
            rcb[0, g * 16 + 8 + t] = 1.0 / (w - t)
    return cos, sin_f, rcb, np.eye(128, dtype=np.float32)


def kernel(x_prompt, x_sample, c, cache_k, cache_v, c_ctx, w_ada, b_ada, norm1_g, w_in, b_gate,
           q_norm_g, k_norm_g, lambda_q1, lambda_k1, lambda_q2, lambda_k2, subln_g, pool_w, pool_scale,
           w_br_attn, w_br_pool, w_out, norm2_g, router_group_w, router_group_b, router_expert_w,
           router_expert_b, expert_w_gate, expert_w_up, expert_w_down, _dump=None):
    f = lambda a: np.ascontiguousarray(np.asarray(a, dtype=np.float32))
    key = tuple(_dump) if _dump else None
    if key not in _CACHE:
        _CACHE[key] = build_program(_dump)
    nc, S = _CACHE[key]
    cos, sin_f, rcb, eye = _consts()
    x_prompt = f(x_prompt); x_sample = f(x_sample); c = f(c); c_ctx = f(c_ctx)
    cache_k = f(cache_k); cache_v = f(cache_v)
    shared = {
        "w_ada": f(w_ada)[0], "b_ada": f(b_ada), "norm1_g": f(norm1_g), "w_in": f(w_in)[0], "b_gate": f(b_gate),
        "q_norm_g": f(q_norm_g), "k_norm_g": f(k_norm_g),
        "lam4": np.concatenate([f(lambda_q1), f(lambda_k1), f(lambda_q2), f(lambda_k2)], axis=1),
        "subln_g": f(subln_g), "pool_w": f(pool_w)[0], "pool_scale": f(pool_scale),
        "w_br_attn": f(w_br_attn)[0], "w_br_pool": f(w_br_pool)[0], "w_out": f(w_out)[0], "norm2_g": f(norm2_g),
        "router_w": np.concatenate([f(router_group_w)[0], f(router_expert_w)[0]], axis=1),
        "router_b": np.concatenate([f(router_group_b), f(router_expert_b)], axis=1),
        "ew_gate": f(expert_w_gate)[0], "ew_up": f(expert_w_up)[0], "ew_down": f(expert_w_down)[0],
        "ident": eye, "cos_t": cos, "sin_t": sin_f, "rcb": rcb,
        "tri": np.triu(np.ones((128, 128), np.float32), k=1),
        "thr": (128.0 * np.arange(16, dtype=np.float32)).reshape(1, 16),
        "tval": np.arange(48, dtype=np.float32).reshape(1, 48),
        "rtab": np.concatenate([384.0 + 256.0 * np.arange(7, dtype=np.float32), np.full(6, 1.0e9, np.float32), np.zeros(3, np.float32),
                                np.arange(16, dtype=np.float32), np.zeros(32, np.float32)]).reshape(1, 64),
    }
    in_maps = []
    for i in range(NCORES):
        m = dict(shared)
        m["x"] = np.concatenate([x_prompt[4 * i:4 * i + 4].reshape(1024, 1024), x_sample[i]], axis=0)
        m["cond"] = np.stack([c_ctx, c[i]], axis=0)
        m["ck"] = cache_k[i, 0].reshape(512, 1024)
        m["cv"] = cache_v[i, 0].reshape(512, 1024)
        in_maps.append(m)
    res = run_bass_kernel_spmd(nc, in_maps, core_ids=list(range(NCORES)))
    R = res.results
    y_prompt = np.concatenate([R[i]["y"][0:1024].reshape(4, 256, 1024) for i in range(NCORES)], axis=0)
    y_sample = np.stack([R[i]["y"][1024:2048] for i in range(NCORES)], axis=0)
    new_k = np.concatenate([R[i]["nk"].reshape(4, 1, 256, 8, 128) for i in range(NCORES)], axis=0)
    new_v = np.concatenate([R[i]["nv"].reshape(4, 1, 256, 8, 128) for i in range(NCORES)], axis=0)
    if _dump:
        kernel.last_dumps = [{nm: R[i]["dbg_" + nm] for nm, _ in _dump} for i in range(NCORES)]
    return (y_prompt.astype(np.float32), y_sample.astype(np.float32), new_k.astype(np.float32), new_v.astype(np.float32))
```

```python
import numpy as np
import concourse.bass as bass
import concourse.mybir as mybir
from concourse.bass_utils import run_bass_kernel_spmd

F32 = mybir.dt.float32
BF16 = mybir.dt.bfloat16
I32 = mybir.dt.int32
U32 = mybir.dt.uint32
AF = mybir.ActivationFunctionType
ALU = mybir.AluOpType
AX = mybir.AxisListType

EPS = 1e-6
NCORES = 8
LAMBDA_INIT = 0.8 - 0.6 * 1.0
BIG = 1.0e9

REGION_OF = {
    'vt': 'X1', 'x1': 'X1',
    'qT': 'ACC', 'mrgT': 'ACC', 'acc': 'ACC', 'st2': 'ACC', 'ost': 'ACC',
    'kT': 'K', 'silu': 'K', 'hid': 'K',
    'pl': 'PL',
    'ew0': 'X1', 'fr': 'X1', 'ew1': 'ACC', 'mg': 'K', 'hT': 'H', 'yst': 'H',
}


class Sched:
    def __init__(self, nc, scratch):
        self.nc = nc
        self.ops = []
        self.lastw = {}
        self.readers = {}
        self.scratch = scratch
        self.seen = {}

    def add(self, eng, fn, reads=(), writes=(), dma=None):
        writes = list(writes) + [k for k in reads if isinstance(k, tuple) and k[0] == 'ps' and k not in writes]
        reads = [k for k in reads if not (isinstance(k, tuple) and k[0] == 'ps')]
        for k in reads + writes:
            nm = k[0] if isinstance(k, tuple) else k
            rg = REGION_OF.get(nm)
            if rg is not None:
                self.seen.setdefault(rg, set()).add(k)
                fk = ('fence', rg)
                if fk not in reads:
                    reads.append(fk)
        idx = len(self.ops)
        deps = set()
        for k in reads:
            w = self.lastw.get(k)
            if w is not None:
                deps.add(w)
        for k in writes:
            w = self.lastw.get(k)
            if w is not None:
                deps.add(w)
            for r in self.readers.get(k, ()):
                deps.add(r)
        deps.discard(idx)
        self.ops.append(dict(eng=eng, fn=fn, deps=deps, dma=dma))
        for k in reads:
            self.readers.setdefault(k, []).append(idx)
        for k in writes:
            self.lastw[k] = idx
            self.readers[k] = []
        return idx

    def fence(self, regions=('X1', 'ACC', 'K', 'PL', 'H')):
        for rg in regions:
            keys = list(self.seen.get(rg, ())) + [('fence', rg), 'fscr']
            sc = self.scratch
            idx = len(self.ops)
            deps = set()
            for k in keys:
                w = self.lastw.get(k)
                if w is not None:
                    deps.add(w)
                for r in self.readers.get(k, ()):
                    deps.add(r)
            self.ops.append(dict(eng='dve', fn=(lambda e: e.memset(sc, 0.0)), deps=deps, dma=None))
            for k in keys:
                self.lastw[k] = idx
                self.readers[k] = []

    def mm(self, out, lhsT, rhs, start, stop, reads, writes, skip=False):
        if skip:
            return self.add('pe', lambda e: e.matmul(out, lhsT, rhs, start=start, stop=stop, skip_group_check=True),
                            reads, writes)
        return self.add('pe', lambda e: e.matmul(out, lhsT, rhs, start=start, stop=stop), reads, writes)

    def tr(self, out, in_, ident, reads, writes):
        return self.add('pe', lambda e: e.transpose(out, in_, ident), reads, writes)

    def act(self, out, in_, func, reads, writes, bias=None, scale=None, accum_out=None):
        kw = {}
        if bias is not None:
            kw['bias'] = bias
        if scale is not None:
            kw['scale'] = scale
        if accum_out is not None:
            kw['accum_out'] = accum_out
        return self.add('act', lambda e: e.activation(out, in_, func, **kw), reads, writes)

    def tt(self, eng, out, in0, in1, op, reads, writes):
        return self.add(eng, lambda e: e.tensor_tensor(out, in0, in1, op), reads, writes)

    def ts(self, eng, out, in0, s1, s2, op0, op1, reads, writes):
        if op1 is None:
            return self.add(eng, lambda e: e.tensor_scalar(out, in0, s1, None, op0), reads, writes)
        return self.add(eng, lambda e: e.tensor_scalar(out, in0, s1, s2, op0, op1), reads, writes)

    def stt(self, out, in0, scalar, in1, op0, op1, reads, writes):
        return self.add('dve', lambda e: e.scalar_tensor_tensor(out, in0, scalar, in1, op0, op1), reads, writes)

    def red(self, out, in_, op, reads, writes):
        return self.add('dve', lambda e: e.tensor_reduce(out, in_, AX.X, op), reads, writes)

    def recip(self, out, in_, reads, writes):
        return self.add('dve', lambda e: e.reciprocal(out, in_), reads, writes)

    def cp(self, eng, out, in_, reads, writes):
        if eng == 'act':
            return self.add('act', lambda e: e.copy(out, in_), reads, writes)
        return self.add(eng, lambda e: e.tensor_copy(out, in_), reads, writes)

    def memset(self, eng, ap, val, writes):
        return self.add(eng, lambda e: e.memset(ap, val), (), writes)

    def dma(self, q, out, in_, reads, writes, key, slow=False):
        if slow:
            return self.add(q, lambda e: e.dma_start(out=out, in_=in_, allow_slow_non_contiguous=True),
                            reads, writes, dma=key)
        return self.add(q, lambda e: e.dma_start(out=out, in_=in_), reads, writes, dma=key)

    def emit(self, final_wait_eng='sp'):
        nc = self.nc
        ops = self.ops
        n = len(ops)
        has_dep = [False] * n
        for i, o in enumerate(ops):
            latest = {}
            keep = set()
            for d in o['deps']:
                od = ops[d]
                if od['dma'] is not None:
                    keep.add(d)
                    continue
                if od['eng'] == 'pe' and o['eng'] == 'pe' and o['dma'] is None:
                    continue
                if d > latest.get(od['eng'], -1):
                    latest[od['eng']] = d
            keep.update(latest.values())
            o['deps'] = keep
            for d in keep:
                has_dep[d] = True
        eng_names = ['sp', 'act', 'pool', 'dve', 'pe']
        eng_sem = {e: nc.alloc_semaphore(name='sem_' + e) for e in eng_names}
        dma_sems = {}
        dma_cnt = {}
        eng_cnt = {e: 0 for e in eng_names}
        sig = [None] * n
        for i, o in enumerate(ops):
            if o['dma'] is not None:
                k = o['dma']
                if k not in dma_sems:
                    dma_sems[k] = nc.alloc_semaphore(name='dsem%d' % len(dma_sems))
                    dma_cnt[k] = 0
                dma_cnt[k] += 16
                sig[i] = (dma_sems[k], dma_cnt[k], 16)
            elif has_dep[i]:
                eng_cnt[o['eng']] += 1
                sig[i] = (eng_sem[o['eng']], eng_cnt[o['eng']], 1)
        self.n_sems = len(dma_sems) + 5
        self.eng_cnt = eng_cnt
        streams = {e: [i for i, o in enumerate(ops) if o['eng'] == e] for e in eng_names}
        finals = [(dma_sems[k], dma_cnt[k]) for k in dma_sems]

        def run_stream(ename, eng):
            waited = {}
            for i in streams[ename]:
                o = ops[i]
                need = {}
                for d in o['deps']:
                    s, v, _ = sig[d]
                    if v > need.get(s.num, (None, 0))[1]:
                        need[s.num] = (s, v)
                for num in sorted(need):
                    s, v = need[num]
                    if waited.get(num, 0) < v:
                        eng.wait_ge(s, v)
                        waited[num] = v
                ins = o['fn'](eng)
                if sig[i] is not None:
                    ins.then_inc(sig[i][0], sig[i][2])
            if ename == final_wait_eng:
                for s, v in finals:
                    if waited.get(s.num, 0) < v:
                        eng.wait_ge(s, v)

        with nc.Block() as block:
            @block.sync
            def _(e):
                run_stream('sp', e)

            @block.scalar
            def _(e):
                run_stream('act', e)

            @block.gpsimd
            def _(e):
                run_stream('pool', e)

            @block.vector
            def _(e):
                run_stream('dve', e)

            @block.tensor
            def _(e):
                run_stream('pe', e)


class WRing:
    R = 10

    def __init__(self, S, ring, units):
        self.S = S
        self.ring = ring
        self.units = units
        self.next_dma = 0
        self.next_acq = 0
        self.rel = set()
        self.pump()

    def pump(self):
        while self.next_dma < len(self.units):
            j = self.next_dma
            if j >= self.R and (j - self.R) not in self.rel:
                break
            src, kc, ncols = self.units[j]
            slot = j % self.R
            dst = self.ring[:, slot, 0:kc * ncols].rearrange("p (k n) -> p k n", k=kc)
            self.S.dma('pool', dst, src, (), [('w', slot)], ('w', slot))
            self.next_dma += 1

    def acquire(self):
        j = self.next_acq
        assert j < self.next_dma, "weight ring deadlock: unit %d not yet issued" % j
        self.next_acq += 1
        src, kc, ncols = self.units[j]
        slot = j % self.R
        view = self.ring[:, slot, 0:kc * ncols].rearrange("p (k n) -> p k n", k=kc)
        return view, ('w', slot), j

    def release(self, j):
        self.rel.add(j)
        self.pump()


def build_program(dump=None):
    nc = bass.Bass("TRN2", target_bir_lowering=False)
    dump = dump or []

    def din(name, shape):
        return nc.dram_tensor(name, list(shape), F32, kind="ExternalInput").ap()

    def dout(name, shape):
        return nc.dram_tensor(name, list(shape), F32, kind="ExternalOutput").ap()

    x = din("x", (2048, 1024))
    cond = din("cond", (2, 1024))
    ck = din("ck", (512, 1024))
    cv = din("cv", (512, 1024))
    w_ada = din("w_ada", (1024, 6144))
    b_ada = din("b_ada", (1, 6144))
    norm1_g = din("norm1_g", (1, 1024))
    w_in = din("w_in", (1024, 5632))
    b_gate = din("b_gate", (1, 2048))
    q_norm_g = din("q_norm_g", (1, 64))
    k_norm_g = din("k_norm_g", (1, 64))
    lam_in = din("lam4", (1, 256))
    subln_g = din("subln_g", (1, 128))
    pool_w = din("pool_w", (4, 128, 128))
    pool_scale = din("pool_scale", (1, 512))
    w_br_attn = din("w_br_attn", (1024, 1024))
    w_br_pool = din("w_br_pool", (512, 1024))
    w_out = din("w_out", (1024, 1024))
    norm2_g = din("norm2_g", (1, 1024))
    router_w = din("router_w", (1024, 20))
    router_b = din("router_b", (1, 20))
    ew_gate = din("ew_gate", (16, 1024, 512))
    ew_up = din("ew_up", (16, 1024, 512))
    ew_down = din("ew_down", (16, 512, 1024))
    ident_d = din("ident", (128, 128))
    cos_d = din("cos_t", (1024, 64))
    sin_d = din("sin_t", (1024, 64))
    rcb_d = din("rcb", (1, 64))

    tri_d = din("tri", (128, 128))
    thr_d = din("thr", (1, 16))
    tval_d = din("tval", (1, 48))
    rtab_d = din("rtab", (1, 64))
    NMAIN = 48
    NTB = 14
    NTAIL = 2 * NTB
    NTS = NMAIN + NTAIL
    modrows = nc.dram_tensor("modrows", [2, 2, 1024], F32, kind="Internal").ap()
    g2row = nc.dram_tensor("g2row", [2, 1024], F32, kind="Internal").ap()
    gps = nc.dram_tensor("gps", [2, 1024], F32, kind="Internal").ap()
    h2d = nc.dram_tensor("h2d", [2048, 1024], BF16, kind="Internal").ap()
    x1d = nc.dram_tensor("x1d", [2048, 1024], F32, kind="Internal").ap()
    slot_tok = nc.dram_tensor("slot_tok", [80 * 128, 16], I32, kind="Internal").ap()
    yslot = nc.dram_tensor("yslot", [NTS * 128, 1024], F32, kind="Internal").ap()
    y = dout("y", (2048, 1024))
    nk = dout("nk", (1024, 1024))
    nv = dout("nv", (1024, 1024))
    dumps = {}
    for nm, shp in dump:
        dumps[nm] = dout("dbg_" + nm, shp)

    A = nc.alloc_sbuf_tensor
    ring = A("ring", [128, 10, 2048], BF16)
    hT = A("hT", [128, 8, 1024], BF16)
    X1 = A("X1", [128, 8192], F32)
    x1 = X1[:].rearrange("p (t d) -> p t d", t=8)
    vtok = X1[:].bitcast(BF16)[:, 0:12 * 8 * 132].rearrange("p (j h e) -> p j h e", j=12, h=8)
    ACC = A("ACC", [128, 8192], F32)
    acc = ACC[:].rearrange("p (t d) -> p t d", t=8)
    accb = ACC[:].bitcast(BF16)
    qT = accb[:, 0:8192].rearrange("p (h t) -> p h t", h=8)
    mrgT = accb[:, 8192:16384].rearrange("p (h t) -> p h t", h=8)
    mrg_f = ACC[:, 4096:8192]
    KR = A("KR", [128, 8 * 1536], BF16)
    kT = KR[:].rearrange("p (h t) -> p h t", h=8)
    silu_b = [KR[:, i * 512:(i + 1) * 512] for i in range(2)]
    hid = [KR[:, 1024 + i * 4096: 1024 + (i + 1) * 4096].rearrange("p (b f t) -> p b f t", b=2, f=4) for i in range(2)]
    mixT = A("mixT", [128, 4, 1024], BF16)
    PL = A("PL", [128, 4096], F32)
    PLb = PL[:].bitcast(BF16)
    xst = [A("xst%d" % i, [128, 1024], F32) for i in range(2)]
    xn = [A("xn%d" % i, [128, 1024], BF16) for i in range(2)]
    ident = A("identb", [128, 128], BF16)
    cosb = A("cosb", [128, 8, 64], F32)
    sinb = A("sinb", [128, 8, 64], F32)
    gq_b = A("gq_b", [128, 64], F32)
    gk_b = A("gk_b", [128, 64], F32)
    gsub_b = A("gsub_b", [128, 128], F32)
    rcb = A("rcbs", [128, 64], F32)
    lam_b = A("lam_b", [128, 256], F32)
    lam_t = A("lam_t", [128, 128], F32)
    lam_s = A("lam_s", [128, 8], F32)
    sc32 = A("sc32", [128, 8, 2], F32)
    scT2 = A("scT2", [128, 8, 2], BF16)
    screp = A("screp", [128, 8, 128], BF16)
    screp1 = A("screp1", [128, 8, 128], BF16)
    modT2 = A("modT2", [128, 48, 2], F32)
    bT = A("bT", [128, 48], F32)
    n1gT = A("n1gT", [128, 8], F32)
    n2gT = A("n2gT", [128, 8], F32)
    a1T2 = A("a1T2", [128, 8, 2], F32)
    a2T2 = A("a2T2", [128, 8, 2], F32)
    g1b = A("g1b", [128, 1024], F32)
    g2b = A("g2b", [128, 1024], F32)
    bgT = A("bgT", [128, 16], F32)
    pscT = A("pscT", [128, 4], F32)
    rb_b = A("rb_b", [128, 20], F32)
    poolw = A("poolw", [128, 4, 128], BF16)
    rw = A("rw", [128, 8, 20], BF16)
    ss = A("ss", [128, 8], F32)
    rstd = A("rstd", [128, 8], F32)
    ss8 = A("ss8", [128, 2, 8], F32)
    rs8 = A("rs8", [128, 2, 8, 1], F32)
    ss8c = A("ss8c", [128, 8], F32)
    rs8c = A("rs8c", [128, 8, 1], F32)
    rz = A("rz", [128, 4, 2, 1], F32)
    r2n = A("r2n", [128, 4, 1], F32)
    O1g = A("O1g", [128, 16, 16], F32)
    O2g = A("O2g", [128, 16, 16], F32)
    W1g = A("W1g", [128, 16, 1], F32)
    W2g = A("W2g", [128, 16, 1], F32)
    Mb = A("Mb", [128, 16, 16], BF16)
    s12i = A("s12i", [128, 2, 16], I32)
    widx = A("widx", [128, 28, 2], I32)
    gidx = A("gidx", [128, 76], I32)
    trib = A("trib", [128, 128], BF16)
    onesb = A("onesb", [128, 128], BF16)
    thr = A("thr_sb", [128, 16], F32)
    tval = A("tval_sb", [128, 48], F32)
    tok16 = A("tok16", [128, 16, 16], I32)
    p2col = A("p2col", [128, 1], F32)
    rtab = A("rtab_sb", [128, 64], F32)
    fscr = A("fscr", [128, 1], F32)

    PSALL = nc.alloc_psum_tensor("psall", [128, 4096], F32)
    PSALLb = PSALL[:].bitcast(BF16)
    PS = [PSALL[:, i * 512:(i + 1) * 512] for i in range(8)]
    PSb = [PSALLb[:, i * 1024:(i + 1) * 1024] for i in range(8)]

    S = Sched(nc, fscr[:])

    def psk(i):
        return ('ps', i)

    def cols(w, c0, n, kc):
        return (w[:, c0:c0 + n].rearrange("(k p) n -> p k n", p=128), kc, n)

    pass_units = []
    for u in range(24):
        pass_units.append(cols(w_ada, 256 * u, 256, 8))
    for u in range(14):
        pass_units.append(cols(w_in, 256 * u, 256, 8))
    for ng in range(4):
        pass_units.append(cols(w_in, 3584 + 256 * ng, 256, 8))
        pass_units.append(cols(w_in, 4608 + 256 * ng, 256, 8))
        pass_units.append(cols(w_br_attn, 256 * ng, 256, 8))
        pass_units.append(cols(w_br_pool, 256 * ng, 256, 4))
    for j in range(4):
        pass_units.append(cols(w_out, 256 * j, 256, 8))
    W = WRing(S, ring, pass_units + pass_units[24:])

    def cload(dst, src, key, q='sp', slow=False):
        S.dma(q, dst, src, (), [key], key, slow=slow)

    for j in range(2):
        S.dma('sp', sc32[:, :, j], cond[j, :].rearrange("(k p) -> p k", p=128), (), [('sc32', j)], ('sc32', j), slow=True)

    cload(ident[:], ident_d[:, :], 'ident', q='pool')
    cload(cosb[:], cos_d.rearrange("(t p) d -> p t d", p=128), 'cosb')
    cload(sinb[:], sin_d.rearrange("(t p) d -> p t d", p=128), 'sinb')
    cload(gq_b[:], q_norm_g[0:1, :].to_broadcast([128, 64]), 'gq_b')
    cload(gk_b[:], k_norm_g[0:1, :].to_broadcast([128, 64]), 'gk_b')
    cload(gsub_b[:], subln_g[0:1, :].to_broadcast([128, 128]), 'gsub_b')
    cload(rcb[:], rcb_d[0:1, :].to_broadcast([128, 64]), 'rcb')
    cload(lam_b[:], lam_in[0:1, :].to_broadcast([128, 256]), 'lam_b')
    cload(bT[:], b_ada[0, :].rearrange("(c p) -> p c", p=128), 'bT', slow=True)
    cload(n1gT[:], norm1_g[0, :].rearrange("(c p) -> p c", p=128), 'n1gT', slow=True)
    cload(n2gT[:], norm2_g[0, :].rearrange("(c p) -> p c", p=128), 'n2gT', slow=True)
    cload(bgT[:], b_gate[0, :].rearrange("(c p) -> p c", p=128), 'bgT', slow=True)
    cload(pscT[:], pool_scale[0, :].rearrange("(c p) -> p c", p=128), 'pscT', slow=True)
    cload(rb_b[:], router_b[0:1, :].to_broadcast([128, 20]), 'rb_b')
    cload(poolw[:], pool_w.rearrange("g c e -> c g e"), 'poolw', q='pool')
    cload(rw[:], router_w.rearrange("(k p) n -> p k n", p=128), 'rw', q='pool')

    cload(trib[:], tri_d[:, :], 'trib', q='pool')
    cload(thr[:], thr_d[0:1, :].to_broadcast([128, 16]), 'thr')
    cload(tval[:], tval_d[0:1, :].to_broadcast([128, 48]), 'tval')
    cload(rtab[:], rtab_d[0:1, :].to_broadcast([128, 64]), 'rtab')
    S.memset('dve', onesb[:], 1.0, ['onesb'])
    S.add('pool', lambda e: e.iota(tok16[:], [[128, 16], [0, 16]], base=0, channel_multiplier=1), (), ['tok16'])
    S.add('pool', lambda e: e.iota(gidx[:, 0:1], [[0, 1]], base=0, channel_multiplier=2), (), ['p2i'])
    S.cp('dve', p2col[:], gidx[:, 0:1], ['p2i'], ['p2col'])
    S.ts('dve', gq_b[:], gq_b[:], 8.0 * 0.125, None, ALU.mult, None, ['gq_b'], ['gq_b'])
    S.ts('dve', gk_b[:], gk_b[:], 8.0, None, ALU.mult, None, ['gk_b'], ['gk_b'])
    S.ts('dve', gsub_b[:], gsub_b[:], (1.0 - LAMBDA_INIT) * (128.0 ** 0.5), None, ALU.mult, None, ['gsub_b'], ['gsub_b'])
    lb = lam_b[:].rearrange("p (a b d) -> p a b d", a=2, b=2)
    lt = lam_t[:].rearrange("p (a d) -> p a d", a=2)
    S.tt('dve', lt, lb[:, :, 0, :], lb[:, :, 1, :], ALU.mult, ['lam_b'], ['lam_t'])
    S.red(lam_s[:, 0:2], lt, ALU.add, ['lam_t'], ['lam_s'])
    S.act(lam_s[:, 2:4], lam_s[:, 0:2], AF.Exp, ['lam_s'], ['lam_s2'])
    S.tt('dve', lam_s[:, 4:5], lam_s[:, 2:3], lam_s[:, 3:4], ALU.subtract, ['lam_s2'], ['lam_s3'])
    S.ts('dve', lam_s[:, 5:6], lam_s[:, 4:5], LAMBDA_INIT, -1.0, ALU.add, ALU.mult, ['lam_s3'], ['neg_lam'])
    neg_lam = lam_s[:, 5:6]

    def dbg(name, src_ap, reads):
        if name in dumps:
            S.dma('pool', dumps[name], src_ap, reads, [], 'dbg_' + name)

    def norm_T(p, src, aT, shT, hkey):
        for blk in range(2):
            banks = [4 * blk + i for i in range(4)]
            for t in range(4):
                tile = blk * 4 + t
                sl = tile % 2
                if src == 'x':
                    xs = xst[sl][:]
                    xkey = ('xst', sl)
                    S.dma('sp', xs, x[p * 1024 + tile * 128: p * 1024 + (tile + 1) * 128, :], (), [xkey], xkey)
                else:
                    xs = x1[:, tile, :]
                    xkey = ('x1', tile)
                S.act(xn[sl][:], xs, AF.Square, [xkey], [('xn', sl), ('ss', tile)], accum_out=ss[:, tile:tile + 1])
                S.act(rstd[:, tile:tile + 1], ss[:, tile:tile + 1], AF.Ln, [('ss', tile)], [('rstd', tile)],
                      bias=EPS, scale=1.0 / 1024.0)
                S.act(rstd[:, tile:tile + 1], rstd[:, tile:tile + 1], AF.Exp, [('rstd', tile)], [('rstd', tile)], scale=-0.5)
                S.act(xn[sl][:], xs, AF.Copy, [xkey, ('rstd', tile)], [('xn', sl)], scale=rstd[:, tile:tile + 1])
                if src == 'x1':
                    tmp = PL[:, sl * 1024:(sl + 1) * 1024]
                    hb = PL[:, 2048 + sl * 512: 2560 + sl * 512].bitcast(BF16)
                    S.tt('pool', tmp, xs, g1b[:], ALU.mult, [xkey, 'g1b'], [('pl', 'h2t', sl)])
                    S.stt(hb, tmp, rstd[:, tile:tile + 1], xst[0][:], ALU.mult, ALU.add,
                          [('pl', 'h2t', sl), ('rstd', tile), ('xst', 0)], [('pl', 'h2b', sl)])
                    S.dma('sp', h2d[p * 1024 + tile * 128: p * 1024 + (tile + 1) * 128, :], hb, [('pl', 'h2b', sl)],
                          [('h2d', p, tile)], ('h2o', sl))
                    S.dma('sp', x1d[p * 1024 + tile * 128: p * 1024 + (tile + 1) * 128, :], xs, [xkey],
                          [('x1d', p, tile)], 'x1o')
                for c in range(8):
                    bv = PSb[banks[c // 2]].rearrange("p (c t) -> p c t", c=2)
                    S.tr(bv[:, c % 2, t * 128:(t + 1) * 128], xn[sl][:, c * 128:(c + 1) * 128], ident[:],
                         [('xn', sl), 'ident'], [psk(banks[c // 2])])
            for c in range(8):
                bv = PSb[banks[c // 2]].rearrange("p (c t) -> p c t", c=2)
                dst = hT[:, c, blk * 512:(blk + 1) * 512]
                if c % 2 == 0:
                    S.act(dst, bv[:, c % 2, :], AF.Identity, [psk(banks[c // 2]), aT[1], shT[1]], [(hkey, blk)],
                          bias=shT[0][:, c:c + 1], scale=aT[0][:, c:c + 1])
                else:
                    S.ts('dve', dst, bv[:, c % 2, :], aT[0][:, c:c + 1], shT[0][:, c:c + 1], ALU.mult, ALU.add,
                         [psk(banks[c // 2]), aT[1], shT[1]], [(hkey, blk)])

    def run_pass(p):
        nseq, L = (4, 256) if p == 0 else (1, 1024)
        nkt = 8 if p == 0 else 12
        S.fence()
        if p == 0:
            S.act(scT2[:], sc32[:], AF.Silu, [('sc32', 0), ('sc32', 1)], ['scT'])
            S.cp('dve', screp[:], scT2[:, :, 0:1].to_broadcast([128, 8, 128]), ['scT'], ['screp'])
            S.cp('dve', screp1[:], scT2[:, :, 1:2].to_broadcast([128, 8, 128]), ['scT'], ['screp1'])
            S.dma('sp', g1b[:], b_ada[0:1, 2048:3072].to_broadcast([128, 1024]), (), ['g1b'], 'g1b')
            S.dma('sp', g2b[:], b_ada[0:1, 5120:6144].to_broadcast([128, 1024]), (), ['g2b'], 'g2b')
            mod2 = PS[0][:, 0:96].rearrange("p (c j) -> p c j", j=2)
            for u in range(24):
                wv, wk, wj = W.acquire()
                if u in (8, 9, 10, 11, 20, 21, 22, 23):
                    gb, gkey, base = (g1b, 'g1b', 8) if u < 12 else (g2b, 'g2b', 20)
                    gi_ = 0 if u < 12 else 1
                    bank = 1 + (u % 2)
                    for kc in range(8):
                        S.mm(PS[bank][:, 0:256], screp[:, kc, :], wv[:, kc, :], kc == 0, kc == 7,
                             ['screp', wk], [psk(bank)])
                    c0 = (u - base) * 256
                    S.tt('dve', gb[:, c0:c0 + 256], PS[bank][:, 0:256], gb[:, c0:c0 + 256], ALU.add,
                         [psk(bank), gkey], [gkey])
                    bank2 = 3 + (u % 2)
                    for kc in range(8):
                        S.mm(PS[bank2][:, 0:256], screp1[:, kc, :], wv[:, kc, :], kc == 0, kc == 7,
                             ['screp1', wk], [psk(bank2)])
                    gst = PL[:, (u % 4) * 256:(u % 4 + 1) * 256]
                    S.cp('act', gst, PS[bank2][:, 0:256], [psk(bank2)], [('pl', 'gst', u % 4)])
                    S.dma('sp', gps[gi_:gi_ + 1, c0:c0 + 256], gst[0:1, :], [('pl', 'gst', u % 4)], [('gps', gi_, u)], ('gpso', u % 4))
                else:
                    for cc in range(2):
                        ch = 2 * u + cc
                        for kc in range(8):
                            S.mm(mod2[:, ch, :], wv[:, kc, cc * 128:(cc + 1) * 128], scT2[:, kc, :], kc == 0, kc == 7,
                                 ['scT', wk], [psk(0)])
                W.release(wj)
            bT3 = bT[:].rearrange("p (c o) -> p c o", o=1)
            S.tt('dve', modT2[:, 0:16, :], mod2[:, 0:16, :], bT3[:, 0:16, :].to_broadcast([128, 16, 2]), ALU.add, [psk(0), 'bT'], ['modT'])
            S.tt('dve', modT2[:, 24:40, :], mod2[:, 24:40, :], bT3[:, 24:40, :].to_broadcast([128, 16, 2]), ALU.add, [psk(0), 'bT'], ['modT'])
            for j in range(2):
                S.stt(a1T2[:, :, j], modT2[:, 8:16, j], 1.0, n1gT[:], ALU.add, ALU.mult, ['modT', 'n1gT'], ['a1T'])
                S.stt(a2T2[:, :, j], modT2[:, 32:40, j], 1.0, n2gT[:], ALU.add, ALU.mult, ['modT', 'n2gT'], ['a2T'])
        else:
            gkeys1 = [('gps', 0, u) for u in (8, 9, 10, 11)]
            gkeys2 = [('gps', 1, u) for u in (20, 21, 22, 23)]
            S.dma('sp', g1b[:], b_ada[0:1, 2048:3072].to_broadcast([128, 1024]), (), ['g1b'], 'g1b')
            S.dma('sp', g2b[:], b_ada[0:1, 5120:6144].to_broadcast([128, 1024]), (), ['g2b'], 'g2b')
            S.dma('sp', xst[0][:], gps[0:1, :].to_broadcast([128, 1024]), gkeys1, [('xst', 0)], ('xst', 0))
            S.dma('sp', xst[1][:], gps[1:2, :].to_broadcast([128, 1024]), gkeys2, [('xst', 1)], ('xst', 1))
            S.tt('dve', g1b[:], g1b[:], xst[0][:], ALU.add, ['g1b', ('xst', 0)], ['g1b'])
            S.tt('dve', g2b[:], g2b[:], xst[1][:], ALU.add, ['g2b', ('xst', 1)], ['g2b'])
        a1T = a1T2[:, :, p]
        a2T = a2T2[:, :, p]
        sh1 = (modT2[:, 0:8, p], 'modT')
        sh2 = (modT2[:, 24:32, p], 'modT')

        norm_T(p, 'x', (a1T, 'a1T'), sh1, 'hT')
        if p == 0:
            for j in range(2):
                S.dma('sp', modrows[j, 0, :].rearrange("(c p) -> p c", p=128), a2T2[:, :, j], ['a2T'], [('modrows', j)], 'mro', slow=True)
                S.dma('sp', modrows[j, 1, :].rearrange("(c p) -> p c", p=128), modT2[:, 24:32, j], ['modT'], [('modrows', j)], 'mro', slow=True)
        S.dma('sp', g2row[p:p + 1, :], g2b[0:1, :], ['g2b'], [('g2row', p)], 'g2o')
        if p == 0:
            dbg('hT', hT[:], [('hT', 0), ('hT', 1)])

        sqs = [mrg_f[:, 0:512], mrg_f[:, 3584:4096]]
        zn = mrg_f[:, 512:1024]
        zg = [mrg_f[:, 1024 + i * 512: 1536 + i * 512] for i in range(2)]
        vst = [mrg_f[:, 2048 + i * 512: 2560 + i * 512] for i in range(2)]
        qkb = [mrg_f[:, 3072 + i * 256: 3328 + i * 256].bitcast(BF16) for i in range(2)]
        zns = [zn, vst[0]]
        if p == 1:
            rtab = {}
            for ti, (gb_, gkey) in enumerate(((gq_b, 'gq_b'), (gk_b, 'gk_b'))):
                Cg = PL[:, ti * 1024: ti * 1024 + 512].rearrange("p (t d) -> p t d", t=8)
                Sg = PL[:, ti * 1024 + 512: ti * 1024 + 1024].rearrange("p (t d) -> p t d", t=8)
                S.tt('pool', Cg, cosb[:], gb_[:].rearrange("p (o d) -> p o d", o=1).to_broadcast([128, 8, 64]), ALU.mult,
                     ['cosb', gkey], [('pl', 'Cg', ti)])
                g4 = gb_[:].rearrange("p (a s d) -> p a s d", a=2, s=2)
                for sidx in range(2):
                    for a_ in range(2):
                        S.tt('pool', Sg[:, :, a_ * 32 + sidx * 16: a_ * 32 + sidx * 16 + 16],
                             sinb[:, :, a_ * 32 + sidx * 16: a_ * 32 + sidx * 16 + 16],
                             g4[:, a_, 1 - sidx, :].rearrange("p (o d) -> p o d", o=1).to_broadcast([128, 8, 16]), ALU.mult,
                             ['sinb', gkey], [('pl', 'Sg', ti)])
                rtab[ti] = (Cg, Sg)
        S.memset('dve', vtok[:, :, :, 128:129], 1.0, [('vt', 'ones')])
        if p == 1:
            for jt in range(4):
                S.dma('pool', vtok[:, 8 + jt, :, 0:128],
                      cv[jt * 128:(jt + 1) * 128, :].rearrange("p (h e) -> p h e", h=8), (), [('vt', 8 + jt)], ('vtc', jt))
                sl = jt % 2
                S.dma('pool', xn[sl][:], ck[jt * 128:(jt + 1) * 128, :], (), [('xn', sl)], ('xnc', sl))
                bv = PSb[7].rearrange("p (h t) -> p h t", h=8)
                for h in range(8):
                    S.tr(bv[:, h, :], xn[sl][:, h * 128:(h + 1) * 128], ident[:], [('xn', sl), 'ident'], [psk(7)])
                S.cp('act', kT[:, :, 1024 + jt * 128: 1024 + (jt + 1) * 128], bv, [psk(7)], [('kT', 8 + jt)])
        qk_units = {}

        def qk_M(n):
            ci, tile = divmod(n, 8)
            if tile == 0:
                qk_units[ci] = (W.acquire(), W.acquire())
            (ua, uak, uaj), (ub, ubk, ubj) = qk_units[ci]
            bank = n % 4
            for half, (u_, uk_) in enumerate(((ua, uak), (ub, ubk))):
                for kc in range(8):
                    S.mm(PS[bank][:, half * 256:(half + 1) * 256], hT[:, kc, tile * 128:(tile + 1) * 128],
                         u_[:, kc, :], kc == 0, kc == 7, [('hT', tile // 4), uk_], [psk(bank)])
            if tile == 7:
                W.release(uaj)
                W.release(ubj)

        def qk_E1(n):
            bank = n % 4
            b2 = n % 2
            zps = PS[bank][:, 0:512]
            sqb = sqs[b2]
            S.act(sqb, zps, AF.Square, [psk(bank)], [('st2', 'sq', b2)])
            S.red(ss8[:, b2, :], sqb.rearrange("p (g d) -> p g d", g=8), ALU.add, [('st2', 'sq', b2)], [('ss8', b2)])
            S.act(rs8[:, b2, :, 0], ss8[:, b2, :], AF.Ln, [('ss8', b2)], [('rs8', b2)], bias=64.0 * EPS)
            S.act(rs8[:, b2, :, 0], rs8[:, b2, :, 0], AF.Exp, [('rs8', b2)], [('rs8', b2)], scale=-0.5)

        def qk_E2(n):
            ci, tile = divmod(n, 8)
            isq = ci < 2
            hc = ci % 2
            gb_, gkey = (gq_b, 'gq_b') if isq else (gk_b, 'gk_b')
            bank = n % 4
            s2 = n % 2
            b2 = n % 2
            zps = PS[bank][:, 0:512]
            sqb = sqs[b2]
            znb = zns[b2] if p == 1 else zn
            znk = ('st2', 'zn', b2) if p == 1 else ('st2', 'zn')
            S.tt('dve', znb.rearrange("p (g d) -> p g d", g=8), zps.rearrange("p (g d) -> p g d", g=8),
                 rs8[:, b2, :, :].to_broadcast([128, 8, 64]), ALU.mult, [psk(bank), ('rs8', b2)], [znk])
            gbb = gb_[:].rearrange("p (o d) -> p o d", o=1).to_broadcast([128, 8, 64])
            if p == 0:
                if isq:
                    S.tt('pool', qkb[s2].rearrange("p (g d) -> p g d", g=8), zn.rearrange("p (g d) -> p g d", g=8),
                         gbb, ALU.mult, [znk, gkey], [('st2', 'qkb', s2)])
                else:
                    S.tt('pool', zg[s2].rearrange("p (g d) -> p g d", g=8), zn.rearrange("p (g d) -> p g d", g=8),
                         gbb, ALU.mult, [znk, gkey], [('st2', 'zg', s2)])
                    S.dma('sp', nk[tile * 128:(tile + 1) * 128, hc * 512:(hc + 1) * 512], zg[s2],
                          [('st2', 'zg', s2)], [], ('nk', s2))
                    S.cp('act', qkb[s2], zg[s2], [('st2', 'zg', s2)], [('st2', 'qkb', s2)])
            else:
                ti = 0 if isq else 1
                Cg, Sg = rtab[ti]
                t1 = zg[s2]
                S.tt('dve', t1.rearrange("p (g d) -> p g d", g=8), znb.rearrange("p (g d) -> p g d", g=8),
                     Cg[:, tile:tile + 1, :].to_broadcast([128, 8, 64]), ALU.mult, [znk, ('pl', 'Cg', ti)], [('st2', 'zg', s2)])
                zz = znb.rearrange("p (g a s d) -> p g a s d", g=8, a=2, s=2)
                t2 = vst[1]
                qq = t2.rearrange("p (g a s d) -> p g a s d", g=8, a=2, s=2)
                sg4 = Sg[:, tile:tile + 1, :].rearrange("p o (a s d) -> p o a s d", a=2, s=2)
                for sidx in range(2):
                    sn = sg4[:, :, :, sidx, :].to_broadcast([128, 8, 2, 16])
                    S.tt('pool', qq[:, :, :, sidx, :], zz[:, :, :, 1 - sidx, :], sn, ALU.mult,
                         [znk, ('pl', 'Sg', ti)], [('st2', 't2', sidx)])
                S.tt('dve', qkb[s2], t1, t2, ALU.add, [('st2', 'zg', s2), ('st2', 't2', 0), ('st2', 't2', 1)], [('st2', 'qkb', s2)])

        def qk_T(n):
            ci, tile = divmod(n, 8)
            isq = ci < 2
            hc = ci % 2
            s2 = n % 2
            bk = 6 + (n % 2)
            bv = PSb[bk].rearrange("p (h t) -> p h t", h=8)
            for hh in range(4):
                S.tr(bv[:, hh, :], qkb[s2][:, hh * 128:(hh + 1) * 128], ident[:], [('st2', 'qkb', s2), 'ident'], [psk(bk)])
            if isq:
                S.cp('act', qT[:, 4 * hc:4 * hc + 4, tile * 128:(tile + 1) * 128], bv[:, 0:4, :], [psk(bk)], [('qT', tile // 2)])
            else:
                S.cp('act', kT[:, 4 * hc:4 * hc + 4, tile * 128:(tile + 1) * 128], bv[:, 0:4, :], [psk(bk)], [('kT', tile)])

        NQK = 32
        for s_ in range(NQK + 3):
            if s_ < NQK:
                qk_M(s_)
            if 0 <= s_ - 1 < NQK:
                qk_E1(s_ - 1)
            if 0 <= s_ - 2 < NQK:
                qk_E2(s_ - 2)
            if 0 <= s_ - 3 < NQK:
                qk_T(s_ - 3)
        for ci in range(2):
            ua, uak, uaj = W.acquire()
            ub, ubk, ubj = W.acquire()
            for tile in range(8):
                bank = tile % 4
                for half, (u_, uk_) in enumerate(((ua, uak), (ub, ubk))):
                    for kc in range(8):
                        S.mm(PS[bank][:, half * 256:(half + 1) * 256], hT[:, kc, tile * 128:(tile + 1) * 128],
                             u_[:, kc, :], kc == 0, kc == 7, [('hT', tile // 4), uk_], [psk(bank)])
                zps = PS[bank][:, 0:512]
                S.cp('act', vtok[:, tile, 4 * ci:4 * ci + 4, 0:128], zps.rearrange("p (h e) -> p h e", h=4),
                     [psk(bank)], [('vt', tile)])
                if p == 0:
                    s2 = tile % 2
                    S.cp('dve', vst[s2], zps, [psk(bank)], [('st2', 'vst', s2)])
                    S.dma('sp', nv[tile * 128:(tile + 1) * 128, ci * 512:(ci + 1) * 512], vst[s2],
                          [('st2', 'vst', s2)], [], ('nv', s2))
            W.release(uaj)
            W.release(ubj)
        S.fence(('PL',))
        Wd = L + 16
        Pb = PL[:, 0:1088]
        Ab = PL[:, 1088:2176]
        Bb = PL[:, 2176:3264]
        pooled = PL[:, 3264:3776].bitcast(BF16)
        tmpb = PL[:, 3776:3776 + 32]
        S.memset('dve', Pb, 0.0, [('pl', 'P')])

        def v3(buf):
            return buf[:, 0:nseq * Wd].rearrange("p (s l) -> p s l", s=nseq)

        def rg(buf, a, b):
            return v3(buf)[:, :, 8 + a: 8 + b]

        for half in range(2):
            up, upk, upj = W.acquire()
            for gg in range(2):
                g = 2 * half + gg
                w_ = (2, 4, 8, 16)[g]
                hw = w_ // 2
                for blk in range(2):
                    bank = 4 + blk
                    for kc in range(8):
                        S.mm(PS[bank][:, 0:512], up[:, kc, gg * 128:(gg + 1) * 128], hT[:, kc, blk * 512:(blk + 1) * 512],
                             kc == 0, kc == 7, [('hT', blk), upk], [psk(bank)])
                    if p == 0:
                        S.cp('act', v3(Pb)[:, 2 * blk:2 * blk + 2, 8:8 + 256], PS[bank][:, 0:512].rearrange("p (s l) -> p s l", s=2),
                             [psk(bank)], [('pl', 'P')])
                    else:
                        S.cp('act', Pb[:, 8 + 512 * blk: 8 + 512 * (blk + 1)], PS[bank][:, 0:512], [psk(bank)], [('pl', 'P')])
                pk, ak, bk_ = ('pl', 'P'), ('pl', 'A'), ('pl', 'B')
                S.tt('dve', rg(Ab, -7, L + 8), rg(Pb, -8, L + 7), rg(Pb, -7, L + 8), ALU.add, [pk], [ak])
                src_, sk = Ab, ak
                if g >= 1:
                    S.tt('dve', rg(Bb, -6, L + 7), rg(Ab, -7, L + 6), rg(Ab, -5, L + 8), ALU.add, [ak], [bk_])
                    src_, sk = Bb, bk_
                if g >= 2:
                    S.tt('dve', rg(Ab, -4, L + 5), rg(Bb, -6, L + 3), rg(Bb, -2, L + 7), ALU.add, [bk_], [ak])
                    src_, sk = Ab, ak
                if g >= 3:
                    S.tt('dve', rg(Bb, 0, L), rg(Ab, -4, L - 4), rg(Ab, 4, L + 4), ALU.add, [ak], [bk_])
                    src_, sk = Bb, bk_
                pl3 = pooled.rearrange("p (s l) -> p s l", s=nseq)
                S.stt(pl3, rg(src_, 0, L), 1.0 / w_, rg(Pb, 0, L), ALU.mult, ALU.subtract, [sk, pk], [('pl', 'pooled')])
                tb = tmpb[:, 0:nseq * hw].rearrange("p (s l) -> p s l", s=nseq)
                for side in range(2):
                    lo, hi = (0, hw) if side == 0 else (L - hw, L)
                    rcv = rcb[:, g * 16 + side * 8: g * 16 + side * 8 + hw].rearrange("p (o l) -> p o l", o=1).to_broadcast([128, nseq, hw])
                    S.tt('dve', tb, rg(src_, lo, hi), rcv, ALU.mult, [sk, 'rcb'], [('pl', 'tmpb')])
                    S.tt('dve', pl3[:, :, lo:hi], tb, rg(Pb, lo, hi), ALU.subtract, [('pl', 'tmpb'), pk], [('pl', 'pooled')])
                for blk in range(2):
                    bank = 6 + blk
                    S.mm(PS[bank][:, 0:512], poolw[:, g, :], pooled[:, blk * 512:(blk + 1) * 512], True, True,
                         [('pl', 'pooled'), 'poolw'], [psk(bank)])
                    S.act(mixT[:, g, blk * 512:(blk + 1) * 512], PS[bank][:, 0:512], AF.Copy, [psk(bank), 'pscT'],
                          [('mixT', blk)], scale=pscT[:, g:g + 1])
            W.release(upj)
        if p == 0:
            dbg('qT', qT, [('qT', i) for i in range(4)])
            dbg('kT', kT, [('kT', i) for i in range(8)])
            dbg('mixT', mixT[:], [('mixT', 0), ('mixT', 1)])

        S.fence(('ACC', 'PL'))
        PT = [PLb[:, i * 512:(i + 1) * 512] for i in range(3)]
        sqo = PL[:, 768:1792]
        tO2 = [PL[:, 1792 + i * 128: 1920 + i * 128] for i in range(4)]
        onb = [PL[:, 2304 + i * 512: 2816 + i * 512].bitcast(BF16) for i in range(2)]
        ost = [[mrg_f[:, (a * 2 + b) * 1024:(a * 2 + b + 1) * 1024].rearrange("p (h e) -> p h e", h=8) for b in range(2)]
               for a in range(2)]
        items = []
        for qb in range(4):
            keytiles = [2 * qb, 2 * qb + 1] if p == 0 else list(range(12))
            for h in range(8):
                for jn, j in enumerate(keytiles):
                    items.append((qb, h, jn, j, len(keytiles)))
        pending_T = []
        o2c = [0]

        def at_A(n):
            qb, h, jn, j, nk_ = items[n]
            sbk = n % 3
            sp_ = n % 2
            for i in range(2):
                S.mm(PS[2 * sp_ + i][:, 0:256], kT[i * 64:(i + 1) * 64, h, j * 128:(j + 1) * 128],
                     qT[i * 64:(i + 1) * 64, h, qb * 256:(qb + 1) * 256], True, True,
                     [('kT', j), ('qT', qb)], [psk(2 * sp_ + i)])
            S.act(PT[sbk].rearrange("p (i q) -> p i q", i=2),
                  PSALL[:, 2 * sp_ * 512:(2 * sp_ + 2) * 512].rearrange("p (i q) -> p i q", i=2)[:, :, 0:256],
                  AF.Exp, [psk(2 * sp_), psk(2 * sp_ + 1)], [('pl', 'PT', sbk)])

        def subln(qb):
            for qt in range(2):
                o = ost[qb % 2][qt]
                ok_ = ('ost', qb % 2, qt)
                tile = qb * 2 + qt
                S.tt('dve', sqo.rearrange("p (h e) -> p h e", h=8), o, o, ALU.mult, [ok_], [('pl', 'sqo')])
                S.red(ss8c[:], sqo.rearrange("p (h e) -> p h e", h=8), ALU.add, [('pl', 'sqo')], ['ss8b'])
                S.act(rs8c[:, :, 0], ss8c[:], AF.Ln, ['ss8b'], ['rs8b'], bias=128.0 * EPS)
                S.act(rs8c[:, :, 0], rs8c[:, :, 0], AF.Exp, ['rs8b'], ['rs8b'], scale=-0.5)
                S.tt('pool', o, o, rs8c[:].to_broadcast([128, 8, 128]), ALU.mult, [ok_, 'rs8b'], [ok_])
                ob = onb[qt].rearrange("p (h e) -> p h e", h=8)
                S.tt('pool', ob, o, gsub_b[:].rearrange("p (o e) -> p o e", o=1).to_broadcast([128, 8, 128]), ALU.mult,
                     [ok_, 'gsub_b'], [('pl', 'onb', qt)])

                def T_on(qb=qb, qt=qt, tile=tile):
                    bv = PSb[0].rearrange("p (h t) -> p h t", h=8)
                    for h in range(8):
                        S.tr(bv[:, h, :], onb[qt][:, h * 128:(h + 1) * 128], ident[:], [('pl', 'onb', qt), 'ident'], [psk(0)])
                    S.cp('act', qT[:, :, tile * 128:(tile + 1) * 128], bv, [psk(0)], [('qT', qb)])
                pending_T.append(T_on)

        def at_B(n):
            qb, h, jn, j, nk_ = items[n]
            sbk = n % 3
            ab = [4 + 2 * (h % 2), 5 + 2 * (h % 2)]
            if h == 2 and jn == 0:
                while pending_T:
                    pending_T.pop(0)()
            for qt in range(2):
                for i in range(2):
                    S.mm(PS[ab[qt]][:, i * 132:i * 132 + 129], PT[sbk][:, i * 256 + qt * 128: i * 256 + (qt + 1) * 128],
                         vtok[:, j, h, 0:129], jn == 0 and i == 0, jn == nk_ - 1,
                         [('pl', 'PT', sbk), ('vt', j), ('vt', 'ones')], [psk(ab[qt])], skip=True)
        def at_C(n):
            qb, h, jn, j, nk_ = items[n]
            ab = [4 + 2 * (h % 2), 5 + 2 * (h % 2)]
            if jn == nk_ - 1:
                for qt in range(2):
                    av = PS[ab[qt]][:, 0:264].rearrange("p (i e) -> p i e", i=2)
                    sl4 = o2c[0] % 4
                    o2c[0] += 1
                    S.recip(rz[:, sl4, :, :], av[:, :, 128:129], [psk(ab[qt])], [('rz', sl4)])
                    S.ts('dve', r2n[:, sl4, :], rz[:, sl4, 1, :], neg_lam, None, ALU.mult, None, [('rz', sl4), 'neg_lam'], [('r2n', sl4)])
                    S.act(tO2[sl4], av[:, 1, 0:128], AF.Copy, [psk(ab[qt]), ('r2n', sl4)], [('pl', 'tO2', sl4)], scale=r2n[:, sl4, :])
                    S.stt(ost[qb % 2][qt][:, h, :], av[:, 0, 0:128], rz[:, sl4, 0, :], tO2[sl4], ALU.mult, ALU.add,
                          [psk(ab[qt]), ('rz', sl4), ('pl', 'tO2', sl4)], [('ost', qb % 2, qt)])
                if h == 7:
                    subln(qb)

        NI = len(items)
        CL = 2 if p == 0 else 3
        for s_ in range(NI + CL):
            if s_ < NI:
                at_A(s_)
            if 0 <= s_ - 1 < NI:
                at_B(s_ - 1)
            if 0 <= s_ - CL < NI:
                at_C(s_ - CL)
        while pending_T:
            pending_T.pop(0)()
        if p == 0:
            dbg('onT', qT, [('qT', i) for i in range(4)])

        S.fence(('ACC', 'PL'))
        sg0 = [PLb[:, i * 512:(i + 1) * 512] for i in range(2)]
        sg1 = [PLb[:, 1024 + i * 512: 1536 + i * 512] for i in range(2)]
        t0 = PL[:, 1024:1536]
        t1 = PL[:, 1536:2048]
        wtmp = [PL[:, 2048 + i * 512: 2560 + i * 512] for i in range(2)]
        it = 0
        for ng in range(4):
            ug0, ug0k, ug0j = W.acquire()
            ug1, ug1k, ug1j = W.acquire()
            ua, uak, uaj = W.acquire()
            up, upk, upj = W.acquire()
            for blk in range(2):
                tsl = slice(blk * 512, (blk + 1) * 512)
                for cc in range(2):
                    c = 2 * ng + cc
                    b0 = (it % 2) * 4
                    s2 = it % 2
                    it += 1
                    csl = slice(cc * 128, (cc + 1) * 128)
                    for kc in range(8):
                        S.mm(PS[b0][:, 0:512], ug0[:, kc, csl], hT[:, kc, tsl], kc == 0, kc == 7, [('hT', blk), ug0k], [psk(b0)])
                    for kc in range(8):
                        S.mm(PS[b0 + 1][:, 0:512], ug1[:, kc, csl], hT[:, kc, tsl], kc == 0, kc == 7, [('hT', blk), ug1k], [psk(b0 + 1)])
                    for kc in range(8):
                        S.mm(PS[b0 + 2][:, 0:512], ua[:, kc, csl], qT[:, kc, tsl], kc == 0, kc == 7,
                             [('qT', 2 * blk), ('qT', 2 * blk + 1), uak], [psk(b0 + 2)])
                    for fc in range(4):
                        S.mm(PS[b0 + 3][:, 0:512], up[:, fc, csl], mixT[:, fc, tsl], fc == 0, fc == 3, [('mixT', blk), upk], [psk(b0 + 3)])
                    S.act(sg0[s2], PS[b0][:, 0:512], AF.Sigmoid, [psk(b0), 'bgT'], [('pl', 'sg0', s2)], bias=bgT[:, c:c + 1])
                    S.act(sg1[s2], PS[b0 + 1][:, 0:512], AF.Sigmoid, [psk(b0 + 1), 'bgT'], [('pl', 'sg1', s2)], bias=bgT[:, 8 + c:9 + c])
                    S.tt('dve', t0, PS[b0 + 2][:, 0:512], sg0[s2], ALU.mult, [psk(b0 + 2), ('pl', 'sg0', s2)], [('pl', 't0')])
                    S.tt('dve', t1, PS[b0 + 3][:, 0:512], sg1[s2], ALU.mult, [psk(b0 + 3), ('pl', 'sg1', s2)], [('pl', 't1')])
                    S.tt('pool', mrgT[:, c, tsl], t0, t1, ALU.add, [('pl', 't0'), ('pl', 't1')], [('mrgT', blk)])
            for j_ in (ug0j, ug1j, uaj, upj):
                W.release(j_)
        if p == 0:
            dbg('mrgT', mrgT, [('mrgT', 0), ('mrgT', 1)])
        S.fence(('X1',))
        uo = [W.acquire() for _ in range(4)]
        it = 0
        for tile in range(8):
            sl = tile % 2
            xkey = ('xst', sl)
            S.dma('sp', xst[sl][:], x[p * 1024 + tile * 128: p * 1024 + (tile + 1) * 128, :], (), [xkey], xkey)
            for nh in range(2):
                bank = it % 4
                s2 = it % 2
                it += 1
                for j2 in range(2):
                    u_, uk_, _ = uo[nh * 2 + j2]
                    for kc in range(8):
                        S.mm(PS[bank][:, j2 * 256:(j2 + 1) * 256], mrgT[:, kc, tile * 128:(tile + 1) * 128], u_[:, kc, :],
                             kc == 0, kc == 7, [('mrgT', tile // 4), uk_], [psk(bank)])
                nsl = slice(nh * 512, (nh + 1) * 512)
                S.tt('dve', wtmp[s2], PS[bank][:, 0:512], g1b[:, nsl], ALU.mult, [psk(bank), 'g1b'], [('pl', 'wtmp', s2)])
                S.tt('pool', x1[:, tile, nsl], wtmp[s2], xst[sl][:, nsl], ALU.add, [('pl', 'wtmp', s2), xkey], [('x1', tile)])
        for (_, _, j_) in uo:
            W.release(j_)
        if p == 0:
            dbg('x1', x1[:, 0, :], [('x1', 0)])

        S.fence(('PL',))
        S.dma('sp', g1b[:], modrows[p, 0:1, :].to_broadcast([128, 1024]), [('modrows', p)], ['g1b'], 'g1b')
        S.dma('sp', xst[0][:], modrows[p, 1:2, :].to_broadcast([128, 1024]), [('modrows', p)], [('xst', 0)], ('xst', 0))
        norm_T(p, 'x1', (a2T, 'a2T'), sh2, 'hT')
        S.fence(('PL', 'ACC', 'K'))
        lgp = PS[7][:, 0:256].rearrange("p (t n) -> p t n", t=8)
        for tile in range(8):
            for kc in range(8):
                S.mm(lgp[:, tile, 0:20], hT[:, kc, tile * 128:(tile + 1) * 128], rw[:, kc, :], kc == 0, kc == 7,
                     [('hT', tile // 4), 'rw'], [psk(7)])
        R_ = PL[:, 0:2560]

        def rbuf(i, n):
            return R_[:, i * 160: i * 160 + 8 * n].rearrange("p (t n) -> p t n", t=8)

        lg = rbuf(0, 20)
        S.tt('dve', lg, lgp[:, :, 0:20], rb_b[:].rearrange("p (o n) -> p o n", o=1).to_broadcast([128, 8, 20]), ALU.add,
             [psk(7), 'rb_b'], [('pl', 'lg')])
        gl = lg[:, :, 0:4]
        el = lg[:, :, 4:20]
        gmax = rbuf(1, 1)
        S.red(gmax[:, :, 0], gl, ALU.max, [('pl', 'lg')], [('pl', 'gmax')])
        ge = rbuf(2, 4)
        S.tt('dve', ge, gl, gmax.to_broadcast([128, 8, 4]), ALU.subtract, [('pl', 'lg'), ('pl', 'gmax')], [('pl', 'ge')])
        eg = rbuf(3, 4)
        S.act(eg, ge, AF.Exp, [('pl', 'ge')], [('pl', 'eg')])
        gsum = rbuf(4, 1)
        S.red(gsum[:, :, 0], eg, ALU.add, [('pl', 'eg')], [('pl', 'gsum')])
        gw = rbuf(5, 1)
        S.recip(gw, gsum, [('pl', 'gsum')], [('pl', 'gw')])
        pen = rbuf(6, 4)
        S.ts('dve', pen, ge, 0.0, None, ALU.is_ge, None, [('pl', 'ge')], [('pl', 'pen')])
        S.ts('dve', pen, pen, -1.0, BIG, ALU.add, ALU.mult, [('pl', 'pen')], [('pl', 'pen')])
        msk = rbuf(7, 16)
        pen4 = R_[:, 6 * 160: 6 * 160 + 32].rearrange("p (t g o) -> p t g o", t=8, o=1).to_broadcast([128, 8, 4, 4])
        S.tt('dve', msk.rearrange("p t (g e) -> p t g e", g=4), el.rearrange("p t (g e) -> p t g e", g=4), pen4, ALU.add,
             [('pl', 'lg'), ('pl', 'pen')], [('pl', 'msk')])
        m1 = rbuf(8, 1)
        S.red(m1[:, :, 0], msk, ALU.max, [('pl', 'msk')], [('pl', 'm1')])
        o1 = rbuf(9, 16)
        S.tt('dve', o1, msk, m1.to_broadcast([128, 8, 16]), ALU.subtract, [('pl', 'msk'), ('pl', 'm1')], [('pl', 'o1')])
        S.ts('dve', o1, o1, 0.0, None, ALU.is_ge, None, [('pl', 'o1')], [('pl', 'o1')])
        msk2 = rbuf(10, 16)
        S.stt(msk2, o1, -BIG, msk, ALU.mult, ALU.add, [('pl', 'o1'), ('pl', 'msk')], [('pl', 'msk2')])
        m2 = rbuf(11, 1)
        S.red(m2[:, :, 0], msk2, ALU.max, [('pl', 'msk2')], [('pl', 'm2')])
        o2 = rbuf(12, 16)
        S.tt('dve', o2, msk2, m2.to_broadcast([128, 8, 16]), ALU.subtract, [('pl', 'msk2'), ('pl', 'm2')], [('pl', 'o2')])
        S.ts('dve', o2, o2, 0.0, None, ALU.is_ge, None, [('pl', 'o2')], [('pl', 'o2')])
        e21 = rbuf(4, 1)
        S.tt('dve', e21, m2, m1, ALU.subtract, [('pl', 'm2'), ('pl', 'm1'), ('pl', 'gw')], [('pl', 'gsum')])
        S.act(e21, e21, AF.Exp, [('pl', 'gsum')], [('pl', 'gsum')])
        den = rbuf(1, 1)
        S.ts('dve', den, e21, 1.0, None, ALU.add, None, [('pl', 'gsum'), ('pl', 'ge')], [('pl', 'gmax')])
        S.recip(den, den, [('pl', 'gmax')], [('pl', 'gmax')])
        w1 = rbuf(2, 1)
        S.tt('dve', w1, den, gw, ALU.mult, [('pl', 'gmax'), ('pl', 'gw'), ('pl', 'pen'), ('pl', 'eg')], [('pl', 'ge')])
        w2 = rbuf(3, 1)
        S.tt('dve', w2, w1, e21, ALU.mult, [('pl', 'ge'), ('pl', 'gsum')], [('pl', 'eg')])
        tsl = slice(p * 8, (p + 1) * 8)
        S.cp('pool', O1g[:, tsl, :], o1, [('pl', 'o1')], [('O1g', p)])
        S.cp('pool', O2g[:, tsl, :], o2, [('pl', 'o2')], [('O2g', p)])
        S.tt('dve', Mb[:, tsl, :], o1, o2, ALU.add, [('pl', 'o1'), ('pl', 'o2')], [('Mb', p)])
        S.cp('dve', W1g[:, tsl, :], w1, [('pl', 'ge')], [('W1g', p)])
        S.cp('dve', W2g[:, tsl, :], w2, [('pl', 'eg')], [('W2g', p)])
        if p == 0:
            dbg('gates', O1g[:, 0, :], [('O1g', 0)])
            dbg('h2T', hT[:], [('hT', 0), ('hT', 1)])

    def routing():
        S.fence(('PL',))
        rankp = PS[0][:, 0:256].rearrange("p (t e) -> p t e", t=16)
        cntp = PS[1][:, 0:16]
        mk = [('Mb', 0), ('Mb', 1)]
        for T in range(16):
            S.mm(rankp[:, T, :], trib[:], Mb[:, T, :], True, T == 0, mk + ['trib'], [psk(0)])
            for T2 in range(T):
                S.mm(rankp[:, T, :], onesb[:], Mb[:, T2, :], False, T2 == T - 1, mk + ['onesb'], [psk(0)])
        for T in range(16):
            S.mm(cntp, onesb[:], Mb[:, T, :], T == 0, T == 15, mk + ['onesb'], [psk(1)])
        R_ = PL[:, 0:4096]
        off = [0]
        nbuf = [0]

        def ra(n):
            v = R_[:, off[0]:off[0] + n]
            k = ('pl', 'r', nbuf[0])
            off[0] += n
            nbuf[0] += 1
            assert off[0] <= 4096
            return v, k

        def b3(ap2, shape):
            return ap2.rearrange("p (o n) -> p o n", o=1).to_broadcast(shape)

        def l3(ap2, shape):
            return ap2.rearrange("p (n o) -> p n o", o=1).to_broadcast(shape)

        cnt, kcnt = ra(16)
        S.cp('dve', cnt, cntp, [psk(1)], [kcnt])
        cmp, kcmp = ra(256)
        cmp3 = cmp.rearrange("p (e k) -> p e k", e=16)
        S.tt('dve', cmp3, l3(cnt, [128, 16, 16]), b3(thr[:], [128, 16, 16]), ALU.is_gt, [kcnt, 'thr'], [kcmp])
        ntl, kntl = ra(16)
        S.red(ntl, cmp3, ALU.add, [kcmp], [kntl])
        thr2 = rtab[:, 0:13]
        eidx = rtab[:, 16:32]
        ocmp, kocmp = ra(208)
        ocmp3 = ocmp.rearrange("p (e q) -> p e q", e=16)
        S.tt('dve', ocmp3, l3(cnt, [128, 16, 13]), b3(thr2, [128, 16, 13]), ALU.is_gt, [kcnt, 'rtab'], [kocmp])
        ovt, kovt = ra(16)
        S.red(ovt, ocmp3, ALU.add, [kocmp], [kovt])
        ones16, kones16 = ra(16)
        S.memset('dve', ones16, 1.0, [kones16])
        ovincl, kovincl = ra(16)
        S.add('dve', lambda e: e.tensor_tensor_scan(ovincl, ones16, ovt, 0.0, ALU.mult, ALU.add), [kones16, kovt], [kovincl])
        ovb, kovb = ra(16)
        S.tt('dve', ovb, ovincl, ovt, ALU.subtract, [kovincl, kovt], [kovb])
        prod, kprod = ra(256)
        prod3 = prod.rearrange("p (t e) -> p t e", t=16)
        sf, ksf = ra(32)
        sf3 = sf.rearrange("p (k t) -> p k t", k=2)
        for k, (Og, okey) in enumerate(((O1g, 'O1g'), (O2g, 'O2g'))):
            ok2 = [(okey, 0), (okey, 1)]
            rk, krk = ra(16)
            S.tt('dve', prod3, Og[:], rankp, ALU.mult, ok2 + [psk(0)], [kprod])
            S.red(rk, prod3, ALU.add, [kprod], [krk])
            Ek, kE = ra(16)
            S.tt('dve', prod3, Og[:], b3(eidx, [128, 16, 16]), ALU.mult, ok2 + ['rtab'], [kprod])
            S.red(Ek, prod3, ALU.add, [kprod], [kE])
            OBk, kOB = ra(16)
            S.tt('dve', prod3, Og[:], b3(ovb, [128, 16, 16]), ALU.mult, ok2 + [kovb], [kprod])
            S.red(OBk, prod3, ALU.add, [kprod], [kOB])
            mainp, kmain = ra(16)
            S.stt(mainp, Ek, 384.0, rk, ALU.mult, ALU.add, [kE, krk], [kmain])
            tailp, ktail = ra(16)
            S.stt(tailp, OBk, 256.0, rk, ALU.mult, ALU.add, [kOB, krk], [ktail])
            S.ts('dve', tailp, tailp, float(NMAIN * 128 - 384), None, ALU.add, None, [ktail], [ktail])
            S.tt('dve', tailp, tailp, mainp, ALU.subtract, [ktail, kmain], [ktail])
            isov, kisov = ra(16)
            S.ts('dve', isov, rk, 384.0, None, ALU.is_ge, None, [krk], [kisov])
            S.tt('dve', tailp, tailp, isov, ALU.mult, [ktail, kisov], [ktail])
            S.tt('dve', sf3[:, k, :], mainp, tailp, ALU.add, [kmain, ktail], [(ksf, k)])
        S.cp('dve', s12i[:], sf3, [(ksf, 0), (ksf, 1)], ['s12i'])
        cmp2, kcmp2 = ra(28 * 16)
        cmp23 = cmp2.rearrange("p (t e) -> p t e", t=28)
        S.tt('dve', cmp23, b3(ovincl, [128, 28, 16]), l3(tval[:, 0:28], [128, 28, 16]), ALU.is_le, [kovincl, 'tval'], [kcmp2])
        etf, ketf = ra(28)
        S.red(etf, cmp23, ALU.add, [kcmp2], [ketf])
        skp, kskp = ra(28)
        S.memset('dve', skp[:, 0:2], 0.0, [kskp])
        S.tt('dve', skp[:, 2:28], etf[:, 2:28], etf[:, 0:26], ALU.is_equal, [ketf], [kskp])
        wifb, kwif = ra(56)
        wif = wifb.rearrange("p (t h) -> p t h", h=2)
        S.ts('dve', wif[:, :, 0], etf, 256.0, p2col[:, 0:1], ALU.mult, ALU.add, [ketf, 'p2col'], [kwif])
        S.stt(wif[:, :, 0], skp, 65536.0, wif[:, :, 0], ALU.mult, ALU.add, [kskp, kwif], [kwif])
        S.ts('dve', wif[:, :, 1], wif[:, :, 0], 1.0, None, ALU.add, None, [kwif], [kwif])
        S.cp('dve', widx[:], wif, [kwif], ['widx'])
        zt = mixT[:].rearrange("p g t -> p (g t)").bitcast(I32)[:, 0:1280]
        mk2 = [('mixT', 0), ('mixT', 1)]
        S.memset('dve', zt, 0, mk2)
        S.dma('sp', slot_tok.rearrange("(p r) c -> p (r c)", p=128), zt, mk2, ['stok0'], 'stok0')
        for T in range(16):
            for k in range(2):
                S.add('pool', (lambda e, T=T, k=k: e.indirect_dma_start(
                    out=slot_tok[:, :], out_offset=bass.IndirectOffsetOnAxis(ap=s12i[:, k, T:T + 1].bitcast(U32), axis=0),
                    in_=tok16[:, T, :], in_offset=None)),
                    ['stok0', 's12i', 'tok16'], [('stok', T, k)], dma='sct')

    X1b_ = X1[:].bitcast(BF16)
    EW = [[X1b_[:, m * 4096:(m + 1) * 4096] for m in range(3)],
          [accb[:, m * 4096:(m + 1) * 4096] for m in range(3)]]
    wmats = (ew_gate, ew_up, ew_down)

    def L_static(e, mats=range(3)):
        b = e % 2
        for m in mats:
            src = wmats[m][e].rearrange("k n -> (k n)").rearrange("(p c) -> p c", p=128)
            for h in range(2):
                S.dma('pool', EW[b][m][:, h * 2048:(h + 1) * 2048], src[:, h * 2048:(h + 1) * 2048], (),
                      [('ew%d' % b, m)], ('ew', b, m, h))

    def moe_prefetch():
        S.fence(('X1', 'ACC'))
        L_static(0)
        L_static(1)

    def moe_sparse():
        S.fence(('K', 'H', 'PL'))
        NG = 4
        G = [KR[:, i * 1024:(i + 1) * 1024] for i in range(NG)]
        hsT = [KR[:, 4096 + i * 1024: 5120 + i * 1024].rearrange("p (k s) -> p k s", k=8) for i in range(2)]
        sil = [KR[:, 6144 + i * 512: 6656 + i * 512] for i in range(2)]
        hidb = [KR[:, 7168 + i * 512: 7680 + i * 512] for i in range(2)]
        hidT = [KR[:, 8192 + i * 512: 8704 + i * 512].rearrange("p (k s) -> p k s", k=4) for i in range(2)]
        hTf = hT[:].rearrange("p c t -> p (c t)").bitcast(F32)
        Yst = [hTf[:, i * 1024:(i + 1) * 1024] for i in range(2)]
        wsrc = [w_.rearrange("e k n -> (e k n)").rearrange("(r c) -> r c", c=2048) for w_ in wmats]
        stok_keys = [('stok', T, k) for T in range(16) for k in range(2)]
        h2keys = [('h2d', p_, t_) for p_ in range(2) for t_ in range(8)]
        GCH = 8
        for c0 in range(0, NTS, GCH):
            c1 = min(NTS, c0 + GCH)
            S.dma('sp', gidx[:, c0:c1], slot_tok[c0 * 128:c1 * 128, 0:1].rearrange("(t p) c -> p (t c)", p=128),
                  stok_keys + ['p2col'], [('gidx', c0 // GCH)], ('gidx', c0 // GCH), slow=True)
        bcreg = {}

        def wbuf(t):
            return (t // 3) % 2 if t < NMAIN else ((t - NMAIN) // 2) % 2

        def wload(e, i, m, h, b):
            if 'r' not in bcreg:
                bcreg['r'] = e.alloc_register("wbound")
                e.reg_mov(bcreg['r'], 4095)
            return e.indirect_dma_start(
                out=EW[b][m][:, h * 2048:(h + 1) * 2048], out_offset=None, in_=wsrc[m],
                in_offset=bass.IndirectOffsetOnAxis(ap=widx[:, i, h:h + 1].bitcast(U32), axis=0),
                bounds_check=bcreg['r'], oob_is_err=False)

        def L_dyn(i, mats=range(3)):
            b = i % 2
            for m in mats:
                for h in range(2):
                    S.add('pool', (lambda e, i=i, m=m, h=h, b=b: wload(e, i, m, h, b)),
                          ['widx'], [('ew%d' % b, m)], dma=('ew', b, m, h))

        def L_g(t):
            b = t % NG
            S.add('pool', (lambda e, t=t, b=b: e.indirect_dma_start(
                out=G[b], out_offset=None, in_=h2d[:, :],
                in_offset=bass.IndirectOffsetOnAxis(ap=gidx[:, t:t + 1].bitcast(U32), axis=0))),
                [('gidx', t // GCH)] + h2keys, [('mg', 'G', b)], dma=('G', b))

        def T1(t):
            b = t % 2
            gb = t % NG
            bv = PSb[b].rearrange("p (k s) -> p k s", k=8)
            g3 = G[gb].rearrange("s (p k) -> s k p", k=8)
            for kc in range(8):
                S.tr(bv[:, kc, :], g3[:, kc, :], ident[:], [('mg', 'G', gb), 'ident'], [psk(b)])
            S.cp('act', hsT[b], bv, [psk(b)], [('mg', 'hsT', b)])

        def A_(t):
            b = t % 2
            wb = wbuf(t)
            for kc in range(8):
                S.mm(PS[2][:, 0:512], hsT[b][:, kc, :], EW[wb][0][:, kc * 512:(kc + 1) * 512], kc == 0, kc == 7,
                     [('mg', 'hsT', b), ('ew%d' % wb, 0)], [psk(2)])
            for kc in range(8):
                S.mm(PS[3][:, 0:512], hsT[b][:, kc, :], EW[wb][1][:, kc * 512:(kc + 1) * 512], kc == 0, kc == 7,
                     [('mg', 'hsT', b), ('ew%d' % wb, 1)], [psk(3)])
            S.act(sil[b], PS[2][:, 0:512], AF.Silu, [psk(2)], [('mg', 'sil', b)])
            S.tt('dve', hidb[b], PS[3][:, 0:512], sil[b], ALU.mult, [psk(3), ('mg', 'sil', b)], [('mg', 'hid', b)])

        def T2D(t):
            b = t % 2
            wb = wbuf(t)
            bv = PSb[4].rearrange("p (k s) -> p k s", k=8)
            h3 = hidb[b].rearrange("s (p k) -> s k p", k=4)
            for fc in range(4):
                S.tr(bv[:, fc, :], h3[:, fc, :], ident[:], [('mg', 'hid', b), 'ident'], [psk(4)])
            S.cp('act', hidT[b], bv[:, 0:4, :], [psk(4)], [('mg', 'hidT', b)])
            for nh in range(2):
                for fc in range(4):
                    S.mm(PS[5 + nh][:, 0:512], hidT[b][:, fc, :], EW[wb][2][:, fc * 1024 + nh * 512:fc * 1024 + (nh + 1) * 512],
                         fc == 0, fc == 3, [('mg', 'hidT', b), ('ew%d' % wb, 2)], [psk(5 + nh)])
            S.cp('act', Yst[b][:, 0:512], PS[5][:, 0:512], [psk(5)], [('yst', b, 0)])
            S.cp('dve', Yst[b][:, 512:1024], PS[6][:, 0:512], [psk(6)], [('yst', b, 1)])
            S.dma('sp', yslot[t * 128:(t + 1) * 128, :], Yst[b], [('yst', b, 0), ('yst', b, 1)], [('ysl', t)], ('yso', b))

        def loads_after_A(t):
            if t < NMAIN:
                e, j = divmod(t, 3)
                if j == 2 and e + 2 < 16:
                    L_static(e + 2, (0, 1))
                if j == 2 and e + 2 >= 16:
                    L_dyn(e + 2 - 16, (0, 1))
            else:
                i, j = divmod(t - NMAIN, 2)
                if j == 1 and i + 2 < NTB:
                    L_dyn(i + 2, (0, 1))

        def loads_after_D(t):
            if t < NMAIN:
                e, j = divmod(t, 3)
                if j == 2 and e + 2 < 16:
                    L_static(e + 2, (2,))
                if j == 2 and e + 2 >= 16:
                    L_dyn(e + 2 - 16, (2,))
            else:
                i, j = divmod(t - NMAIN, 2)
                if j == 1 and i + 2 < NTB:
                    L_dyn(i + 2, (2,))

        for t in range(3):
            L_g(t)
        T1(0)
        for s_ in range(NTS + 1):
            if s_ + 3 < NTS:
                L_g(s_ + 3)
            if s_ + 1 < NTS:
                T1(s_ + 1)
            if s_ < NTS:
                A_(s_)
                loads_after_A(s_)
            if 0 <= s_ - 1 < NTS:
                T2D(s_ - 1)
                loads_after_D(s_ - 1)

    def final(p):
        S.fence(('X1', 'PL'))
        S.dma('sp', g2b[:], g2row[p:p + 1, :].to_broadcast([128, 1024]), [('g2row', p)], ['g2b'], 'g2b')
        FR = [[X1[:, (k * 2 + i) * 1024:(k * 2 + i + 1) * 1024] for i in range(2)] for k in range(2)]
        ysl_keys = [('ysl', t) for t in range(NTS)]
        for tile in range(8):
            T = p * 8 + tile
            sl = tile % 2
            xkey = ('xst', sl)
            S.dma('sp', xst[sl][:], x1d[p * 1024 + tile * 128: p * 1024 + (tile + 1) * 128, :],
                  [('x1d', p, t_) for t_ in range(8)], [xkey], xkey)
            for k in range(2):
                S.add('pool', (lambda e, k=k, T=T, sl=sl: e.indirect_dma_start(
                    out=FR[k][sl], out_offset=None, in_=yslot[:, :],
                    in_offset=bass.IndirectOffsetOnAxis(ap=s12i[:, k, T:T + 1].bitcast(U32), axis=0))),
                    ['s12i'] + ysl_keys, [('fr', k, sl)], dma=('fr', k, sl))
            r1, r2 = FR[0][sl], FR[1][sl]
            S.act(r1, r1, AF.Copy, [('fr', 0, sl), ('W1g', p)], [('fr', 0, sl)], scale=W1g[:, T, :])
            S.stt(r1, r2, W2g[:, T, :], r1, ALU.mult, ALU.add, [('fr', 1, sl), ('fr', 0, sl), ('W2g', p)], [('fr', 0, sl)])
            S.tt('dve', r1, r1, g2b[:], ALU.mult, [('fr', 0, sl), 'g2b'], [('fr', 0, sl)])
            S.tt('dve', r1, r1, xst[sl][:], ALU.add, [('fr', 0, sl), xkey], [('fr', 0, sl)])
            S.dma('sp', y[p * 1024 + tile * 128: p * 1024 + (tile + 1) * 128, :], r1, [('fr', 0, sl)], [], ('yo', sl))

    run_pass(0)
    run_pass(1)
    moe_prefetch()
    routing()
    moe_sparse()
    final(0)
    final(1)
    S.emit()
    return nc, S


_CACHE = {}


def _consts():
    rows = 1024 // 64
    row_ids = np.repeat(np.arange(rows, dtype=np.float32), 64)
    col_ids = np.tile(np.arange(64, dtype=np.float32), rows)
    inv_freq = np.power(np.float32(10000.0), -np.arange(16, dtype=np.float32) / np.float32(16)).astype(np.float32)
    ang_r = row_ids[:, None] * inv_freq[None, :]
    ang_c = col_ids[:, None] * inv_freq[None, :]
    ang = np.concatenate([ang_r, ang_r, ang_c, ang_c], axis=-1).astype(np.float32)
    cos = np.cos(ang).astype(np.float32)
    sin = np.sin(ang).astype(np.float32)
    sgn = np.concatenate([-np.ones(16), np.ones(16), -np.ones(16), np.ones(16)]).astype(np.float32)
    sin_f = (sin * sgn[None, :]).astype(np.float32)
    rcb = np.zeros((1, 64), np.float32)
    for g, w in enumerate((2, 4, 8, 16)):
        hw = w // 2
        for t in range(hw):
            rcb[0, g * 16 + t] = 1.0 / (t + hw)
            rcb[0, g * 16 + 8 + t] = 1.0 / (w - t)
    return cos, sin_f, rcb, np.eye(128, dtype=np.float32)


def kernel(x_prompt, x_sample, c, cache_k, cache_v, c_ctx, w_ada, b_ada, norm1_g, w_in, b_gate,
           q_norm_g, k_norm_g, lambda_q1, lambda_k1, lambda_q2, lambda_k2, subln_g, pool_w, pool_scale,
           w_br_attn, w_br_pool, w_out, norm2_g, router_group_w, router_group_b, router_expert_w,
           router_expert_b, expert_w_gate, expert_w_up, expert_w_down, _dump=None):
    f = lambda a: np.ascontiguousarray(np.asarray(a, dtype=np.float32))
    key = tuple(_dump) if _dump else None
    if key not in _CACHE:
        _CACHE[key] = build_program(_dump)
    nc, S = _CACHE[key]
    cos, sin_f, rcb, eye = _consts()
    x_prompt = f(x_prompt); x_sample = f(x_sample); c = f(c); c_ctx = f(c_ctx)
    cache_k = f(cache_k); cache_v = f(cache_v)
    shared = {
        "w_ada": f(w_ada)[0], "b_ada": f(b_ada), "norm1_g": f(norm1_g), "w_in": f(w_in)[0], "b_gate": f(b_gate),
        "q_norm_g": f(q_norm_g), "k_norm_g": f(k_norm_g),
        "lam4": np.concatenate([f(lambda_q1), f(lambda_k1), f(lambda_q2), f(lambda_k2)], axis=1),
        "subln_g": f(subln_g), "pool_w": f(pool_w)[0], "pool_scale": f(pool_scale),
        "w_br_attn": f(w_br_attn)[0], "w_br_pool": f(w_br_pool)[0], "w_out": f(w_out)[0], "norm2_g": f(norm2_g),
        "router_w": np.concatenate([f(router_group_w)[0], f(router_expert_w)[0]], axis=1),
        "router_b": np.concatenate([f(router_group_b), f(router_expert_b)], axis=1),
        "ew_gate": f(expert_w_gate)[0], "ew_up": f(expert_w_up)[0], "ew_down": f(expert_w_down)[0],
        "ident": eye, "cos_t": cos, "sin_t": sin_f, "rcb": rcb,
        "tri": np.triu(np.ones((128, 128), np.float32), k=1),
        "thr": (128.0 * np.arange(16, dtype=np.float32)).reshape(1, 16),
        "tval": np.arange(48, dtype=np.float32).reshape(1, 48),
        "rtab": np.concatenate([384.0 + 256.0 * np.arange(7, dtype=np.float32), np.full(6, 1.0e9, np.float32), np.zeros(3, np.float32),
                                np.arange(16, dtype=np.float32), np.zeros(32, np.float32)]).reshape(1, 64),
    }
    in_maps = []
    for i in range(NCORES):
        m = dict(shared)
        m["x"] = np.concatenate([x_prompt[4 * i:4 * i + 4].reshape(1024, 1024), x_sample[i]], axis=0)
        m["cond"] = np.stack([c_ctx, c[i]], axis=0)
        m["ck"] = cache_k[i, 0].reshape(512, 1024)
        m["cv"] = cache_v[i, 0].reshape(512, 1024)
        in_maps.append(m)
    res = run_bass_kernel_spmd(nc, in_maps, core_ids=list(range(NCORES)))
    R = res.results
    y_prompt = np.concatenate([R[i]["y"][0:1024].reshape(4, 256, 1024) for i in range(NCORES)], axis=0)
    y_sample = np.stack([R[i]["y"][1024:2048] for i in range(NCORES)], axis=0)
    new_k = np.concatenate([R[i]["nk"].reshape(4, 1, 256, 8, 128) for i in range(NCORES)], axis=0)
    new_v = np.concatenate([R[i]["nv"].reshape(4, 1, 256, 8, 128) for i in range(NCORES)], axis=0)
    if _dump:
        kernel.last_dumps = [{nm: R[i]["dbg_" + nm] for nm, _ in _dump} for i in range(NCORES)]
    return (y_prompt.astype(np.float32), y_sample.astype(np.float32), new_k.astype(np.float32), new_v.astype(np.float32))
```

```python
import numpy as np
import concourse.bass as bass
import concourse.mybir as mybir
from concourse.bass_utils import run_bass_kernel_spmd

F32 = mybir.dt.float32
BF16 = mybir.dt.bfloat16
I32 = mybir.dt.int32
U32 = mybir.dt.uint32
AF = mybir.ActivationFunctionType
ALU = mybir.AluOpType
AX = mybir.AxisListType

EPS = 1e-6
NCORES = 8
LAMBDA_INIT = 0.8 - 0.6 * 1.0
BIG = 1.0e9

REGION_OF = {
    'vt': 'X1', 'x1': 'X1',
    'qT': 'ACC', 'mrgT': 'ACC', 'acc': 'ACC', 'st2': 'ACC', 'ost': 'ACC',
    'kT': 'K', 'silu': 'K', 'hid': 'K',
    'pl': 'PL',
    'ew0': 'X1', 'fr': 'X1', 'ew1': 'ACC', 'mg': 'K', 'hT': 'H', 'yst': 'H',
}


class Sched:
    def __init__(self, nc, scratch):
        self.nc = nc
        self.ops = []
        self.lastw = {}
        self.readers = {}
        self.scratch = scratch
        self.seen = {}

    def add(self, eng, fn, reads=(), writes=(), dma=None):
        writes = list(writes) + [k for k in reads if isinstance(k, tuple) and k[0] == 'ps' and k not in writes]
        reads = [k for k in reads if not (isinstance(k, tuple) and k[0] == 'ps')]
        for k in reads + writes:
            nm = k[0] if isinstance(k, tuple) else k
            rg = REGION_OF.get(nm)
            if rg is not None:
                self.seen.setdefault(rg, set()).add(k)
                fk = ('fence', rg)
                if fk not in reads:
                    reads.append(fk)
        idx = len(self.ops)
        deps = set()
        for k in reads:
            w = self.lastw.get(k)
            if w is not None:
                deps.add(w)
        for k in writes:
            w = self.lastw.get(k)
            if w is not None:
                deps.add(w)
            for r in self.readers.get(k, ()):
                deps.add(r)
        deps.discard(idx)
        self.ops.append(dict(eng=eng, fn=fn, deps=deps, dma=dma))
        for k in reads:
            self.readers.setdefault(k, []).append(idx)
        for k in writes:
            self.lastw[k] = idx
            self.readers[k] = []
        return idx

    def fence(self, regions=('X1', 'ACC', 'K', 'PL', 'H')):
        for rg in regions:
            keys = list(self.seen.get(rg, ())) + [('fence', rg), 'fscr']
            sc = self.scratch
            idx = len(self.ops)
            deps = set()
            for k in keys:
                w = self.lastw.get(k)
                if w is not None:
                    deps.add(w)
                for r in self.readers.get(k, ()):
                    deps.add(r)
            self.ops.append(dict(eng='dve', fn=(lambda e: e.memset(sc, 0.0)), deps=deps, dma=None))
            for k in keys:
                self.lastw[k] = idx
                self.readers[k] = []

    def mm(self, out, lhsT, rhs, start, stop, reads, writes, skip=False):
        if skip:
            return self.add('pe', lambda e: e.matmul(out, lhsT, rhs, start=start, stop=stop, skip_group_check=True),
                            reads, writes)
        return self.add('pe', lambda e: e.matmul(out, lhsT, rhs, start=start, stop=stop), reads, writes)

    def tr(self, out, in_, ident, reads, writes):
        return self.add('pe', lambda e: e.transpose(out, in_, ident), reads, writes)

    def act(self, out, in_, func, reads, writes, bias=None, scale=None, accum_out=None):
        kw = {}
        if bias is not None:
            kw['bias'] = bias
        if scale is not None:
            kw['scale'] = scale
        if accum_out is not None:
            kw['accum_out'] = accum_out
        return self.add('act', lambda e: e.activation(out, in_, func, **kw), reads, writes)

    def tt(self, eng, out, in0, in1, op, reads, writes):
        return self.add(eng, lambda e: e.tensor_tensor(out, in0, in1, op), reads, writes)

    def ts(self, eng, out, in0, s1, s2, op0, op1, reads, writes):
        if op1 is None:
            return self.add(eng, lambda e: e.tensor_scalar(out, in0, s1, None, op0), reads, writes)
        return self.add(eng, lambda e: e.tensor_scalar(out, in0, s1, s2, op0, op1), reads, writes)

    def stt(self, out, in0, scalar, in1, op0, op1, reads, writes):
        return self.add('dve', lambda e: e.scalar_tensor_tensor(out, in0, scalar, in1, op0, op1), reads, writes)

    def red(self, out, in_, op, reads, writes):
        return self.add('dve', lambda e: e.tensor_reduce(out, in_, AX.X, op), reads, writes)

    def recip(self, out, in_, reads, writes):
        return self.add('dve', lambda e: e.reciprocal(out, in_), reads, writes)

    def cp(self, eng, out, in_, reads, writes):
        if eng == 'act':
            return self.add('act', lambda e: e.copy(out, in_), reads, writes)
        return self.add(eng, lambda e: e.tensor_copy(out, in_), reads, writes)

    def memset(self, eng, ap, val, writes):
        return self.add(eng, lambda e: e.memset(ap, val), (), writes)

    def dma(self, q, out, in_, reads, writes, key, slow=False):
        if slow:
            return self.add(q, lambda e: e.dma_start(out=out, in_=in_, allow_slow_non_contiguous=True),
                            reads, writes, dma=key)
        return self.add(q, lambda e: e.dma_start(out=out, in_=in_), reads, writes, dma=key)

    def emit(self, final_wait_eng='sp'):
        nc = self.nc
        ops = self.ops
        n = len(ops)
        has_dep = [False] * n
        for i, o in enumerate(ops):
            latest = {}
            keep = set()
            for d in o['deps']:
                od = ops[d]
                if od['dma'] is not None:
                    keep.add(d)
                    continue
                if od['eng'] == 'pe' and o['eng'] == 'pe' and o['dma'] is None:
                    continue
                if d > latest.get(od['eng'], -1):
                    latest[od['eng']] = d
            keep.update(latest.values())
            o['deps'] = keep
            for d in keep:
                has_dep[d] = True
        eng_names = ['sp', 'act', 'pool', 'dve', 'pe']
        eng_sem = {e: nc.alloc_semaphore(name='sem_' + e) for e in eng_names}
        dma_sems = {}
        dma_cnt = {}
        eng_cnt = {e: 0 for e in eng_names}
        sig = [None] * n
        for i, o in enumerate(ops):
            if o['dma'] is not None:
                k = o['dma']
                if k not in dma_sems:
                    dma_sems[k] = nc.alloc_semaphore(name='dsem%d' % len(dma_sems))
                    dma_cnt[k] = 0
                dma_cnt[k] += 16
                sig[i] = (dma_sems[k], dma_cnt[k], 16)
            elif has_dep[i]:
                eng_cnt[o['eng']] += 1
                sig[i] = (eng_sem[o['eng']], eng_cnt[o['eng']], 1)
        self.n_sems = len(dma_sems) + 5
        self.eng_cnt = eng_cnt
        streams = {e: [i for i, o in enumerate(ops) if o['eng'] == e] for e in eng_names}
        finals = [(dma_sems[k], dma_cnt[k]) for k in dma_sems]

        def run_stream(ename, eng):
            waited = {}
            for i in streams[ename]:
                o = ops[i]
                need = {}
                for d in o['deps']:
                    s, v, _ = sig[d]
                    if v > need.get(s.num, (None, 0))[1]:
                        need[s.num] = (s, v)
                for num in sorted(need):
                    s, v = need[num]
                    if waited.get(num, 0) < v:
                        eng.wait_ge(s, v)
                        waited[num] = v
                ins = o['fn'](eng)
                if sig[i] is not None:
                    ins.then_inc(sig[i][0], sig[i][2])
            if ename == final_wait_eng:
                for s, v in finals:
                    if waited.get(s.num, 0) < v:
                        eng.wait_ge(s, v)

        with nc.Block() as block:
            @block.sync
            def _(e):
                run_stream('sp', e)

            @block.scalar
            def _(e):
                run_stream('act', e)

            @block.gpsimd
            def _(e):
                run_stream('pool', e)

            @block.vector
            def _(e):
                run_stream('dve', e)

            @block.tensor
            def _(e):
                run_stream('pe', e)


class WRing:
    R = 10

    def __init__(self, S, ring, units):
        self.S = S
        self.ring = ring
        self.units = units
        self.next_dma = 0
        self.next_acq = 0
        self.rel = set()
        self.pump()

    def pump(self):
        while self.next_dma < len(self.units):
            j = self.next_dma
            if j >= self.R and (j - self.R) not in self.rel:
                break
            src, kc, ncols = self.units[j]
            slot = j % self.R
            dst = self.ring[:, slot, 0:kc * ncols].rearrange("p (k n) -> p k n", k=kc)
            self.S.dma('pool', dst, src, (), [('w', slot)], ('w', slot))
            self.next_dma += 1

    def acquire(self):
        j = self.next_acq
        assert j < self.next_dma, "weight ring deadlock: unit %d not yet issued" % j
        self.next_acq += 1
        src, kc, ncols = self.units[j]
        slot = j % self.R
        view = self.ring[:, slot, 0:kc * ncols].rearrange("p (k n) -> p k n", k=kc)
        return view, ('w', slot), j

    def release(self, j):
        self.rel.add(j)
        self.pump()


def build_program(dump=None):
    nc = bass.Bass("TRN2", target_bir_lowering=False)
    dump = dump or []

    def din(name, shape):
        return nc.dram_tensor(name, list(shape), F32, kind="ExternalInput").ap()

    def dout(name, shape):
        return nc.dram_tensor(name, list(shape), F32, kind="ExternalOutput").ap()

    x = din("x", (2048, 1024))
    cond = din("cond", (2, 1024))
    ck = din("ck", (512, 1024))
    cv = din("cv", (512, 1024))
    w_ada = din("w_ada", (1024, 6144))
    b_ada = din("b_ada", (1, 6144))
    norm1_g = din("norm1_g", (1, 1024))
    w_in = din("w_in", (1024, 5632))
    b_gate = din("b_gate", (1, 2048))
    q_norm_g = din("q_norm_g", (1, 64))
    k_norm_g = din("k_norm_g", (1, 64))
    lam_in = din("lam4", (1, 256))
    subln_g = din("subln_g", (1, 128))
    pool_w = din("pool_w", (4, 128, 128))
    pool_scale = din("pool_scale", (1, 512))
    w_br_attn = din("w_br_attn", (1024, 1024))
    w_br_pool = din("w_br_pool", (512, 1024))
    w_out = din("w_out", (1024, 1024))
    norm2_g = din("norm2_g", (1, 1024))
    router_w = din("router_w", (1024, 20))
    router_b = din("router_b", (1, 20))
    ew_gate = din("ew_gate", (16, 1024, 512))
    ew_up = din("ew_up", (16, 1024, 512))
    ew_down = din("ew_down", (16, 512, 1024))
    ident_d = din("ident", (128, 128))
    cos_d = din("cos_t", (1024, 64))
    sin_d = din("sin_t", (1024, 64))
    rcb_d = din("rcb", (1, 64))

    tri_d = din("tri", (128, 128))
    thr_d = din("thr", (1, 16))
    tval_d = din("tval", (1, 48))
    rtab_d = din("rtab", (1, 64))
    NMAIN = 48
    NTB = 14
    NTAIL = 2 * NTB
    NTS = NMAIN + NTAIL
    modrows = nc.dram_tensor("modrows", [2, 2, 1024], F32, kind="Internal").ap()
    g2row = nc.dram_tensor("g2row", [2, 1024], F32, kind="Internal").ap()
    gps = nc.dram_tensor("gps", [2, 1024], F32, kind="Internal").ap()
    h2d = nc.dram_tensor("h2d", [2048, 1024], BF16, kind="Internal").ap()
    x1d = nc.dram_tensor("x1d", [2048, 1024], F32, kind="Internal").ap()
    slot_tok = nc.dram_tensor("slot_tok", [80 * 128, 16], I32, kind="Internal").ap()
    yslot = nc.dram_tensor("yslot", [NTS * 128, 1024], F32, kind="Internal").ap()
    y = dout("y", (2048, 1024))
    nk = dout("nk", (1024, 1024))
    nv = dout("nv", (1024, 1024))
    dumps = {}
    for nm, shp in dump:
        dumps[nm] = dout("dbg_" + nm, shp)

    A = nc.alloc_sbuf_tensor
    ring = A("ring", [128, 10, 2048], BF16)
    hT = A("hT", [128, 8, 1024], BF16)
    X1 = A("X1", [128, 8192], F32)
    x1 = X1[:].rearrange("p (t d) -> p t d", t=8)
    vtok = X1[:].bitcast(BF16)[:, 0:12 * 8 * 132].rearrange("p (j h e) -> p j h e", j=12, h=8)
    ACC = A("ACC", [128, 8192], F32)
    acc = ACC[:].rearrange("p (t d) -> p t d", t=8)
    accb = ACC[:].bitcast(BF16)
    qT = accb[:, 0:8192].rearrange("p (h t) -> p h t", h=8)
    mrgT = accb[:, 8192:16384].rearrange("p (h t) -> p h t", h=8)
    mrg_f = ACC[:, 4096:8192]
    KR = A("KR", [128, 8 * 1536], BF16)
    kT = KR[:].rearrange("p (h t) -> p h t", h=8)
    silu_b = [KR[:, i * 512:(i + 1) * 512] for i in range(2)]
    hid = [KR[:, 1024 + i * 4096: 1024 + (i + 1) * 4096].rearrange("p (b f t) -> p b f t", b=2, f=4) for i in range(2)]
    mixT = A("mixT", [128, 4, 1024], BF16)
    PL = A("PL", [128, 4096], F32)
    PLb = PL[:].bitcast(BF16)
    xst = [A("xst%d" % i, [128, 1024], F32) for i in range(2)]
    xn = [A("xn%d" % i, [128, 1024], BF16) for i in range(2)]
    ident = A("identb", [128, 128], BF16)
    cosb = A("cosb", [128, 8, 64], F32)
    sinb = A("sinb", [128, 8, 64], F32)
    gq_b = A("gq_b", [128, 64], F32)
    gk_b = A("gk_b", [128, 64], F32)
    gsub_b = A("gsub_b", [128, 128], F32)
    rcb = A("rcbs", [128, 64], F32)
    lam_b = A("lam_b", [128, 256], F32)
    lam_t = A("lam_t", [128, 128], F32)
    lam_s = A("lam_s", [128, 8], F32)
    sc32 = A("sc32", [128, 8, 2], F32)
    scT2 = A("scT2", [128, 8, 2], BF16)
    screp = A("screp", [128, 8, 128], BF16)
    screp1 = A("screp1", [128, 8, 128], BF16)
    modT2 = A("modT2", [128, 48, 2], F32)
    bT = A("bT", [128, 48], F32)
    n1gT = A("n1gT", [128, 8], F32)
    n2gT = A("n2gT", [128, 8], F32)
    a1T2 = A("a1T2", [128, 8, 2], F32)
    a2T2 = A("a2T2", [128, 8, 2], F32)
    g1b = A("g1b", [128, 1024], F32)
    g2b = A("g2b", [128, 1024], F32)
    bgT = A("bgT", [128, 16], F32)
    pscT = A("pscT", [128, 4], F32)
    rb_b = A("rb_b", [128, 20], F32)
    poolw = A("poolw", [128, 4, 128], BF16)
    rw = A("rw", [128, 8, 20], BF16)
    ss = A("ss", [128, 8], F32)
    rstd = A("rstd", [128, 8], F32)
    ss8 = A("ss8", [128, 2, 8], F32)
    rs8 = A("rs8", [128, 2, 8, 1], F32)
    ss8c = A("ss8c", [128, 8], F32)
    rs8c = A("rs8c", [128, 8, 1], F32)
    rz = A("rz", [128, 4, 2, 1], F32)
    r2n = A("r2n", [128, 4, 1], F32)
    O1g = A("O1g", [128, 16, 16], F32)
    O2g = A("O2g", [128, 16, 16], F32)
    W1g = A("W1g", [128, 16, 1], F32)
    W2g = A("W2g", [128, 16, 1], F32)
    Mb = A("Mb", [128, 16, 16], BF16)
    s12i = A("s12i", [128, 2, 16], I32)
    widx = A("widx", [128, 28, 2], I32)
    gidx = A("gidx", [128, 76], I32)
    trib = A("trib", [128, 128], BF16)
    onesb = A("onesb", [128, 128], BF16)
    thr = A("thr_sb", [128, 16], F32)
    tval = A("tval_sb", [128, 48], F32)
    tok16 = A("tok16", [128, 16, 16], I32)
    p2col = A("p2col", [128, 1], F32)
    rtab = A("rtab_sb", [128, 64], F32)
    fscr = A("fscr", [128, 1], F32)

    PSALL = nc.alloc_psum_tensor("psall", [128, 4096], F32)
    PSALLb = PSALL[:].bitcast(BF16)
    PS = [PSALL[:, i * 512:(i + 1) * 512] for i in range(8)]
    PSb = [PSALLb[:, i * 1024:(i + 1) * 1024] for i in range(8)]

    S = Sched(nc, fscr[:])

    def psk(i):
        return ('ps', i)

    def cols(w, c0, n, kc):
        return (w[:, c0:c0 + n].rearrange("(k p) n -> p k n", p=128), kc, n)

    pass_units = []
    for u in range(24):
        pass_units.append(cols(w_ada, 256 * u, 256, 8))
    for u in range(14):
        pass_units.append(cols(w_in, 256 * u, 256, 8))
    for ng in range(4):
        pass_units.append(cols(w_in, 3584 + 256 * ng, 256, 8))
        pass_units.append(cols(w_in, 4608 + 256 * ng, 256, 8))
        pass_units.append(cols(w_br_attn, 256 * ng, 256, 8))
        pass_units.append(cols(w_br_pool, 256 * ng, 256, 4))
    for j in range(4):
        pass_units.append(cols(w_out, 256 * j, 256, 8))
    W = WRing(S, ring, pass_units + pass_units[24:])

    def cload(dst, src, key, q='sp', slow=False):
        S.dma(q, dst, src, (), [key], key, slow=slow)

    for j in range(2):
        S.dma('sp', sc32[:, :, j], cond[j, :].rearrange("(k p) -> p k", p=128), (), [('sc32', j)], ('sc32', j), slow=True)

    cload(ident[:], ident_d[:, :], 'ident', q='pool')
    cload(cosb[:], cos_d.rearrange("(t p) d -> p t d", p=128), 'cosb')
    cload(sinb[:], sin_d.rearrange("(t p) d -> p t d", p=128), 'sinb')
    cload(gq_b[:], q_norm_g[0:1, :].to_broadcast([128, 64]), 'gq_b')
    cload(gk_b[:], k_norm_g[0:1, :].to_broadcast([128, 64]), 'gk_b')
    cload(gsub_b[:], subln_g[0:1, :].to_broadcast([128, 128]), 'gsub_b')
    cload(rcb[:], rcb_d[0:1, :].to_broadcast([128, 64]), 'rcb')
    cload(lam_b[:], lam_in[0:1, :].to_broadcast([128, 256]), 'lam_b')
    cload(bT[:], b_ada[0, :].rearrange("(c p) -> p c", p=128), 'bT', slow=True)
    cload(n1gT[:], norm1_g[0, :].rearrange("(c p) -> p c", p=128), 'n1gT', slow=True)
    cload(n2gT[:], norm2_g[0, :].rearrange("(c p) -> p c", p=128), 'n2gT', slow=True)
    cload(bgT[:], b_gate[0, :].rearrange("(c p) -> p c", p=128), 'bgT', slow=True)
    cload(pscT[:], pool_scale[0, :].rearrange("(c p) -> p c", p=128), 'pscT', slow=True)
    cload(rb_b[:], router_b[0:1, :].to_broadcast([128, 20]), 'rb_b')
    cload(poolw[:], pool_w.rearrange("g c e -> c g e"), 'poolw', q='pool')
    cload(rw[:], router_w.rearrange("(k p) n -> p k n", p=128), 'rw', q='pool')

    cload(trib[:], tri_d[:, :], 'trib', q='pool')
    cload(thr[:], thr_d[0:1, :].to_broadcast([128, 16]), 'thr')
    cload(tval[:], tval_d[0:1, :].to_broadcast([128, 48]), 'tval')
    cload(rtab[:], rtab_d[0:1, :].to_broadcast([128, 64]), 'rtab')
    S.memset('dve', onesb[:], 1.0, ['onesb'])
    S.add('pool', lambda e: e.iota(tok16[:], [[128, 16], [0, 16]], base=0, channel_multiplier=1), (), ['tok16'])
    S.add('pool', lambda e: e.iota(gidx[:, 0:1], [[0, 1]], base=0, channel_multiplier=2), (), ['p2i'])
    S.cp('dve', p2col[:], gidx[:, 0:1], ['p2i'], ['p2col'])
    S.ts('dve', gq_b[:], gq_b[:], 8.0 * 0.125, None, ALU.mult, None, ['gq_b'], ['gq_b'])
    S.ts('dve', gk_b[:], gk_b[:], 8.0, None, ALU.mult, None, ['gk_b'], ['gk_b'])
    S.ts('dve', gsub_b[:], gsub_b[:], (1.0 - LAMBDA_INIT) * (128.0 ** 0.5), None, ALU.mult, None, ['gsub_b'], ['gsub_b'])
    lb = lam_b[:].rearrange("p (a b d) -> p a b d", a=2, b=2)
    lt = lam_t[:].rearrange("p (a d) -> p a d", a=2)
    S.tt('dve', lt, lb[:, :, 0, :], lb[:, :, 1, :], ALU.mult, ['lam_b'], ['lam_t'])
    S.red(lam_s[:, 0:2], lt, ALU.add, ['lam_t'], ['lam_s'])
    S.act(lam_s[:, 2:4], lam_s[:, 0:2], AF.Exp, ['lam_s'], ['lam_s2'])
    S.tt('dve', lam_s[:, 4:5], lam_s[:, 2:3], lam_s[:, 3:4], ALU.subtract, ['lam_s2'], ['lam_s3'])
    S.ts('dve', lam_s[:, 5:6], lam_s[:, 4:5], LAMBDA_INIT, -1.0, ALU.add, ALU.mult, ['lam_s3'], ['neg_lam'])
    neg_lam = lam_s[:, 5:6]

    def dbg(name, src_ap, reads):
        if name in dumps:
            S.dma('pool', dumps[name], src_ap, reads, [], 'dbg_' + name)

    def norm_T(p, src, aT, shT, hkey):
        for blk in range(2):
            banks = [4 * blk + i for i in range(4)]
            for t in range(4):
                tile = blk * 4 + t
                sl = tile % 2
                if src == 'x':
                    xs = xst[sl][:]
                    xkey = ('xst', sl)
                    S.dma('sp', xs, x[p * 1024 + tile * 128: p * 1024 + (tile + 1) * 128, :], (), [xkey], xkey)
                else:
                    xs = x1[:, tile, :]
                    xkey = ('x1', tile)
                S.act(xn[sl][:], xs, AF.Square, [xkey], [('xn', sl), ('ss', tile)], accum_out=ss[:, tile:tile + 1])
                S.act(rstd[:, tile:tile + 1], ss[:, tile:tile + 1], AF.Ln, [('ss', tile)], [('rstd', tile)],
                      bias=EPS, scale=1.0 / 1024.0)
                S.act(rstd[:, tile:tile + 1], rstd[:, tile:tile + 1], AF.Exp, [('rstd', tile)], [('rstd', tile)], scale=-0.5)
                S.act(xn[sl][:], xs, AF.Copy, [xkey, ('rstd', tile)], [('xn', sl)], scale=rstd[:, tile:tile + 1])
                if src == 'x1':
                    tmp = PL[:, sl * 1024:(sl + 1) * 1024]
                    hb = PL[:, 2048 + sl * 512: 2560 + sl * 512].bitcast(BF16)
                    S.tt('pool', tmp, xs, g1b[:], ALU.mult, [xkey, 'g1b'], [('pl', 'h2t', sl)])
                    S.stt(hb, tmp, rstd[:, tile:tile + 1], xst[0][:], ALU.mult, ALU.add,
                          [('pl', 'h2t', sl), ('rstd', tile), ('xst', 0)], [('pl', 'h2b', sl)])
                    S.dma('sp', h2d[p * 1024 + tile * 128: p * 1024 + (tile + 1) * 128, :], hb, [('pl', 'h2b', sl)],
                          [('h2d', p, tile)], ('h2o', sl))
                    S.dma('sp', x1d[p * 1024 + tile * 128: p * 1024 + (tile + 1) * 128, :], xs, [xkey],
                          [('x1d', p, tile)], 'x1o')
                for c in range(8):
                    bv = PSb[banks[c // 2]].rearrange("p (c t) -> p c t", c=2)
                    S.tr(bv[:, c % 2, t * 128:(t + 1) * 128], xn[sl][:, c * 128:(c + 1) * 128], ident[:],
                         [('xn', sl), 'ident'], [psk(banks[c // 2])])
            for c in range(8):
                bv = PSb[banks[c // 2]].rearrange("p (c t) -> p c t", c=2)
                dst = hT[:, c, blk * 512:(blk + 1) * 512]
                if c % 2 == 0:
                    S.act(dst, bv[:, c % 2, :], AF.Identity, [psk(banks[c // 2]), aT[1], shT[1]], [(hkey, blk)],
                          bias=shT[0][:, c:c + 1], scale=aT[0][:, c:c + 1])
                else:
                    S.ts('dve', dst, bv[:, c % 2, :], aT[0][:, c:c + 1], shT[0][:, c:c + 1], ALU.mult, ALU.add,
                         [psk(banks[c // 2]), aT[1], shT[1]], [(hkey, blk)])

    def run_pass(p):
        nseq, L = (4, 256) if p == 0 else (1, 1024)
        nkt = 8 if p == 0 else 12
        S.fence()
        if p == 0:
            S.act(scT2[:], sc32[:], AF.Silu, [('sc32', 0), ('sc32', 1)], ['scT'])
            S.cp('dve', screp[:], scT2[:, :, 0:1].to_broadcast([128, 8, 128]), ['scT'], ['screp'])
            S.cp('dve', screp1[:], scT2[:, :, 1:2].to_broadcast([128, 8, 128]), ['scT'], ['screp1'])
            S.dma('sp', g1b[:], b_ada[0:1, 2048:3072].to_broadcast([128, 1024]), (), ['g1b'], 'g1b')
            S.dma('sp', g2b[:], b_ada[0:1, 5120:6144].to_broadcast([128, 1024]), (), ['g2b'], 'g2b')
            mod2 = PS[0][:, 0:96].rearrange("p (c j) -> p c j", j=2)
            for u in range(24):
                wv, wk, wj = W.acquire()
                if u in (8, 9, 10, 11, 20, 21, 22, 23):
                    gb, gkey, base = (g1b, 'g1b', 8) if u < 12 else (g2b, 'g2b', 20)
                    gi_ = 0 if u < 12 else 1
                    bank = 1 + (u % 2)
                    for kc in range(8):
                        S.mm(PS[bank][:, 0:256], screp[:, kc, :], wv[:, kc, :], kc == 0, kc == 7,
                             ['screp', wk], [psk(bank)])
                    c0 = (u - base) * 256
                    S.tt('dve', gb[:, c0:c0 + 256], PS[bank][:, 0:256], gb[:, c0:c0 + 256], ALU.add,
                         [psk(bank), gkey], [gkey])
                    bank2 = 3 + (u % 2)
                    for kc in range(8):
                        S.mm(PS[bank2][:, 0:256], screp1[:, kc, :], wv[:, kc, :], kc == 0, kc == 7,
                             ['screp1', wk], [psk(bank2)])
                    gst = PL[:, (u % 4) * 256:(u % 4 + 1) * 256]
                    S.cp('act', gst, PS[bank2][:, 0:256], [psk(bank2)], [('pl', 'gst', u % 4)])
                    S.dma('sp', gps[gi_:gi_ + 1, c0:c0 + 256], gst[0:1, :], [('pl', 'gst', u % 4)], [('gps', gi_, u)], ('gpso', u % 4))
                else:
                    for cc in range(2):
                        ch = 2 * u + cc
                        for kc in range(8):
                            S.mm(mod2[:, ch, :], wv[:, kc, cc * 128:(cc + 1) * 128], scT2[:, kc, :], kc == 0, kc == 7,
                                 ['scT', wk], [psk(0)])
                W.release(wj)
            bT3 = bT[:].rearrange("p (c o) -> p c o", o=1)
            S.tt('dve', modT2[:, 0:16, :], mod2[:, 0:16, :], bT3[:, 0:16, :].to_broadcast([128, 16, 2]), ALU.add, [psk(0), 'bT'], ['modT'])
            S.tt('dve', modT2[:, 24:40, :], mod2[:, 24:40, :], bT3[:, 24:40, :].to_broadcast([128, 16, 2]), ALU.add, [psk(0), 'bT'], ['modT'])
            for j in range(2):
                S.stt(a1T2[:, :, j], modT2[:, 8:16, j], 1.0, n1gT[:], ALU.add, ALU.mult, ['modT', 'n1gT'], ['a1T'])
                S.stt(a2T2[:, :, j], modT2[:, 32:40, j], 1.0, n2gT[:], ALU.add, ALU.mult, ['modT', 'n2gT'], ['a2T'])
        else:
            gkeys1 = [('gps', 0, u) for u in (8, 9, 10, 11)]
            gkeys2 = [('gps', 1, u) for u in (20, 21, 22, 23)]
            S.dma('sp', g1b[:], b_ada[0:1, 2048:3072].to_broadcast([128, 1024]), (), ['g1b'], 'g1b')
            S.dma('sp', g2b[:], b_ada[0:1, 5120:6144].to_broadcast([128, 1024]), (), ['g2b'], 'g2b')
            S.dma('sp', xst[0][:], gps[0:1, :].to_broadcast([128, 1024]), gkeys1, [('xst', 0)], ('xst', 0))
            S.dma('sp', xst[1][:], gps[1:2, :].to_broadcast([128, 1024]), gkeys2, [('xst', 1)], ('xst', 1))
            S.tt('dve', g1b[:], g1b[:], xst[0][:], ALU.add, ['g1b', ('xst', 0)], ['g1b'])
            S.tt('dve', g2b[:], g2b[:], xst[1][:], ALU.add, ['g2b', ('xst', 1)], ['g2b'])
        a1T = a1T2[:, :, p]
        a2T = a2T2[:, :, p]
        sh1 = (modT2[:, 0:8, p], 'modT')
        sh2 = (modT2[:, 24:32, p], 'modT')

        norm_T(p, 'x', (a1T, 'a1T'), sh1, 'hT')
        if p == 0:
            for j in range(2):
                S.dma('sp', modrows[j, 0, :].rearrange("(c p) -> p c", p=128), a2T2[:, :, j], ['a2T'], [('modrows', j)], 'mro', slow=True)
                S.dma('sp', modrows[j, 1, :].rearrange("(c p) -> p c", p=128), modT2[:, 24:32, j], ['modT'], [('modrows', j)], 'mro', slow=True)
        S.dma('sp', g2row[p:p + 1, :], g2b[0:1, :], ['g2b'], [('g2row', p)], 'g2o')
        if p == 0:
            dbg('hT', hT[:], [('hT', 0), ('hT', 1)])

        sqs = [mrg_f[:, 0:512], mrg_f[:, 3584:4096]]
        zn = mrg_f[:, 512:1024]
        zg = [mrg_f[:, 1024 + i * 512: 1536 + i * 512] for i in range(2)]
        vst = [mrg_f[:, 2048 + i * 512: 2560 + i * 512] for i in range(2)]
        qkb = [mrg_f[:, 3072 + i * 256: 3328 + i * 256].bitcast(BF16) for i in range(2)]
        zns = [zn, vst[0]]
        if p == 1:
            rtab = {}
            for ti, (gb_, gkey) in enumerate(((gq_b, 'gq_b'), (gk_b, 'gk_b'))):
                Cg = PL[:, ti * 1024: ti * 1024 + 512].rearrange("p (t d) -> p t d", t=8)
                Sg = PL[:, ti * 1024 + 512: ti * 1024 + 1024].rearrange("p (t d) -> p t d", t=8)
                S.tt('pool', Cg, cosb[:], gb_[:].rearrange("p (o d) -> p o d", o=1).to_broadcast([128, 8, 64]), ALU.mult,
                     ['cosb', gkey], [('pl', 'Cg', ti)])
                g4 = gb_[:].rearrange("p (a s d) -> p a s d", a=2, s=2)
                for sidx in range(2):
                    for a_ in range(2):
                        S.tt('pool', Sg[:, :, a_ * 32 + sidx * 16: a_ * 32 + sidx * 16 + 16],
                             sinb[:, :, a_ * 32 + sidx * 16: a_ * 32 + sidx * 16 + 16],
                             g4[:, a_, 1 - sidx, :].rearrange("p (o d) -> p o d", o=1).to_broadcast([128, 8, 16]), ALU.mult,
                             ['sinb', gkey], [('pl', 'Sg', ti)])
                rtab[ti] = (Cg, Sg)
        S.memset('dve', vtok[:, :, :, 128:129], 1.0, [('vt', 'ones')])
        if p == 1:
            for jt in range(4):
                S.dma('pool', vtok[:, 8 + jt, :, 0:128],
                      cv[jt * 128:(jt + 1) * 128, :].rearrange("p (h e) -> p h e", h=8), (), [('vt', 8 + jt)], ('vtc', jt))
                sl = jt % 2
                S.dma('pool', xn[sl][:], ck[jt * 128:(jt + 1) * 128, :], (), [('xn', sl)], ('xnc', sl))
                bv = PSb[7].rearrange("p (h t) -> p h t", h=8)
                for h in range(8):
                    S.tr(bv[:, h, :], xn[sl][:, h * 128:(h + 1) * 128], ident[:], [('xn', sl), 'ident'], [psk(7)])
                S.cp('act', kT[:, :, 1024 + jt * 128: 1024 + (jt + 1) * 128], bv, [psk(7)], [('kT', 8 + jt)])
        qk_units = {}

        def qk_M(n):
            ci, tile = divmod(n, 8)
            if tile == 0:
                qk_units[ci] = (W.acquire(), W.acquire())
            (ua, uak, uaj), (ub, ubk, ubj) = qk_units[ci]
            bank = n % 4
            for half, (u_, uk_) in enumerate(((ua, uak), (ub, ubk))):
                for kc in range(8):
                    S.mm(PS[bank][:, half * 256:(half + 1) * 256], hT[:, kc, tile * 128:(tile + 1) * 128],
                         u_[:, kc, :], kc == 0, kc == 7, [('hT', tile // 4), uk_], [psk(bank)])
            if tile == 7:
                W.release(uaj)
                W.release(ubj)

        def qk_E1(n):
            bank = n % 4
            b2 = n % 2
            zps = PS[bank][:, 0:512]
            sqb = sqs[b2]
            S.act(sqb, zps, AF.Square, [psk(bank)], [('st2', 'sq', b2)])
            S.red(ss8[:, b2, :], sqb.rearrange("p (g d) -> p g d", g=8), ALU.add, [('st2', 'sq', b2)], [('ss8', b2)])
            S.act(rs8[:, b2, :, 0], ss8[:, b2, :], AF.Ln, [('ss8', b2)], [('rs8', b2)], bias=64.0 * EPS)
            S.act(rs8[:, b2, :, 0], rs8[:, b2, :, 0], AF.Exp, [('rs8', b2)], [('rs8', b2)], scale=-0.5)

        def qk_E2(n):
            ci, tile = divmod(n, 8)
            isq = ci < 2
            hc = ci % 2
            gb_, gkey = (gq_b, 'gq_b') if isq else (gk_b, 'gk_b')
            bank = n % 4
            s2 = n % 2
            b2 = n % 2
            zps = PS[bank][:, 0:512]
            sqb = sqs[b2]
            znb = zns[b2] if p == 1 else zn
            znk = ('st2', 'zn', b2) if p == 1 else ('st2', 'zn')
            S.tt('dve', znb.rearrange("p (g d) -> p g d", g=8), zps.rearrange("p (g d) -> p g d", g=8),
                 rs8[:, b2, :, :].to_broadcast([128, 8, 64]), ALU.mult, [psk(bank), ('rs8', b2)], [znk])
            gbb = gb_[:].rearrange("p (o d) -> p o d", o=1).to_broadcast([128, 8, 64])
            if p == 0:
                if isq:
                    S.tt('pool', qkb[s2].rearrange("p (g d) -> p g d", g=8), zn.rearrange("p (g d) -> p g d", g=8),
                         gbb, ALU.mult, [znk, gkey], [('st2', 'qkb', s2)])
                else:
                    S.tt('pool', zg[s2].rearrange("p (g d) -> p g d", g=8), zn.rearrange("p (g d) -> p g d", g=8),
                         gbb, ALU.mult, [znk, gkey], [('st2', 'zg', s2)])
                    S.dma('sp', nk[tile * 128:(tile + 1) * 128, hc * 512:(hc + 1) * 512], zg[s2],
                          [('st2', 'zg', s2)], [], ('nk', s2))
                    S.cp('act', qkb[s2], zg[s2], [('st2', 'zg', s2)], [('st2', 'qkb', s2)])
            else:
                ti = 0 if isq else 1
                Cg, Sg = rtab[ti]
                t1 = zg[s2]
                S.tt('dve', t1.rearrange("p (g d) -> p g d", g=8), znb.rearrange("p (g d) -> p g d", g=8),
                     Cg[:, tile:tile + 1, :].to_broadcast([128, 8, 64]), ALU.mult, [znk, ('pl', 'Cg', ti)], [('st2', 'zg', s2)])
                zz = znb.rearrange("p (g a s d) -> p g a s d", g=8, a=2, s=2)
                t2 = vst[1]
                qq = t2.rearrange("p (g a s d) -> p g a s d", g=8, a=2, s=2)
                sg4 = Sg[:, tile:tile + 1, :].rearrange("p o (a s d) -> p o a s d", a=2, s=2)
                for sidx in range(2):
                    sn = sg4[:, :, :, sidx, :].to_broadcast([128, 8, 2, 16])
                    S.tt('pool', qq[:, :, :, sidx, :], zz[:, :, :, 1 - sidx, :], sn, ALU.mult,
                         [znk, ('pl', 'Sg', ti)], [('st2', 't2', sidx)])
                S.tt('dve', qkb[s2], t1, t2, ALU.add, [('st2', 'zg', s2), ('st2', 't2', 0), ('st2', 't2', 1)], [('st2', 'qkb', s2)])

        def qk_T(n):
            ci, tile = divmod(n, 8)
            isq = ci < 2
            hc = ci % 2
            s2 = n % 2
            bk = 6 + (n % 2)
            bv = PSb[bk].rearrange("p (h t) -> p h t", h=8)
            for hh in range(4):
                S.tr(bv[:, hh, :], qkb[s2][:, hh * 128:(hh + 1) * 128], ident[:], [('st2', 'qkb', s2), 'ident'], [psk(bk)])
            if isq:
                S.cp('act', qT[:, 4 * hc:4 * hc + 4, tile * 128:(tile + 1) * 128], bv[:, 0:4, :], [psk(bk)], [('qT', tile // 2)])
            else:
                S.cp('act', kT[:, 4 * hc:4 * hc + 4, tile * 128:(tile + 1) * 128], bv[:, 0:4, :], [psk(bk)], [('kT', tile)])

        NQK = 32
        for s_ in range(NQK + 3):
            if s_ < NQK:
                qk_M(s_)
            if 0 <= s_ - 1 < NQK:
                qk_E1(s_ - 1)
            if 0 <= s_ - 2 < NQK:
                qk_E2(s_ - 2)
            if 0 <= s_ - 3 < NQK:
                qk_T(s_ - 3)
        for ci in range(2):
            ua, uak, uaj = W.acquire()
            ub, ubk, ubj = W.acquire()
            for tile in range(8):
                bank = tile % 4
                for half, (u_, uk_) in enumerate(((ua, uak), (ub, ubk))):
                    for kc in range(8):
                        S.mm(PS[bank][:, half * 256:(half + 1) * 256], hT[:, kc, tile * 128:(tile + 1) * 128],
                             u_[:, kc, :], kc == 0, kc == 7, [('hT', tile // 4), uk_], [psk(bank)])
                zps = PS[bank][:, 0:512]
                S.cp('act', vtok[:, tile, 4 * ci:4 * ci + 4, 0:128], zps.rearrange("p (h e) -> p h e", h=4),
                     [psk(bank)], [('vt', tile)])
                if p == 0:
                    s2 = tile % 2
                    S.cp('dve', vst[s2], zps, [psk(bank)], [('st2', 'vst', s2)])
                    S.dma('sp', nv[tile * 128:(tile + 1) * 128, ci * 512:(ci + 1) * 512], vst[s2],
                          [('st2', 'vst', s2)], [], ('nv', s2))
            W.release(uaj)
            W.release(ubj)
        S.fence(('PL',))
        Wd = L + 16
        Pb = PL[:, 0:1088]
        Ab = PL[:, 1088:2176]
        Bb = PL[:, 2176:3264]
        pooled = PL[:, 3264:3776].bitcast(BF16)
        tmpb = PL[:, 3776:3776 + 32]
        S.memset('dve', Pb, 0.0, [('pl', 'P')])

        def v3(buf):
            return buf[:, 0:nseq * Wd].rearrange("p (s l) -> p s l", s=nseq)

        def rg(buf, a, b):
            return v3(buf)[:, :, 8 + a: 8 + b]

        for half in range(2):
            up, upk, upj = W.acquire()
            for gg in range(2):
                g = 2 * half + gg
                w_ = (2, 4, 8, 16)[g]
                hw = w_ // 2
                for blk in range(2):
                    bank = 4 + blk
                    for kc in range(8):
                        S.mm(PS[bank][:, 0:512], up[:, kc, gg * 128:(gg + 1) * 128], hT[:, kc, blk * 512:(blk + 1) * 512],
                             kc == 0, kc == 7, [('hT', blk), upk], [psk(bank)])
                    if p == 0:
                        S.cp('act', v3(Pb)[:, 2 * blk:2 * blk + 2, 8:8 + 256], PS[bank][:, 0:512].rearrange("p (s l) -> p s l", s=2),
                             [psk(bank)], [('pl', 'P')])
                    else:
                        S.cp('act', Pb[:, 8 + 512 * blk: 8 + 512 * (blk + 1)], PS[bank][:, 0:512], [psk(bank)], [('pl', 'P')])
                pk, ak, bk_ = ('pl', 'P'), ('pl', 'A'), ('pl', 'B')
                S.tt('dve', rg(Ab, -7, L + 8), rg(Pb, -8, L + 7), rg(Pb, -7, L + 8), ALU.add, [pk], [ak])
                src_, sk = Ab, ak
                if g >= 1:
                    S.tt('dve', rg(Bb, -6, L + 7), rg(Ab, -7, L + 6), rg(Ab, -5, L + 8), ALU.add, [ak], [bk_])
                    src_, sk = Bb, bk_
                if g >= 2:
                    S.tt('dve', rg(Ab, -4, L + 5), rg(Bb, -6, L + 3), rg(Bb, -2, L + 7), ALU.add, [bk_], [ak])
                    src_, sk = Ab, ak
                if g >= 3:
                    S.tt('dve', rg(Bb, 0, L), rg(Ab, -4, L - 4), rg(Ab, 4, L + 4), ALU.add, [ak], [bk_])
                    src_, sk = Bb, bk_
                pl3 = pooled.rearrange("p (s l) -> p s l", s=nseq)
                S.stt(pl3, rg(src_, 0, L), 1.0 / w_, rg(Pb, 0, L), ALU.mult, ALU.subtract, [sk, pk], [('pl', 'pooled')])
                tb = tmpb[:, 0:nseq * hw].rearrange("p (s l) -> p s l", s=nseq)
                for side in range(2):
                    lo, hi = (0, hw) if side == 0 else (L - hw, L)
                    rcv = rcb[:, g * 16 + side * 8: g * 16 + side * 8 + hw].rearrange("p (o l) -> p o l", o=1).to_broadcast([128, nseq, hw])
                    S.tt('dve', tb, rg(src_, lo, hi), rcv, ALU.mult, [sk, 'rcb'], [('pl', 'tmpb')])
                    S.tt('dve', pl3[:, :, lo:hi], tb, rg(Pb, lo, hi), ALU.subtract, [('pl', 'tmpb'), pk], [('pl', 'pooled')])
                for blk in range(2):
                    bank = 6 + blk
                    S.mm(PS[bank][:, 0:512], poolw[:, g, :], pooled[:, blk * 512:(blk + 1) * 512], True, True,
                         [('pl', 'pooled'), 'poolw'], [psk(bank)])
                    S.act(mixT[:, g, blk * 512:(blk + 1) * 512], PS[bank][:, 0:512], AF.Copy, [psk(bank), 'pscT'],
                          [('mixT', blk)], scale=pscT[:, g:g + 1])
            W.release(upj)
        if p == 0:
            dbg('qT', qT, [('qT', i) for i in range(4)])
            dbg('kT', kT, [('kT', i) for i in range(8)])
            dbg('mixT', mixT[:], [('mixT', 0), ('mixT', 1)])

        S.fence(('ACC', 'PL'))
        PT = [PLb[:, i * 512:(i + 1) * 512] for i in range(3)]
        sqo = PL[:, 768:1792]
        tO2 = [PL[:, 1792 + i * 128: 1920 + i * 128] for i in range(4)]
        onb = [PL[:, 2304 + i * 512: 2816 + i * 512].bitcast(BF16) for i in range(2)]
        ost = [[mrg_f[:, (a * 2 + b) * 1024:(a * 2 + b + 1) * 1024].rearrange("p (h e) -> p h e", h=8) for b in range(2)]
               for a in range(2)]
        items = []
        for qb in range(4):
            keytiles = [2 * qb, 2 * qb + 1] if p == 0 else list(range(12))
            for h in range(8):
                for jn, j in enumerate(keytiles):
                    items.append((qb, h, jn, j, len(keytiles)))
        pending_T = []
        o2c = [0]

        def at_A(n):
            qb, h, jn, j, nk_ = items[n]
            sbk = n % 3
            sp_ = n % 2
            for i in range(2):
                S.mm(PS[2 * sp_ + i][:, 0:256], kT[i * 64:(i + 1) * 64, h, j * 128:(j + 1) * 128],
                     qT[i * 64:(i + 1) * 64, h, qb * 256:(qb + 1) * 256], True, True,
                     [('kT', j), ('qT', qb)], [psk(2 * sp_ + i)])
            S.act(PT[sbk].rearrange("p (i q) -> p i q", i=2),
                  PSALL[:, 2 * sp_ * 512:(2 * sp_ + 2) * 512].rearrange("p (i q) -> p i q", i=2)[:, :, 0:256],
                  AF.Exp, [psk(2 * sp_), psk(2 * sp_ + 1)], [('pl', 'PT', sbk)])

        def subln(qb):
            for qt in range(2):
                o = ost[qb % 2][qt]
                ok_ = ('ost', qb % 2, qt)
                tile = qb * 2 + qt
                S.tt('dve', sqo.rearrange("p (h e) -> p h e", h=8), o, o, ALU.mult, [ok_], [('pl', 'sqo')])
                S.red(ss8c[:], sqo.rearrange("p (h e) -> p h e", h=8), ALU.add, [('pl', 'sqo')], ['ss8b'])
                S.act(rs8c[:, :, 0], ss8c[:], AF.Ln, ['ss8b'], ['rs8b'], bias=128.0 * EPS)
                S.act(rs8c[:, :, 0], rs8c[:, :, 0], AF.Exp, ['rs8b'], ['rs8b'], scale=-0.5)
                S.tt('pool', o, o, rs8c[:].to_broadcast([128, 8, 128]), ALU.mult, [ok_, 'rs8b'], [ok_])
                ob = onb[qt].rearrange("p (h e) -> p h e", h=8)
                S.tt('pool', ob, o, gsub_b[:].rearrange("p (o e) -> p o e", o=1).to_broadcast([128, 8, 128]), ALU.mult,
                     [ok_, 'gsub_b'], [('pl', 'onb', qt)])

                def T_on(qb=qb, qt=qt, tile=tile):
                    bv = PSb[0].rearrange("p (h t) -> p h t", h=8)
                    for h in range(8):
                        S.tr(bv[:, h, :], onb[qt][:, h * 128:(h + 1) * 128], ident[:], [('pl', 'onb', qt), 'ident'], [psk(0)])
                    S.cp('act', qT[:, :, tile * 128:(tile + 1) * 128], bv, [psk(0)], [('qT', qb)])
                pending_T.append(T_on)

        def at_B(n):
            qb, h, jn, j, nk_ = items[n]
            sbk = n % 3
            ab = [4 + 2 * (h % 2), 5 + 2 * (h % 2)]
            if h == 2 and jn == 0:
                while pending_T:
                    pending_T.pop(0)()
            for qt in range(2):
                for i in range(2):
                    S.mm(PS[ab[qt]][:, i * 132:i * 132 + 129], PT[sbk][:, i * 256 + qt * 128: i * 256 + (qt + 1) * 128],
                         vtok[:, j, h, 0:129], jn == 0 and i == 0, jn == nk_ - 1,
                         [('pl', 'PT', sbk), ('vt', j), ('vt', 'ones')], [psk(ab[qt])], skip=True)
        def at_C(n):
            qb, h, jn, j, nk_ = items[n]
            ab = [4 + 2 * (h % 2), 5 + 2 * (h % 2)]
            if jn == nk_ - 1:
                for qt in range(2):
                    av = PS[ab[qt]][:, 0:264].rearrange("p (i e) -> p i e", i=2)
                    sl4 = o2c[0] % 4
                    o2c[0] += 1
                    S.recip(rz[:, sl4, :, :], av[:, :, 128:129], [psk(ab[qt])], [('rz', sl4)])
                    S.ts('dve', r2n[:, sl4, :], rz[:, sl4, 1, :], neg_lam, None, ALU.mult, None, [('rz', sl4), 'neg_lam'], [('r2n', sl4)])
                    S.act(tO2[sl4], av[:, 1, 0:128], AF.Copy, [psk(ab[qt]), ('r2n', sl4)], [('pl', 'tO2', sl4)], scale=r2n[:, sl4, :])
                    S.stt(ost[qb % 2][qt][:, h, :], av[:, 0, 0:128], rz[:, sl4, 0, :], tO2[sl4], ALU.mult, ALU.add,
                          [psk(ab[qt]), ('rz', sl4), ('pl', 'tO2', sl4)], [('ost', qb % 2, qt)])
                if h == 7:
                    subln(qb)

        NI = len(items)
        CL = 2 if p == 0 else 3
        for s_ in range(NI + CL):
            if s_ < NI:
                at_A(s_)
            if 0 <= s_ - 1 < NI:
                at_B(s_ - 1)
            if 0 <= s_ - CL < NI:
                at_C(s_ - CL)
        while pending_T:
            pending_T.pop(0)()
        if p == 0:
            dbg('onT', qT, [('qT', i) for i in range(4)])

        S.fence(('ACC', 'PL'))
        sg0 = [PLb[:, i * 512:(i + 1) * 512] for i in range(2)]
        sg1 = [PLb[:, 1024 + i * 512: 1536 + i * 512] for i in range(2)]
        t0 = PL[:, 1024:1536]
        t1 = PL[:, 1536:2048]
        wtmp = [PL[:, 2048 + i * 512: 2560 + i * 512] for i in range(2)]
        it = 0
        for ng in range(4):
            ug0, ug0k, ug0j = W.acquire()
            ug1, ug1k, ug1j = W.acquire()
            ua, uak, uaj = W.acquire()
            up, upk, upj = W.acquire()
            for blk in range(2):
                tsl = slice(blk * 512, (blk + 1) * 512)
                for cc in range(2):
                    c = 2 * ng + cc
                    b0 = (it % 2) * 4
                    s2 = it % 2
                    it += 1
                    csl = slice(cc * 128, (cc + 1) * 128)
                    for kc in range(8):
                        S.mm(PS[b0][:, 0:512], ug0[:, kc, csl], hT[:, kc, tsl], kc == 0, kc == 7, [('hT', blk), ug0k], [psk(b0)])
                    for kc in range(8):
                        S.mm(PS[b0 + 1][:, 0:512], ug1[:, kc, csl], hT[:, kc, tsl], kc == 0, kc == 7, [('hT', blk), ug1k], [psk(b0 + 1)])
                    for kc in range(8):
                        S.mm(PS[b0 + 2][:, 0:512], ua[:, kc, csl], qT[:, kc, tsl], kc == 0, kc == 7,
                             [('qT', 2 * blk), ('qT', 2 * blk + 1), uak], [psk(b0 + 2)])
                    for fc in range(4):
                        S.mm(PS[b0 + 3][:, 0:512], up[:, fc, csl], mixT[:, fc, tsl], fc == 0, fc == 3, [('mixT', blk), upk], [psk(b0 + 3)])
                    S.act(sg0[s2], PS[b0][:, 0:512], AF.Sigmoid, [psk(b0), 'bgT'], [('pl', 'sg0', s2)], bias=bgT[:, c:c + 1])
                    S.act(sg1[s2], PS[b0 + 1][:, 0:512], AF.Sigmoid, [psk(b0 + 1), 'bgT'], [('pl', 'sg1', s2)], bias=bgT[:, 8 + c:9 + c])
                    S.tt('dve', t0, PS[b0 + 2][:, 0:512], sg0[s2], ALU.mult, [psk(b0 + 2), ('pl', 'sg0', s2)], [('pl', 't0')])
                    S.tt('dve', t1, PS[b0 + 3][:, 0:512], sg1[s2], ALU.mult, [psk(b0 + 3), ('pl', 'sg1', s2)], [('pl', 't1')])
                    S.tt('pool', mrgT[:, c, tsl], t0, t1, ALU.add, [('pl', 't0'), ('pl', 't1')], [('mrgT', blk)])
            for j_ in (ug0j, ug1j, uaj, upj):
                W.release(j_)
        if p == 0:
            dbg('mrgT', mrgT, [('mrgT', 0), ('mrgT', 1)])
        S.fence(('X1',))
        uo = [W.acquire() for _ in range(4)]
        it = 0
        for tile in range(8):
            sl = tile % 2
            xkey = ('xst', sl)
            S.dma('sp', xst[sl][:], x[p * 1024 + tile * 128: p * 1024 + (tile + 1) * 128, :], (), [xkey], xkey)
            for nh in range(2):
                bank = it % 4
                s2 = it % 2
                it += 1
                for j2 in range(2):
                    u_, uk_, _ = uo[nh * 2 + j2]
                    for kc in range(8):
                        S.mm(PS[bank][:, j2 * 256:(j2 + 1) * 256], mrgT[:, kc, tile * 128:(tile + 1) * 128], u_[:, kc, :],
                             kc == 0, kc == 7, [('mrgT', tile // 4), uk_], [psk(bank)])
                nsl = slice(nh * 512, (nh + 1) * 512)
                S.tt('dve', wtmp[s2], PS[bank][:, 0:512], g1b[:, nsl], ALU.mult, [psk(bank), 'g1b'], [('pl', 'wtmp', s2)])
                S.tt('pool', x1[:, tile, nsl], wtmp[s2], xst[sl][:, nsl], ALU.add, [('pl', 'wtmp', s2), xkey], [('x1', tile)])
        for (_, _, j_) in uo:
            W.release(j_)
        if p == 0:
            dbg('x1', x1[:, 0, :], [('x1', 0)])

        S.fence(('PL',))
        S.dma('sp', g1b[:], modrows[p, 0:1, :].to_broadcast([128, 1024]), [('modrows', p)], ['g1b'], 'g1b')
        S.dma('sp', xst[0][:], modrows[p, 1:2, :].to_broadcast([128, 1024]), [('modrows', p)], [('xst', 0)], ('xst', 0))
        norm_T(p, 'x1', (a2T, 'a2T'), sh2, 'hT')
        S.fence(('PL', 'ACC', 'K'))
        lgp = PS[7][:, 0:256].rearrange("p (t n) -> p t n", t=8)
        for tile in range(8):
            for kc in range(8):
                S.mm(lgp[:, tile, 0:20], hT[:, kc, tile * 128:(tile + 1) * 128], rw[:, kc, :], kc == 0, kc == 7,
                     [('hT', tile // 4), 'rw'], [psk(7)])
        R_ = PL[:, 0:2560]

        def rbuf(i, n):
            return R_[:, i * 160: i * 160 + 8 * n].rearrange("p (t n) -> p t n", t=8)

        lg = rbuf(0, 20)
        S.tt('dve', lg, lgp[:, :, 0:20], rb_b[:].rearrange("p (o n) -> p o n", o=1).to_broadcast([128, 8, 20]), ALU.add,
             [psk(7), 'rb_b'], [('pl', 'lg')])
        gl = lg[:, :, 0:4]
        el = lg[:, :, 4:20]
        gmax = rbuf(1, 1)
        S.red(gmax[:, :, 0], gl, ALU.max, [('pl', 'lg')], [('pl', 'gmax')])
        ge = rbuf(2, 4)
        S.tt('dve', ge, gl, gmax.to_broadcast([128, 8, 4]), ALU.subtract, [('pl', 'lg'), ('pl', 'gmax')], [('pl', 'ge')])
        eg = rbuf(3, 4)
        S.act(eg, ge, AF.Exp, [('pl', 'ge')], [('pl', 'eg')])
        gsum = rbuf(4, 1)
        S.red(gsum[:, :, 0], eg, ALU.add, [('pl', 'eg')], [('pl', 'gsum')])
        gw = rbuf(5, 1)
        S.recip(gw, gsum, [('pl', 'gsum')], [('pl', 'gw')])
        pen = rbuf(6, 4)
        S.ts('dve', pen, ge, 0.0, None, ALU.is_ge, None, [('pl', 'ge')], [('pl', 'pen')])
        S.ts('dve', pen, pen, -1.0, BIG, ALU.add, ALU.mult, [('pl', 'pen')], [('pl', 'pen')])
        msk = rbuf(7, 16)
        pen4 = R_[:, 6 * 160: 6 * 160 + 32].rearrange("p (t g o) -> p t g o", t=8, o=1).to_broadcast([128, 8, 4, 4])
        S.tt('dve', msk.rearrange("p t (g e) -> p t g e", g=4), el.rearrange("p t (g e) -> p t g e", g=4), pen4, ALU.add,
             [('pl', 'lg'), ('pl', 'pen')], [('pl', 'msk')])
        m1 = rbuf(8, 1)
        S.red(m1[:, :, 0], msk, ALU.max, [('pl', 'msk')], [('pl', 'm1')])
        o1 = rbuf(9, 16)
        S.tt('dve', o1, msk, m1.to_broadcast([128, 8, 16]), ALU.subtract, [('pl', 'msk'), ('pl', 'm1')], [('pl', 'o1')])
        S.ts('dve', o1, o1, 0.0, None, ALU.is_ge, None, [('pl', 'o1')], [('pl', 'o1')])
        msk2 = rbuf(10, 16)
        S.stt(msk2, o1, -BIG, msk, ALU.mult, ALU.add, [('pl', 'o1'), ('pl', 'msk')], [('pl', 'msk2')])
        m2 = rbuf(11, 1)
        S.red(m2[:, :, 0], msk2, ALU.max, [('pl', 'msk2')], [('pl', 'm2')])
        o2 = rbuf(12, 16)
        S.tt('dve', o2, msk2, m2.to_broadcast([128, 8, 16]), ALU.subtract, [('pl', 'msk2'), ('pl', 'm2')], [('pl', 'o2')])
        S.ts('dve', o2, o2, 0.0, None, ALU.is_ge, None, [('pl', 'o2')], [('pl', 'o2')])
        e21 = rbuf(4, 1)
        S.tt('dve', e21, m2, m1, ALU.subtract, [('pl', 'm2'), ('pl', 'm1'), ('pl', 'gw')], [('pl', 'gsum')])
        S.act(e21, e21, AF.Exp, [('pl', 'gsum')], [('pl', 'gsum')])
        den = rbuf(1, 1)
        S.ts('dve', den, e21, 1.0, None, ALU.add, None, [('pl', 'gsum'), ('pl', 'ge')], [('pl', 'gmax')])
        S.recip(den, den, [('pl', 'gmax')], [('pl', 'gmax')])
        w1 = rbuf(2, 1)
        S.tt('dve', w1, den, gw, ALU.mult, [('pl', 'gmax'), ('pl', 'gw'), ('pl', 'pen'), ('pl', 'eg')], [('pl', 'ge')])
        w2 = rbuf(3, 1)
        S.tt('dve', w2, w1, e21, ALU.mult, [('pl', 'ge'), ('pl', 'gsum')], [('pl', 'eg')])
        tsl = slice(p * 8, (p + 1) * 8)
        S.cp('pool', O1g[:, tsl, :], o1, [('pl', 'o1')], [('O1g', p)])
        S.cp('pool', O2g[:, tsl, :], o2, [('pl', 'o2')], [('O2g', p)])
        S.tt('dve', Mb[:, tsl, :], o1, o2, ALU.add, [('pl', 'o1'), ('pl', 'o2')], [('Mb', p)])
        S.cp('dve', W1g[:, tsl, :], w1, [('pl', 'ge')], [('W1g', p)])
        S.cp('dve', W2g[:, tsl, :], w2, [('pl', 'eg')], [('W2g', p)])
        if p == 0:
            dbg('gates', O1g[:, 0, :], [('O1g', 0)])
            dbg('h2T', hT[:], [('hT', 0), ('hT', 1)])

    def routing():
        S.fence(('PL',))
        rankp = PS[0][:, 0:256].rearrange("p (t e) -> p t e", t=16)
        cntp = PS[1][:, 0:16]
        mk = [('Mb', 0), ('Mb', 1)]
        for T in range(16):
            S.mm(rankp[:, T, :], trib[:], Mb[:, T, :], True, T == 0, mk + ['trib'], [psk(0)])
            for T2 in range(T):
                S.mm(rankp[:, T, :], onesb[:], Mb[:, T2, :], False, T2 == T - 1, mk + ['onesb'], [psk(0)])
        for T in range(16):
            S.mm(cntp, onesb[:], Mb[:, T, :], T == 0, T == 15, mk + ['onesb'], [psk(1)])
        R_ = PL[:, 0:4096]
        off = [0]
        nbuf = [0]

        def ra(n):
            v = R_[:, off[0]:off[0] + n]
            k = ('pl', 'r', nbuf[0])
            off[0] += n
            nbuf[0] += 1
            assert off[0] <= 4096
            return v, k

        def b3(ap2, shape):
            return ap2.rearrange("p (o n) -> p o n", o=1).to_broadcast(shape)

        def l3(ap2, shape):
            return ap2.rearrange("p (n o) -> p n o", o=1).to_broadcast(shape)

        cnt, kcnt = ra(16)
        S.cp('dve', cnt, cntp, [psk(1)], [kcnt])
        cmp, kcmp = ra(256)
        cmp3 = cmp.rearrange("p (e k) -> p e k", e=16)
        S.tt('dve', cmp3, l3(cnt, [128, 16, 16]), b3(thr[:], [128, 16, 16]), ALU.is_gt, [kcnt, 'thr'], [kcmp])
        ntl, kntl = ra(16)
        S.red(ntl, cmp3, ALU.add, [kcmp], [kntl])
        thr2 = rtab[:, 0:13]
        eidx = rtab[:, 16:32]
        ocmp, kocmp = ra(208)
        ocmp3 = ocmp.rearrange("p (e q) -> p e q", e=16)
        S.tt('dve', ocmp3, l3(cnt, [128, 16, 13]), b3(thr2, [128, 16, 13]), ALU.is_gt, [kcnt, 'rtab'], [kocmp])
        ovt, kovt = ra(16)
        S.red(ovt, ocmp3, ALU.add, [kocmp], [kovt])
        ones16, kones16 = ra(16)
        S.memset('dve', ones16, 1.0, [kones16])
        ovincl, kovincl = ra(16)
        S.add('dve', lambda e: e.tensor_tensor_scan(ovincl, ones16, ovt, 0.0, ALU.mult, ALU.add), [kones16, kovt], [kovincl])
        ovb, kovb = ra(16)
        S.tt('dve', ovb, ovincl, ovt, ALU.subtract, [kovincl, kovt], [kovb])
        prod, kprod = ra(256)
        prod3 = prod.rearrange("p (t e) -> p t e", t=16)
        sf, ksf = ra(32)
        sf3 = sf.rearrange("p (k t) -> p k t", k=2)
        for k, (Og, okey) in enumerate(((O1g, 'O1g'), (O2g, 'O2g'))):
            ok2 = [(okey, 0), (okey, 1)]
            rk, krk = ra(16)
            S.tt('dve', prod3, Og[:], rankp, ALU.mult, ok2 + [psk(0)], [kprod])
            S.red(rk, prod3, ALU.add, [kprod], [krk])
            Ek, kE = ra(16)
            S.tt('dve', prod3, Og[:], b3(eidx, [128, 16, 16]), ALU.mult, ok2 + ['rtab'], [kprod])
            S.red(Ek, prod3, ALU.add, [kprod], [kE])
            OBk, kOB = ra(16)
            S.tt('dve', prod3, Og[:], b3(ovb, [128, 16, 16]), ALU.mult, ok2 + [kovb], [kprod])
            S.red(OBk, prod3, ALU.add, [kprod], [kOB])
            mainp, kmain = ra(16)
            S.stt(mainp, Ek, 384.0, rk, ALU.mult, ALU.add, [kE, krk], [kmain])
            tailp, ktail = ra(16)
            S.stt(tailp, OBk, 256.0, rk, ALU.mult, ALU.add, [kOB, krk], [ktail])
            S.ts('dve', tailp, tailp, float(NMAIN * 128 - 384), None, ALU.add, None, [ktail], [ktail])
            S.tt('dve', tailp, tailp, mainp, ALU.subtract, [ktail, kmain], [ktail])
            isov, kisov = ra(16)
            S.ts('dve', isov, rk, 384.0, None, ALU.is_ge, None, [krk], [kisov])
            S.tt('dve', tailp, tailp, isov, ALU.mult, [ktail, kisov], [ktail])
            S.tt('dve', sf3[:, k, :], mainp, tailp, ALU.add, [kmain, ktail], [(ksf, k)])
        S.cp('dve', s12i[:], sf3, [(ksf, 0), (ksf, 1)], ['s12i'])
        cmp2, kcmp2 = ra(28 * 16)
        cmp23 = cmp2.rearrange("p (t e) -> p t e", t=28)
        S.tt('dve', cmp23, b3(ovincl, [128, 28, 16]), l3(tval[:, 0:28], [128, 28, 16]), ALU.is_le, [kovincl, 'tval'], [kcmp2])
        etf, ketf = ra(28)
        S.red(etf, cmp23, ALU.add, [kcmp2], [ketf])
        skp, kskp = ra(28)
        S.memset('dve', skp[:, 0:2], 0.0, [kskp])
        S.tt('dve', skp[:, 2:28], etf[:, 2:28], etf[:, 0:26], ALU.is_equal, [ketf], [kskp])
        wifb, kwif = ra(56)
        wif = wifb.rearrange("p (t h) -> p t h", h=2)
        S.ts('dve', wif[:, :, 0], etf, 256.0, p2col[:, 0:1], ALU.mult, ALU.add, [ketf, 'p2col'], [kwif])
        S.stt(wif[:, :, 0], skp, 65536.0, wif[:, :, 0], ALU.mult, ALU.add, [kskp, kwif], [kwif])
        S.ts('dve', wif[:, :, 1], wif[:, :, 0], 1.0, None, ALU.add, None, [kwif], [kwif])
        S.cp('dve', widx[:], wif, [kwif], ['widx'])
        zt = mixT[:].rearrange("p g t -> p (g t)").bitcast(I32)[:, 0:1280]
        mk2 = [('mixT', 0), ('mixT', 1)]
        S.memset('dve', zt, 0, mk2)
        S.dma('sp', slot_tok.rearrange("(p r) c -> p (r c)", p=128), zt, mk2, ['stok0'], 'stok0')
        for T in range(16):
            for k in range(2):
                S.add('pool', (lambda e, T=T, k=k: e.indirect_dma_start(
                    out=slot_tok[:, :], out_offset=bass.IndirectOffsetOnAxis(ap=s12i[:, k, T:T + 1].bitcast(U32), axis=0),
                    in_=tok16[:, T, :], in_offset=None)),
                    ['stok0', 's12i', 'tok16'], [('stok', T, k)], dma='sct')

    X1b_ = X1[:].bitcast(BF16)
    ringflat = ring[:].rearrange("p s n -> p (s n)")
    EW = [[X1b_[:, m * 4096:(m + 1) * 4096] for m in range(3)],
          [accb[:, m * 4096:(m + 1) * 4096] for m in range(3)],
          [ringflat[:, m * 4096:(m + 1) * 4096] for m in range(3)]]
    wmats = (ew_gate, ew_up, ew_down)

    def L_static(e, mats=range(3)):
        b = e % 3
        for m in mats:
            src = wmats[m][e].rearrange("k n -> (k n)").rearrange("(p c) -> p c", p=128)
            for h in range(2):
                wk_ = [('ew%d' % b, m, h)]
                if b == 2:
                    wk_.append(('w', 2 * m + h))
                S.dma('pool', EW[b][m][:, h * 2048:(h + 1) * 2048], src[:, h * 2048:(h + 1) * 2048], (),
                      wk_, ('ew', b, m, h))

    def moe_prefetch():
        S.fence(('X1', 'ACC'))
        L_static(0)
        L_static(1)
        L_static(2)

    def moe_sparse():
        S.fence(('K', 'H', 'PL'))
        NG = 4
        G = [KR[:, i * 1024:(i + 1) * 1024] for i in range(NG)]
        hsT = [KR[:, 4096 + i * 1024: 5120 + i * 1024].rearrange("p (k s) -> p k s", k=8) for i in range(2)]
        sil = [KR[:, 6144 + i * 512: 6656 + i * 512] for i in range(2)]
        hidb = [KR[:, 7168 + i * 512: 7680 + i * 512] for i in range(2)]
        hidT = [KR[:, 8192 + i * 512: 8704 + i * 512].rearrange("p (k s) -> p k s", k=4) for i in range(2)]
        hTf = hT[:].rearrange("p c t -> p (c t)").bitcast(F32)
        Yst = [hTf[:, i * 1024:(i + 1) * 1024] for i in range(2)]
        wsrc = [w_.rearrange("e k n -> (e k n)").rearrange("(r c) -> r c", c=2048) for w_ in wmats]
        stok_keys = [('stok', T, k) for T in range(16) for k in range(2)]
        h2keys = [('h2d', p_, t_) for p_ in range(2) for t_ in range(8)]
        GCH = 16
        for c0 in range(0, NTS, GCH):
            c1 = min(NTS, c0 + GCH)
            S.dma('sp', gidx[:, c0:c1], slot_tok[c0 * 128:c1 * 128, 0:1].rearrange("(t p) c -> p (t c)", p=128),
                  stok_keys + ['p2col'], [('gidx', c0 // GCH)], ('gidx', c0 // GCH), slow=True)
        bcreg = {}

        def wbuf(t):
            return (t // 3) % 3 if t < NMAIN else ((t - NMAIN) // 2) % 2

        def wload(e, i, m, h, b):
            if 'r' not in bcreg:
                bcreg['r'] = e.alloc_register("wbound")
                e.reg_mov(bcreg['r'], 4095)
            return e.indirect_dma_start(
                out=EW[b][m][:, h * 2048:(h + 1) * 2048], out_offset=None, in_=wsrc[m],
                in_offset=bass.IndirectOffsetOnAxis(ap=widx[:, i, h:h + 1].bitcast(U32), axis=0),
                bounds_check=bcreg['r'], oob_is_err=False)

        def L_dyn(i, mats=range(3)):
            b = i % 2
            for m in mats:
                for h in range(2):
                    S.add('pool', (lambda e, i=i, m=m, h=h, b=b: wload(e, i, m, h, b)),
                          ['widx'], [('ew%d' % b, m, h)], dma=('ew', b, m, h))

        def L_g(t):
            b = t % NG
            S.add('pool', (lambda e, t=t, b=b: e.indirect_dma_start(
                out=G[b], out_offset=None, in_=h2d[:, :],
                in_offset=bass.IndirectOffsetOnAxis(ap=gidx[:, t:t + 1].bitcast(U32), axis=0))),
                [('gidx', t // GCH)] + h2keys, [('mg', 'G', b)], dma=('G', b))

        def T1(t):
            b = t % 2
            gb = t % NG
            bv = PSb[b].rearrange("p (k s) -> p k s", k=8)
            g3 = G[gb].rearrange("s (p k) -> s k p", k=8)
            for kc in range(8):
                S.tr(bv[:, kc, :], g3[:, kc, :], ident[:], [('mg', 'G', gb), 'ident'], [psk(b)])
            S.cp('act', hsT[b], bv, [psk(b)], [('mg', 'hsT', b)])

        def A_(t):
            b = t % 2
            wb = wbuf(t)
            for kc in range(8):
                S.mm(PS[2][:, 0:512], hsT[b][:, kc, :], EW[wb][0][:, kc * 512:(kc + 1) * 512], kc == 0, kc == 7,
                     [('mg', 'hsT', b), ('ew%d' % wb, 0, kc // 4)], [psk(2)])
            for kc in range(8):
                S.mm(PS[3][:, 0:512], hsT[b][:, kc, :], EW[wb][1][:, kc * 512:(kc + 1) * 512], kc == 0, kc == 7,
                     [('mg', 'hsT', b), ('ew%d' % wb, 1, kc // 4)], [psk(3)])
            S.act(sil[b], PS[2][:, 0:512], AF.Silu, [psk(2)], [('mg', 'sil', b)])
            S.tt('dve', hidb[b], PS[3][:, 0:512], sil[b], ALU.mult, [psk(3), ('mg', 'sil', b)], [('mg', 'hid', b)])

        def T2D(t):
            b = t % 2
            wb = wbuf(t)
            bv = PSb[4].rearrange("p (k s) -> p k s", k=8)
            h3 = hidb[b].rearrange("s (p k) -> s k p", k=4)
            for fc in range(4):
                S.tr(bv[:, fc, :], h3[:, fc, :], ident[:], [('mg', 'hid', b), 'ident'], [psk(4)])
            S.cp('act', hidT[b], bv[:, 0:4, :], [psk(4)], [('mg', 'hidT', b)])
            for nh in range(2):
                for fc in range(4):
                    S.mm(PS[5 + nh][:, 0:512], hidT[b][:, fc, :], EW[wb][2][:, fc * 1024 + nh * 512:fc * 1024 + (nh + 1) * 512],
                         fc == 0, fc == 3, [('mg', 'hidT', b), ('ew%d' % wb, 2, fc // 2)], [psk(5 + nh)])
            S.cp('act', Yst[b][:, 0:512], PS[5][:, 0:512], [psk(5)], [('yst', b, 0)])
            S.cp('dve', Yst[b][:, 512:1024], PS[6][:, 0:512], [psk(6)], [('yst', b, 1)])
            S.dma('sp', yslot[t * 128:(t + 1) * 128, :], Yst[b], [('yst', b, 0), ('yst', b, 1)], [('ysl', t)], ('yso', b))

        def loads_after_A(t):
            if t < NMAIN:
                e, j = divmod(t, 3)
                if j == 2 and e + 3 < 16:
                    L_static(e + 3, (0, 1))
                if j == 2 and e == 15:
                    L_dyn(0, (0, 1))
                    L_dyn(1, (0, 1))
            else:
                i, j = divmod(t - NMAIN, 2)
                if j == 1 and i + 2 < NTB:
                    L_dyn(i + 2, (0, 1))

        def loads_after_D(t):
            if t < NMAIN:
                e, j = divmod(t, 3)
                if j == 2 and e + 3 < 16:
                    L_static(e + 3, (2,))
                if j == 2 and e == 15:
                    L_dyn(0, (2,))
                    L_dyn(1, (2,))
            else:
                i, j = divmod(t - NMAIN, 2)
                if j == 1 and i + 2 < NTB:
                    L_dyn(i + 2, (2,))

        for t in range(3):
            L_g(t)
        T1(0)
        for s_ in range(NTS + 1):
            if s_ + 3 < NTS:
                L_g(s_ + 3)
            if s_ + 1 < NTS:
                T1(s_ + 1)
            if s_ < NTS:
                A_(s_)
                loads_after_A(s_)
            if 0 <= s_ - 1 < NTS:
                T2D(s_ - 1)
                loads_after_D(s_ - 1)

    def final(p):
        S.fence(('X1', 'PL'))
        S.dma('sp', g2b[:], g2row[p:p + 1, :].to_broadcast([128, 1024]), [('g2row', p)], ['g2b'], 'g2b')
        FR = [[X1[:, (k * 2 + i) * 1024:(k * 2 + i + 1) * 1024] for i in range(2)] for k in range(2)]
        ysl_keys = [('ysl', t) for t in range(NTS)]
        for tile in range(8):
            T = p * 8 + tile
            sl = tile % 2
            xkey = ('xst', sl)
            S.dma('sp', xst[sl][:], x1d[p * 1024 + tile * 128: p * 1024 + (tile + 1) * 128, :],
                  [('x1d', p, t_) for t_ in range(8)], [xkey], xkey)
            for k in range(2):
                S.add('pool', (lambda e, k=k, T=T, sl=sl: e.indirect_dma_start(
                    out=FR[k][sl], out_offset=None, in_=yslot[:, :],
                    in_offset=bass.IndirectOffsetOnAxis(ap=s12i[:, k, T:T + 1].bitcast(U32), axis=0))),
                    ['s12i'] + ysl_keys, [('fr', k, sl)], dma=('fr', k, sl))
            r1, r2 = FR[0][sl], FR[1][sl]
            S.act(r1, r1, AF.Copy, [('fr', 0, sl), ('W1g', p)], [('fr', 0, sl)], scale=W1g[:, T, :])
            S.stt(r1, r2, W2g[:, T, :], r1, ALU.mult, ALU.add, [('fr', 1, sl), ('fr', 0, sl), ('W2g', p)], [('fr', 0, sl)])
            S.tt('dve', r1, r1, g2b[:], ALU.mult, [('fr', 0, sl), 'g2b'], [('fr', 0, sl)])
            S.tt('dve', r1, r1, xst[sl][:], ALU.add, [('fr', 0, sl), xkey], [('fr', 0, sl)])
            S.dma('sp', y[p * 1024 + tile * 128: p * 1024 + (tile + 1) * 128, :], r1, [('fr', 0, sl)], [], ('yo', sl))

    run_pass(0)
    run_pass(1)
    moe_prefetch()
    routing()
    moe_sparse()
    final(0)
    final(1)
    S.emit()
    return nc, S


_CACHE = {}


def _consts():
    rows = 1024 // 64
    row_ids = np.repeat(np.arange(rows, dtype=np.float32), 64)
    col_ids = np.tile(np.arange(64, dtype=np.float32), rows)
    inv_freq = np.power(np.float32(10000.0), -np.arange(16, dtype=np.float32) / np.float32(16)).astype(np.float32)
    ang_r = row_ids[:, None] * inv_freq[None, :]
    ang_c = col_ids[:, None] * inv_freq[None, :]
    ang = np.concatenate([ang_r, ang_r, ang_c, ang_c], axis=-1).astype(np.float32)
    cos = np.cos(ang).astype(np.float32)
    sin = np.sin(ang).astype(np.float32)
    sgn = np.concatenate([-np.ones(16), np.ones(16), -np.ones(16), np.ones(16)]).astype(np.float32)
    sin_f = (sin * sgn[None, :]).astype(np.float32)
    rcb = np.zeros((1, 64), np.float32)
    for g, w in enumerate((2, 4, 8, 16)):
        hw = w // 2
        for t in range(hw):
            rcb[0, g * 16 + t] = 1.0 / (t + hw)
            rcb[0, g * 16 + 8 + t] = 1.0 / (w - t)
    return cos, sin_f, rcb, np.eye(128, dtype=np.float32)


def kernel(x_prompt, x_sample, c, cache_k, cache_v, c_ctx, w_ada, b_ada, norm1_g, w_in, b_gate,
           q_norm_g, k_norm_g, lambda_q1, lambda_k1, lambda_q2, lambda_k2, subln_g, pool_w, pool_scale,
           w_br_attn, w_br_pool, w_out, norm2_g, router_group_w, router_group_b, router_expert_w,
           router_expert_b, expert_w_gate, expert_w_up, expert_w_down, _dump=None):
    f = lambda a: np.ascontiguousarray(np.asarray(a, dtype=np.float32))
    key = tuple(_dump) if _dump else None
    if key not in _CACHE:
        _CACHE[key] = build_program(_dump)
    nc, S = _CACHE[key]
    cos, sin_f, rcb, eye = _consts()
    x_prompt = f(x_prompt); x_sample = f(x_sample); c = f(c); c_ctx = f(c_ctx)
    cache_k = f(cache_k); cache_v = f(cache_v)
    shared = {
        "w_ada": f(w_ada)[0], "b_ada": f(b_ada), "norm1_g": f(norm1_g), "w_in": f(w_in)[0], "b_gate": f(b_gate),
        "q_norm_g": f(q_norm_g), "k_norm_g": f(k_norm_g),
        "lam4": np.concatenate([f(lambda_q1), f(lambda_k1), f(lambda_q2), f(lambda_k2)], axis=1),
        "subln_g": f(subln_g), "pool_w": f(pool_w)[0], "pool_scale": f(pool_scale),
        "w_br_attn": f(w_br_attn)[0], "w_br_pool": f(w_br_pool)[0], "w_out": f(w_out)[0], "norm2_g": f(norm2_g),
        "router_w": np.concatenate([f(router_group_w)[0], f(router_expert_w)[0]], axis=1),
        "router_b": np.concatenate([f(router_group_b), f(router_expert_b)], axis=1),
        "ew_gate": f(expert_w_gate)[0], "ew_up": f(expert_w_up)[0], "ew_down": f(expert_w_down)[0],
        "ident": eye, "cos_t": cos, "sin_t": sin_f, "rcb": rcb,
        "tri": np.triu(np.ones((128, 128), np.float32), k=1),
        "thr": (128.0 * np.arange(16, dtype=np.float32)).reshape(1, 16),
        "tval": np.arange(48, dtype=np.float32).reshape(1, 48),
        "rtab": np.concatenate([384.0 + 256.0 * np.arange(7, dtype=np.float32), np.full(6, 1.0e9, np.float32), np.zeros(3, np.float32),
                                np.arange(16, dtype=np.float32), np.zeros(32, np.float32)]).reshape(1, 64),
    }
    in_maps = []
    for i in range(NCORES):
        m = dict(shared)
        m["x"] = np.concatenate([x_prompt[4 * i:4 * i + 4].reshape(1024, 1024), x_sample[i]], axis=0)
        m["cond"] = np.stack([c_ctx, c[i]], axis=0)
        m["ck"] = cache_k[i, 0].reshape(512, 1024)
        m["cv"] = cache_v[i, 0].reshape(512, 1024)
        in_maps.append(m)
    res = run_bass_kernel_spmd(nc, in_maps, core_ids=list(range(NCORES)))
    R = res.results
    y_prompt = np.concatenate([R[i]["y"][0:1024].reshape(4, 256, 1024) for i in range(NCORES)], axis=0)
    y_sample = np.stack([R[i]["y"][1024:2048] for i in range(NCORES)], axis=0)
    new_k = np.concatenate([R[i]["nk"].reshape(4, 1, 256, 8, 128) for i in range(NCORES)], axis=0)
    new_v = np.concatenate([R[i]["nv"].reshape(4, 1, 256, 8, 128) for i in range(NCORES)], axis=0)
    if _dump:
        kernel.last_dumps = [{nm: R[i]["dbg_" + nm] for nm, _ in _dump} for i in range(NCORES)]
    return (y_prompt.astype(np.float32), y_sample.astype(np.float32), new_k.astype(np.float32), new_v.astype(np.float32))
```
